# Optimizing a Trainium2 kernel written in Bass

```python
import math
import jax, jax.numpy as jnp
from jax import lax
import numpy as np

D_MODEL = 2048
BATCH = 4
SEQ = 2048
DEPTH = 1
DEC_BATCH = 32
DEC_SEQ = 4
PAST_LEN = 16384
PAGE_SIZE = 128

MIX_WIDTH = D_MODEL
ATTN_WIDTH = MIX_WIDTH // 2
HG_WIDTH = MIX_WIDTH - ATTN_WIDTH
QK_DIM = 64
V_DIM = 2 * QK_DIM
ATTN_HEADS = ATTN_WIDTH // V_DIM
ATTN_KV_HEADS = ATTN_HEADS // 2
ATTN_GROUP = ATTN_HEADS // ATTN_KV_HEADS
HG_EXPAND = 128
HG_V_DIM = 128
HG_HEADS = HG_WIDTH // HG_V_DIM
HG_F_DIM = HG_HEADS * HG_EXPAND
Q_COLS = ATTN_HEADS * 2 * QK_DIM
K_COLS = ATTN_KV_HEADS * 2 * QK_DIM
V_COLS = ATTN_KV_HEADS * V_DIM
IN_COLS = Q_COLS + K_COLS + V_COLS + 2 * HG_F_DIM + 2 * HG_WIDTH
Q_BLOCK = 128
HG_CHUNK = 64
N_GROUPS = 4
EXPERTS_PER_GROUP = 4
N_EXPERTS = N_GROUPS * EXPERTS_PER_GROUP
TOP_K_INNER = 2
D_FF_EXPERT = D_MODEL // 4
NORM_EPS = 1e-6
ALIBI_MAX_BIAS = 8.0

kernel_name = "hymba_diffattn_hgrn2_hmoe_step"


def _rmsnorm(x, g):
    xf = x.astype(jnp.float32)
    y = xf * lax.rsqrt(jnp.mean(xf * xf, axis=-1, keepdims=True) + NORM_EPS)
    return (y * g.astype(jnp.float32)).astype(x.dtype)


def _alibi_slopes():
    h = jnp.arange(1, ATTN_HEADS + 1, dtype=jnp.float32)
    return jnp.exp2(-ALIBI_MAX_BIAS * h / ATTN_HEADS).reshape(ATTN_KV_HEADS, ATTN_GROUP)


def _adaln(c, w_ada, b_ada):
    mod = jax.nn.silu(c) @ w_ada + b_ada
    return [m[:, None, :] for m in jnp.split(mod, 6, axis=-1)]


def _project(h, w_in, q_norm_g, k_norm_g, lb):
    B, T, _ = h.shape
    p = h @ w_in
    cuts = np.cumsum([Q_COLS, K_COLS, V_COLS, HG_F_DIM, HG_F_DIM, HG_WIDTH]).tolist()
    q, k, v, hq, hf, hi, hg = jnp.split(p, cuts, axis=-1)
    q = _rmsnorm(q.reshape(B, T, ATTN_KV_HEADS, ATTN_GROUP, 2, QK_DIM), q_norm_g)
    k = _rmsnorm(k.reshape(B, T, ATTN_KV_HEADS, 2, QK_DIM), k_norm_g)
    v = v.reshape(B, T, ATTN_KV_HEADS, V_DIM)
    lb = lb.reshape(HG_HEADS, HG_EXPAND)
    z = hf.reshape(B, T, HG_HEADS, HG_EXPAND).astype(jnp.float32)
    log_f = jnp.log(lb + (1.0 - lb) * jax.nn.sigmoid(z))
    hk = (1.0 - lb) * jax.nn.sigmoid(-z)
    hq = jax.nn.silu(hq.reshape(B, T, HG_HEADS, HG_EXPAND).astype(jnp.float32))
    hi = hi.reshape(B, T, HG_HEADS, HG_V_DIM)
    hg = hg.reshape(B, T, HG_HEADS, HG_V_DIM)
    return q, k, v, hq, hk, hi, log_f, hg


def _attn_logits(q, k, q_pos, k_pos, slopes):
    s = jnp.einsum('bqkgmd,bskmd->bkgmqs', q, k, preferred_element_type=jnp.float32) * (QK_DIM ** -0.5)
    dist = (q_pos[:, None] - k_pos[None, :]).astype(jnp.float32)
    s = s - slopes[None, :, :, None, None, None] * dist
    return jnp.where(dist >= 0, s, -jnp.inf)


def _diff_combine(logits, values, lam):
    sizes = [l.shape[-1] for l in logits]
    p = jax.nn.softmax(jnp.concatenate(logits, axis=-1), axis=-1)
    d = p[:, :, :, 0] - lam * p[:, :, :, 1]
    parts = [d] if len(sizes) == 1 else jnp.split(d, np.cumsum(sizes)[:-1].tolist(), axis=-1)
    out = jnp.einsum('bkgqs,bskv->bqkgv', parts[0], values[0])
    for dp, vv in zip(parts[1:], values[1:]):
        out = out + jnp.einsum('bkgqs,bskv->bqkgv', dp, vv)
    return out


def _prompt_attention(q, k, v, slopes, lam):
    B, S = q.shape[:2]
    nb = S // Q_BLOCK
    qb = q.reshape(B, nb, Q_BLOCK, ATTN_KV_HEADS, ATTN_GROUP, 2, QK_DIM).swapaxes(0, 1)
    pos = jnp.arange(S)
    qpos = pos.reshape(nb, Q_BLOCK)

    def blk(args):
        qi, qp = args
        return _diff_combine([_attn_logits(qi, k, qp, pos, slopes)], [v], lam)

    o = lax.map(blk, (qb, qpos))
    return o.swapaxes(0, 1).reshape(B, S, ATTN_KV_HEADS, ATTN_GROUP, V_DIM)


def _sample_attention(q, k, v, cache_k, cache_v, page_table, layer, slopes, lam):
    B, T = q.shape[:2]
    past = page_table.shape[1] * cache_k.shape[2]
    kp = cache_k[layer, page_table].reshape(B, past, ATTN_KV_HEADS, 2, QK_DIM)
    vp = cache_v[layer, page_table].reshape(B, past, ATTN_KV_HEADS, V_DIM)
    past_pos = jnp.arange(past)
    q_pos = past + jnp.arange(T)
    logits = [_attn_logits(q, kp, q_pos, past_pos, slopes), _attn_logits(q, k, q_pos, q_pos, slopes)]
    return _diff_combine(logits, [vp, v], lam)


def _attn_finish(o, subln_g, lam_init):
    B, T = o.shape[:2]
    o = _rmsnorm(o, subln_g) * (1.0 - lam_init)
    return o.reshape(B, T, ATTN_WIDTH)


def _hgrn2(q, k, v, log_f, s0):
    B, T, H, K = q.shape
    V = v.shape[-1]
    c = min(HG_CHUNK, T)
    n = -(-T // c)
    pad = n * c - T

    def chunks(a):
        a = jnp.pad(a.astype(jnp.float32), ((0, 0), (0, pad), (0, 0), (0, 0)))
        return a.reshape(B, n, c, H, a.shape[-1]).transpose(1, 0, 3, 2, 4)

    tri = jnp.tril(jnp.ones((c, c), dtype=bool))

    def step(S, inp):
        qc, kc, vc, gc = inp
        G = jnp.cumsum(gc, axis=2)
        inter = jnp.einsum('bhtk,bhkv->bhtv', qc * jnp.exp(G), S)
        rel = jnp.where(tri[None, None, :, :, None], G[:, :, :, None, :] - G[:, :, None, :, :], -jnp.inf)
        a = jnp.einsum('bhtk,bhsk,bhtsk->bhts', qc, kc, jnp.exp(rel))
        intra = jnp.einsum('bhts,bhsv->bhtv', a, vc)
        g_end = G[:, :, -1, :]
        S = jnp.exp(g_end)[..., None] * S + jnp.einsum('bhsk,bhsv->bhkv', kc * jnp.exp(g_end[:, :, None, :] - G), vc)
        return S, inter + intra

    S, o = lax.scan(step, s0.astype(jnp.float32), (chunks(q), chunks(k), chunks(v), chunks(log_f)))
    o = o.transpose(1, 0, 3, 2, 4).reshape(B, n * c, H, V)[:, :T]
    return o, S


def _hgrn2_finish(o, g, hg_norm_g):
    B, T = o.shape[:2]
    o = _rmsnorm(o, hg_norm_g) * jax.nn.silu(g.astype(jnp.float32))
    return o.reshape(B, T, HG_WIDTH)


def _hier_moe(h, w_rg, b_rg, w_re, b_re, w_gate, w_up, w_down):
    B, T, D = h.shape
    t = h.reshape(B * T, D)
    pg = jax.nn.softmax((t @ w_rg + b_rg).astype(jnp.float32), axis=-1)
    pg_top, g_idx = lax.top_k(pg, 1)
    le = jnp.einsum('nd,gde->nge', t, w_re) + b_re[None]
    le_sel = jnp.einsum('nge,ng->ne', le.astype(jnp.float32), jax.nn.one_hot(g_idx[:, 0], N_GROUPS, dtype=jnp.float32))
    pe_top, e_idx = lax.top_k(jax.nn.softmax(le_sel, axis=-1), TOP_K_INNER)
    pe_top = pe_top / jnp.sum(pe_top, axis=-1, keepdims=True)
    expert_id = g_idx * EXPERTS_PER_GROUP + e_idx
    combine = jnp.sum(jax.nn.one_hot(expert_id, N_EXPERTS, dtype=jnp.float32) * (pg_top * pe_top)[..., None], axis=1)
    a = jnp.einsum('nd,edf->nef', t, w_gate)
    u = jnp.einsum('nd,edf->nef', t, w_up)
    hid = jax.nn.silu(a) * u * combine[:, :, None]
    y = jnp.einsum('nef,efd->nd', hid, w_down)
    return y.reshape(B, T, D).astype(h.dtype)


def _layer(x, c, s0, attend, lb, lw):
    (n1, n2, w_ada, b_ada, w_in, qg, kg, subln_g, lam, lam_init, hg_g, w_out,
     w_rg, b_rg, w_re, b_re, w_gate, w_up, w_down) = lw
    sh1, sc1, ga1, sh2, sc2, ga2 = _adaln(c, w_ada, b_ada)
    h = _rmsnorm(x, n1) * (1.0 + sc1) + sh1
    q, k, v, hq, hk, hi, log_f, hg = _project(h, w_in, qg, kg, lb)
    a = _attn_finish(attend(q, k, v, lam), subln_g, lam_init)
    o_hg, s_new = _hgrn2(hq, hk, hi, log_f, s0)
    m = _hgrn2_finish(o_hg, hg, hg_g)
    mix = jnp.concatenate([a.astype(x.dtype), m.astype(x.dtype)], axis=-1) @ w_out
    x = x + ga1 * mix
    h2 = _rmsnorm(x, n2) * (1.0 + sc2) + sh2
    x = x + ga2 * _hier_moe(h2, w_rg, b_rg, w_re, b_re, w_gate, w_up, w_down)
    B, T = k.shape[:2]
    return x, k.reshape(B, T, ATTN_KV_HEADS, 2 * QK_DIM), v, s_new.astype(s0.dtype)


def setup_inputs(seed: int = 0) -> dict:
    key = jax.random.key(seed)
    ks = jax.random.split(key, 40)
    f32 = jnp.float32
    n_pages = PAST_LEN // PAGE_SIZE
    n_pool = (5 * DEC_BATCH * n_pages + 3) // 4

    def nrm(k, shape, scale):
        return jax.random.normal(k, shape, f32) * scale

    def gain(k, shape):
        return 1.0 + 0.01 * jax.random.normal(k, shape, f32)

    page_table = jax.random.permutation(ks[5], n_pool)[:DEC_BATCH * n_pages].reshape(DEC_BATCH, n_pages).astype(jnp.int32)
    return {
        "x_prompt": nrm(ks[0], (BATCH, SEQ, D_MODEL), 1.0),
        "x_sample": nrm(ks[1], (DEC_BATCH, DEC_SEQ, D_MODEL), 1.0),
        "cache_k": nrm(ks[2], (DEPTH, n_pool, PAGE_SIZE, ATTN_KV_HEADS, 2 * QK_DIM), 1.0),
        "cache_v": nrm(ks[3], (DEPTH, n_pool, PAGE_SIZE, ATTN_KV_HEADS, V_DIM), 1.0),
        "state_hgrn": nrm(ks[4], (DEPTH, DEC_BATCH, HG_HEADS, HG_EXPAND, HG_V_DIM), 1.0),
        "page_table": page_table,
        "c_prompt": nrm(ks[6], (BATCH, D_MODEL), 1.0),
        "c_sample": nrm(ks[7], (DEC_BATCH, D_MODEL), 1.0),
        "norm1_g": gain(ks[8], (DEPTH, D_MODEL)),
        "norm2_g": gain(ks[9], (DEPTH, D_MODEL)),
        "w_ada": nrm(ks[10], (DEPTH, D_MODEL, 6 * D_MODEL), 0.5 * D_MODEL ** -0.5),
        "b_ada": nrm(ks[11], (DEPTH, 6 * D_MODEL), 0.01),
        "w_in": nrm(ks[12], (DEPTH, D_MODEL, IN_COLS), D_MODEL ** -0.5),
        "q_norm_g": gain(ks[13], (DEPTH, QK_DIM)),
        "k_norm_g": gain(ks[14], (DEPTH, QK_DIM)),
        "lambda_q1": nrm(ks[15], (DEPTH, QK_DIM), 0.1),
        "lambda_k1": nrm(ks[16], (DEPTH, QK_DIM), 0.1),
        "lambda_q2": nrm(ks[17], (DEPTH, QK_DIM), 0.1),
        "lambda_k2": nrm(ks[18], (DEPTH, QK_DIM), 0.1),
        "subln_g": gain(ks[19], (DEPTH, V_DIM)),
        "hg_lower_bound": nrm(ks[20], (DEPTH + 1, HG_F_DIM), 0.1),
        "hg_norm_g": gain(ks[21], (DEPTH, HG_V_DIM)),
        "w_out": nrm(ks[22], (DEPTH, MIX_WIDTH, D_MODEL), MIX_WIDTH ** -0.5),
        "w_router_group": nrm(ks[23], (DEPTH, D_MODEL, N_GROUPS), D_MODEL ** -0.5),
        "b_router_group": nrm(ks[24], (DEPTH, N_GROUPS), 0.01),
        "w_router_expert": nrm(ks[25], (DEPTH, N_GROUPS, D_MODEL, EXPERTS_PER_GROUP), D_MODEL ** -0.5),
        "b_router_expert": nrm(ks[26], (DEPTH, N_GROUPS, EXPERTS_PER_GROUP), 0.01),
        "w_exp_gate": nrm(ks[27], (DEPTH, N_EXPERTS, D_MODEL, D_FF_EXPERT), D_MODEL ** -0.5),
        "w_exp_up": nrm(ks[28], (DEPTH, N_EXPERTS, D_MODEL, D_FF_EXPERT), D_MODEL ** -0.5),
        "w_exp_down": nrm(ks[29], (DEPTH, N_EXPERTS, D_FF_EXPERT, D_MODEL), D_FF_EXPERT ** -0.5),
    }


def reference(x_prompt, x_sample, cache_k, cache_v, state_hgrn, page_table, c_prompt, c_sample,
              norm1_g, norm2_g, w_ada, b_ada, w_in, q_norm_g, k_norm_g,
              lambda_q1, lambda_k1, lambda_q2, lambda_k2, subln_g, hg_lower_bound, hg_norm_g, w_out,
              w_router_group, b_router_group, w_router_expert, b_router_expert,
              w_exp_gate, w_exp_up, w_exp_down):
    slopes = _alibi_slopes()
    lbs = jnp.cumsum(jax.nn.softmax(hg_lower_bound.astype(jnp.float32), axis=0), axis=0)
    yp, ys = x_prompt, x_sample
    kp_l, vp_l, ks_l, vs_l, sp_l, ss_l = [], [], [], [], [], []
    for l in range(DEPTH):
        lam_init = 0.8 - 0.6 * math.exp(-0.3 * l)
        lam = (jnp.exp(jnp.sum(lambda_q1[l].astype(jnp.float32) * lambda_k1[l].astype(jnp.float32)))
               - jnp.exp(jnp.sum(lambda_q2[l].astype(jnp.float32) * lambda_k2[l].astype(jnp.float32))) + lam_init)
        lw = (norm1_g[l], norm2_g[l], w_ada[l], b_ada[l], w_in[l], q_norm_g[l], k_norm_g[l], subln_g[l],
              lam, lam_init, hg_norm_g[l], w_out[l], w_router_group[l], b_router_group[l],
              w_router_expert[l], b_router_expert[l], w_exp_gate[l], w_exp_up[l], w_exp_down[l])
        s0_prompt = jnp.zeros((yp.shape[0], HG_HEADS, HG_EXPAND, HG_V_DIM), yp.dtype)
        yp, kp, vp, sp = _layer(yp, c_prompt, s0_prompt,
                                lambda q, k, v, lm: _prompt_attention(q, k, v, slopes, lm), lbs[l], lw)
        ys, kn, vn, sn = _layer(ys, c_sample, state_hgrn[l],
                                lambda q, k, v, lm: _sample_attention(q, k, v, cache_k, cache_v, page_table, l, slopes, lm),
                                lbs[l], lw)
        kp_l.append(kp); vp_l.append(vp); ks_l.append(kn); vs_l.append(vn); sp_l.append(sp); ss_l.append(sn)
    new_k_prompt = jnp.stack(kp_l, axis=0)
    new_v_prompt = jnp.stack(vp_l, axis=0)
    new_k_sample = jnp.stack(ks_l, axis=0)
    new_v_sample = jnp.stack(vs_l, axis=0)
    new_state_prompt = jnp.stack(sp_l, axis=0)
    new_state_sample = jnp.stack(ss_l, axis=0)
    return (yp, ys, new_k_prompt, new_v_prompt, new_k_sample, new_v_sample, new_state_prompt, new_state_sample)
```

```python
import math
from contextlib import ExitStack
import numpy as np
import concourse.bass as bass
import concourse.mybir as mybir
from concourse.bass_utils import run_bass_kernel_spmd

F32 = mybir.dt.float32
BF16 = mybir.dt.bfloat16
I32 = mybir.dt.int32
AF = mybir.ActivationFunctionType
ALU = mybir.AluOpType
AX = mybir.AxisListType

NT = 1040
EPS = 1e-6
LAM_INIT = 0.2
SLOPES = [2.0 ** (-(h + 1)) for h in range(8)]
PAST = 16384
ENG = ["pe", "act", "dve", "pool", "sp"]
STOP_AFTER = None
DBG = {}


class _Rec:
    def __init__(self):
        self.call = None

    def __getattr__(self, name):
        def f(*a, **kw):
            self.call = (name, a, kw)
            return None
        return f


def _bind(fn):
    if fn is None:
        return None
    r = _Rec()
    fn(r)
    name, a, kw = r.call
    return lambda e: getattr(e, name)(*a, **kw)


class Sched:
    def __init__(self):
        self.ops = {e: [] for e in ENG}
        self.state = {}
        self.wE = {e: {} for e in ENG}
        self.wD = {e: {} for e in ENG}
        self.dcount = {}
        self.sig = {e: set() for e in ENG}
        self.last_real = {}

    def _deps(self, reads, writes):
        deps = []
        for k in reads:
            st = self.state.get(k)
            if st and st[0] is not None:
                deps.append((st[0], True))
            if st and isinstance(k, tuple) and k[0] == "ps":
                deps.extend((t, False) for t in st[1].values())
        for k in writes:
            st = self.state.get(k)
            if st:
                if st[0] is not None:
                    deps.append((st[0], False))
                deps.extend((t, False) for t in st[1].values())
        return deps

    def _update(self, reads, writes, tok):
        key = (tok[0], tok[1])
        for k in reads:
            st = self.state.setdefault(k, [None, {}])
            st[1][key] = tok
        for k in writes:
            self.state[k] = [tok, {}]

    def _waits(self, eng, deps):
        waits = []
        for (t, raw) in deps:
            if t[0] == "E":
                _, f, i = t
                if f == eng and (eng == "pe" or not raw):
                    continue
                if self.wE[eng].get(f, -1) >= i:
                    continue
                self.wE[eng][f] = i
                self.sig[f].add(i)
                waits.append(t)
            else:
                _, k, v = t
                if self.wD[eng].get(k, 0) >= v:
                    continue
                self.wD[eng][k] = v
                waits.append(t)
        return waits

    def op(self, eng, fn, reads=(), writes=()):
        waits = self._waits(eng, self._deps(reads, writes))
        idx = len(self.ops[eng])
        self.ops[eng].append((waits, _bind(fn), None))
        self.last_real[eng] = idx
        tok = ("E", eng, idx)
        self._update(reads, writes, tok)
        return tok

    def dma(self, q, fn, semkey, reads=(), writes=()):
        waits = self._waits(q, self._deps(reads, writes))
        v = self.dcount.get(semkey, 0) + 16
        self.dcount[semkey] = v
        self.ops[q].append((waits, _bind(fn), (semkey, v)))
        tok = ("D", semkey, v)
        self._update(reads, writes, tok)
        return tok

    def barrier(self):
        toks = [("E", e, self.last_real[e]) for e in ENG if e in self.last_real]
        for e in ENG:
            waits = []
            for t in toks:
                if t[1] != e and self.wE[e].get(t[1], -1) < t[2]:
                    self.wE[e][t[1]] = t[2]
                    self.sig[t[1]].add(t[2])
                    waits.append(t)
            for k, v in self.dcount.items():
                if k[0] == "out" and self.wD[e].get(k, 0) < v:
                    self.wD[e][k] = v
                    waits.append(("D", k, v))
            if waits:
                self.ops[e].append((waits, None, None))

    def emit(self, nc, es):
        esem = {e: es.enter_context(nc.semaphore("sem_" + e)) for e in ENG}
        dsem = {k: es.enter_context(nc.semaphore("dsem%d" % i)) for i, k in enumerate(self.dcount)}
        rank = {}
        for e in ENG:
            r = 0
            rank[e] = {}
            for i in range(len(self.ops[e])):
                if i in self.sig[e]:
                    r += 1
                    rank[e][i] = r
        block = es.enter_context(nc.Block())

        def run(e, eng):
            for i, (waits, fn, dm) in enumerate(self.ops[e]):
                for t in waits:
                    if t[0] == "E":
                        eng.wait_ge(esem[t[1]], rank[t[1]][t[2]])
                    else:
                        eng.wait_ge(dsem[t[1]], t[2])
                if fn is None:
                    continue
                ins = fn(eng)
                if dm is not None:
                    ins.then_inc(dsem[dm[0]], 16)
                elif i in self.sig[e]:
                    ins.then_inc(esem[e], 1)
            if e in ("sp", "pool", "act"):
                for k, v in self.dcount.items():
                    if k[0] == "out":
                        eng.wait_ge(dsem[k], v)

        @block.tensor
        def _(eng):
            run("pe", eng)

        @block.scalar
        def _(eng):
            run("act", eng)

        @block.vector
        def _(eng):
            run("dve", eng)

        @block.gpsimd
        def _(eng):
            run("pool", eng)

        @block.sync
        def _(eng):
            run("sp", eng)


C_ID, C_ONES, C_BONES, C_TRI, C_SCAN, C_SCANS, C_ND, C_BIASC = 0, 128, 256, 384, 448, 960, 976, 2000
NCST = 2000 + 256
ND_D0 = [0, -128, -256, -384]


def _tile_list(qb):
    return list(range(12)) if qb == 0 else list(range(16))


def make_consts(half):
    c = np.zeros((128, NCST), np.float32)
    p = np.arange(128)
    c[:, C_ID:C_ID + 128] = np.eye(128)
    c[:, C_ONES:C_ONES + 128] = 1.0
    c[:, C_BONES:C_BONES + 128] = (p[:, None] // 64 == p[None, :] // 64)
    c[:, C_TRI:C_TRI + 64] = ((p[:, None] % 64) <= np.arange(64)[None, :])
    sm = np.ones(512, np.float32)
    sm[::64] = 0.0
    c[:, C_SCAN:C_SCAN + 512] = sm[None]
    sms = np.ones(16, np.float32)
    sms[::4] = 0.0
    c[:, C_SCANS:C_SCANS + 16] = sms[None]
    u = np.arange(1024)
    dist = u[None, :] - 384 - p[:, None]
    c[:, C_ND:C_ND + 1024] = np.where(dist >= 0, -dist, -1.0e6)
    for h in range(8):
        for qb in range(2):
            for kt in range(16):
                d0 = 1024 + 512 * qb - 128 * kt
                val = 0.0 if d0 < 128 else -SLOPES[h] * (d0 - 128)
                if kt < 8 and half == 0:
                    val += -30000.0
                c[:, C_BIASC + (h * 2 + qb) * 16 + kt] = val
    return c


def make_sample_tables():
    posT = np.zeros((3, 16, 128), np.float32)
    posT[0] = np.arange(128)[None, :]
    posT[1] = 1.0
    posT[2] = np.arange(16)[:, None]
    R3 = np.zeros((3, 8, 64), np.float32)
    newb = np.zeros((4, 64), np.float32)
    for kvh in range(4):
        for m in range(2):
            for g in range(2):
                for t in range(4):
                    col = kvh * 16 + m * 8 + g * 4 + t
                    sl = SLOPES[kvh * 2 + g]
                    R3[0, :, col] = sl
                    R3[1, :, col] = -sl * (PAST + t - 128.0 * np.arange(8))
                    R3[2, :, col] = sl * 1024.0
                    for tp in range(4):
                        newb[tp, col] = -sl * (t - tp) if tp <= t else -1.0e6
    return posT.reshape(3, 2048), R3.reshape(3, 512), newb


P_N1, P_N2, P_BT, P_GQ, P_GK, P_SG, P_HGG, P_LB, P_FLAG = 0, 16, 32, 128, 129, 130, 131, 132, 148
P_GQR, P_GKR, P_LAMR, P_BRG, P_BRE = 149, 213, 277, 533, 537
NPRM = 553


def build(stop=None, small_cache=False):
    nc = bass.Bass("TRN2", target_bir_lowering=False)
    S = Sched()
    es = ExitStack()

    def din(name, shape, dt=F32):
        return nc.dram_tensor(name, list(shape), dt, kind="ExternalInput").ap()

    def dout(name, shape, dt=F32):
        return nc.dram_tensor(name, list(shape), dt, kind="ExternalOutput").ap()

    d_cst = din("cst", [128, NCST])
    d_prm = din("prm", [128, NPRM])
    d_cT = din("cT", [128, 80])
    d_wr = din("wr", [128, 320])
    d_xo = din("xo", [128, 16, 1024])
    d_xp = din("xp", [128, 16, 1024])
    d_xs = din("xs", [128, 16, 16])
    d_wada = din("wada", [24, 128, 8192])
    d_win = din("win", [18, 128, 8192])
    d_wout = din("wout", [4, 128, 8192])
    d_wgu = din("wgu", [16, 8, 128, 2048])
    d_wdn = din("wdn", [16, 4, 128, 2048])
    d_ckv = din("ckv", [128 if small_cache else 655360, 1024])
    d_pt = din("pt", [4, 128], I32)
    d_st0 = din("st0", [4, 8, 128, 128])
    d_posT = din("posT", [3, 2048])
    d_R3 = din("R3", [3, 512])
    d_newb = din("newb", [4, 64])
    out_yT = dout("yT", [128, 16 * NT])
    out_kT = dout("kTo", [128, 4 * NT])
    out_v = dout("vo", [NT, 512])
    out_s = dout("so", [128, 1024])
    out_ss = dout("sso", [4, 128, 1024])

    ARENA = 207 * 1024
    A = es.enter_context(nc.sbuf_tensor("arena", [128, ARENA // 4], F32))
    PS = [es.enter_context(nc.psum_tensor("ps%d" % i, [128, 512], F32)) for i in range(8)]

    def f32v(off, n, parts=128):
        assert off % 4 == 0
        return A[0:parts, off // 4: off // 4 + n]

    def bfv(off, n, parts=128):
        assert off % 4 == 0 and n % 2 == 0
        return A[0:parts, off // 4: off // 4 + n // 2].bitcast(BF16)

    def i32v(off, n, parts=128):
        return A[0:parts, off // 4: off // 4 + n].bitcast(I32)

    class Region:
        def __init__(self, base, size):
            self.base, self.size, self.cur = base, size, base

        def alloc(self, nbytes):
            nbytes = (nbytes + 63) // 64 * 64
            off = self.cur
            self.cur += nbytes
            assert self.cur <= self.base + self.size, ("region overflow", self.cur - self.base, self.size)
            return off

        def reset(self):
            self.cur = self.base

    R_CONST = Region(0, 22 * 1024)
    R_RING = Region(R_CONST.base + R_CONST.size, 32 * 1024)
    R_H = Region(R_RING.base + R_RING.size, 33280)
    R_AM = Region(R_H.base + R_H.size, 33280)
    R_X = Region(R_AM.base + R_AM.size, 66560)
    R_M = Region(R_X.base + R_X.size, ARENA - (R_X.base + R_X.size))

    class _Stop(Exception):
        pass

    def chk(name):
        if stop == name:
            raise _Stop()

    try:
        o_cst = R_CONST.alloc(NCST * 4)
        cst = f32v(o_cst, NCST)
        ident = cst[:, C_ID:C_ID + 128]
        ones = cst[:, C_ONES:C_ONES + 128]
        bones = cst[:, C_BONES:C_BONES + 128]
        tri = cst[:, C_TRI:C_TRI + 64]
        scanm = cst[:, C_SCAN:C_SCAN + 512]
        scanms = cst[:, C_SCANS:C_SCANS + 16]
        biasc = cst[:, C_BIASC:C_BIASC + 256]
        o_prm = R_CONST.alloc(NPRM * 4)
        prm = f32v(o_prm, NPRM)
        o_bfc = R_CONST.alloc(768 * 2)
        identb = bfv(o_bfc, 768)[:, 0:128]
        onesb = bfv(o_bfc, 768)[:, 128:256]
        zerosb = bfv(o_bfc, 768)[:, 256:768]
        o_mod = R_CONST.alloc(480 * 4)
        DBG["mod"] = o_mod
        modT = f32v(o_mod, 480).rearrange("p (c r) -> p c r", r=5)
        o_g = R_CONST.alloc(160 * 4)
        DBG["g"] = o_g
        G1 = f32v(o_g, 160)[:, 0:80].rearrange("p (c r) -> p c r", r=5)
        G2 = f32v(o_g, 160)[:, 80:160].rearrange("p (c r) -> p c r", r=5)
        o_sc = R_CONST.alloc(64 * 4)
        DBG["sc"] = o_sc
        sc = f32v(o_sc, 64)
        negM, neglam, gq8, sg8, oml, noml, mq, mk = (sc[:, i:i + 1] for i in range(8))
        omlh = sc[:, 8:16]
        nomlh = sc[:, 16:24]
        lbh = sc[:, 24:32]
        epsc = sc[:, 32:33]
        o_wr = R_CONST.alloc(320 * 4)
        wr = f32v(o_wr, 320).rearrange("p (k c) -> p k c", c=20)
        o_cT = R_CONST.alloc(80 * 4)
        cTt = f32v(o_cT, 80)
        o_sil = R_CONST.alloc(80 * 2)
        silT = bfv(o_sil, 80).rearrange("p (k r) -> p k r", r=5)

        S.dma("sp", lambda e: e.dma_start(out=cst, in_=d_cst), ("ld", "c0"), writes=["cst"])
        S.dma("sp", lambda e: e.dma_start(out=prm, in_=d_prm), ("ld", "c1"), writes=["prm"])
        S.dma("sp", lambda e: e.dma_start(out=cTt, in_=d_cT), ("ld", "c2"), writes=["cT"])
        S.dma("sp", lambda e: e.dma_start(out=wr.rearrange("p k c -> p (k c)"), in_=d_wr), ("ld", "c3"), writes=["wr"])

        S.op("dve", lambda e: e.tensor_copy(out=identb, in_=ident), ["cst"], ["bfc"])
        S.op("dve", lambda e: e.tensor_copy(out=onesb, in_=ones), ["cst"], ["bfc"])
        S.op("dve", lambda e: e.memset(zerosb, 0.0), [], ["bfc"])
        S.op("dve", lambda e: e.memset(epsc, EPS), [], ["sc"])
        o_gt = R_M.alloc(128 * 4)
        gt = f32v(o_gt, 128)
        S.op("dve", lambda e: e.tensor_tensor(out=gt, in0=prm[:, P_GQR:P_GQR + 128], in1=prm[:, P_GQR:P_GQR + 128], op=ALU.mult), ["prm"], ["gt"])
        S.op("dve", lambda e: e.tensor_reduce(out=sc[:, 6:8], in_=gt.rearrange("p (a b) -> p a b", b=64), axis=AX.X, op=ALU.max), ["gt"], ["sc"])
        S.op("dve", lambda e: e.tensor_tensor(out=negM, in0=mq, in1=mk, op=ALU.mult), ["sc"], ["sc"])
        S.op("dve", lambda e: e.tensor_scalar(out=negM, in0=negM, scalar1=1.0, scalar2=-8.0, op0=ALU.max, op1=ALU.mult), ["sc"], ["sc"])
        S.op("dve", lambda e: e.tensor_scalar(out=gq8, in0=prm[:, P_GQ:P_GQ + 1], scalar1=0.125, scalar2=None, op0=ALU.mult), ["prm"], ["sc"])
        S.op("dve", lambda e: e.tensor_scalar(out=sg8, in0=prm[:, P_SG:P_SG + 1], scalar1=1.0 - LAM_INIT, scalar2=None, op0=ALU.mult), ["prm"], ["sc"])
        o_t = R_M.alloc(256 * 4)
        tl = f32v(o_t, 256)
        lr = prm[:, P_LAMR:P_LAMR + 256]
        S.op("dve", lambda e: e.tensor_tensor(out=tl[:, 0:64], in0=lr[:, 0:64], in1=lr[:, 64:128], op=ALU.mult), ["prm"], ["tl"])
        S.op("dve", lambda e: e.tensor_tensor(out=tl[:, 64:128], in0=lr[:, 128:192], in1=lr[:, 192:256], op=ALU.mult), ["prm"], ["tl"])
        S.op("dve", lambda e: e.tensor_reduce(out=tl[:, 128:130], in_=tl[:, 0:128].rearrange("p (a b) -> p a b", b=64), axis=AX.X, op=ALU.add), ["tl"], ["tl2"])
        S.op("act", lambda e: e.activation(out=tl[:, 130:132], in_=tl[:, 128:130], func=AF.Exp), ["tl2"], ["tl3"])
        S.op("dve", lambda e: e.scalar_tensor_tensor(out=neglam, in0=tl[:, 131:132], scalar=-LAM_INIT, in1=tl[:, 130:131], op0=ALU.add, op1=ALU.subtract), ["tl3"], ["sc"])
        lb2 = prm[:, P_LB:P_LB + 16].rearrange("p (s h) -> p s h", s=2)
        S.op("dve", lambda e: e.tensor_tensor(out=tl[:, 132:140], in0=lb2[:, 0, :], in1=lb2[:, 1, :], op=ALU.subtract), ["prm"], ["tl4"])
        S.op("act", lambda e: e.activation(out=lbh, in_=tl[:, 132:140], func=AF.Sigmoid), ["tl4"], ["sc"])
        S.op("dve", lambda e: e.tensor_scalar(out=omlh, in0=lbh, scalar1=-1.0, scalar2=1.0, op0=ALU.mult, op1=ALU.add), ["sc"], ["sc"])
        S.op("dve", lambda e: e.tensor_scalar(out=nomlh, in0=omlh, scalar1=-1.0, scalar2=None, op0=ALU.mult), ["sc"], ["sc"])
        S.op("dve", lambda e: e.tensor_scalar(out=biasc, in0=biasc, scalar1=negM, scalar2=None, op0=ALU.add), ["cst", "sc"], ["cst"])
        S.op("act", lambda e: e.activation(out=silT.rearrange("p k r -> p (k r)"), in_=cTt, func=AF.Silu), ["cT"], ["silT"])

        ring_off = [R_RING.alloc(16384) for _ in range(2)]
        ring_i = [0]

        def load_w16(src_ap):
            slot = ring_i[0] % 2
            ring_i[0] += 1
            v = bfv(ring_off[slot], 8192)
            key = ("ring", slot)
            S.dma("pool", lambda e: e.dma_start(out=v.rearrange("p (a b) -> p a b", b=2048),
                                                in_=src_ap.rearrange("p (a b) -> p a b", b=2048)),
                  ("ring", slot), writes=[key])
            return v, key

        for grp in range(24):
            w, wk = load_w16(d_wada[grp])
            wv = w.rearrange("p (b k c) -> p b k c", b=4, k=16)
            for blk in range(4):
                cb = grp * 4 + blk
                for k in range(16):
                    S.op("pe", lambda e, cb=cb, blk=blk, k=k, wv=wv: e.matmul(
                        PS[0][:, cb * 5:cb * 5 + 5], lhsT=wv[:, blk, k, :], rhs=silT[:, k, :],
                        start=(k == 0), stop=(k == 15)), [wk, "silT"], [("ps", 0)])
        bT = prm[:, P_BT:P_BT + 96]
        S.op("dve", lambda e: e.tensor_tensor(out=modT, in0=PS[0][:, 0:480].rearrange("p (c r) -> p c r", r=5),
                                              in1=bT.unsqueeze(2).to_broadcast([128, 96, 5]), op=ALU.add),
             [("ps", 0), "prm"], ["modT"])
        n1b = prm[:, P_N1:P_N1 + 16].unsqueeze(2).to_broadcast([128, 16, 5])
        n2b = prm[:, P_N2:P_N2 + 16].unsqueeze(2).to_broadcast([128, 16, 5])
        S.op("dve", lambda e: e.scalar_tensor_tensor(out=G1, in0=modT[:, 16:32, :], scalar=1.0, in1=n1b, op0=ALU.add, op1=ALU.mult), ["modT", "prm"], ["G"])
        S.op("dve", lambda e: e.scalar_tensor_tensor(out=G2, in0=modT[:, 64:80, :], scalar=1.0, in1=n2b, op0=ALU.add, op1=ALU.mult), ["modT", "prm"], ["G"])
        SH1 = modT[:, 0:16, :]
        GA1 = modT[:, 32:48, :]
        SH2 = modT[:, 48:64, :]
        GA2 = modT[:, 80:96, :]
        S.barrier()
        chk("p1")
        R_M.reset()

        o_h = R_H.alloc(16 * NT * 2)
        DBG["hT"] = o_h
        hT = bfv(o_h, 16 * NT).rearrange("p (k t) -> p k t", t=NT)

        def rstd_from_ps(ps_ap, n, inv_n, out_ap, tmp_ap, rd, wr_key):
            S.op("act", lambda e: e.activation(out=out_ap, in_=ps_ap, func=AF.Ln, bias=epsc, scale=inv_n), rd + ["sc"], [wr_key])
            S.op("act", lambda e: e.activation(out=out_ap, in_=out_ap, func=AF.Exp, scale=-0.5), [wr_key], [wr_key])

        def make_hT(d_x, blocks, gmod, shmod, dst, G, SHm, name):
            o_xb = [R_M.alloc(2 * 512 * 4) for _ in range(2)]
            o_sq = [R_M.alloc(512 * 4) for _ in range(2)]
            o_rs = R_M.alloc(512 * 4)
            o_tp = [R_M.alloc(512 * 4) for _ in range(2)]
            cnt = [0]
            for (src, t0, n, rsel) in blocks:
                rs = f32v(o_rs, 512)[:, 0:n]
                for kg in range(8):
                    b = cnt[0] % 2
                    cnt[0] += 1
                    xb = f32v(o_xb[b], 1024).rearrange("p (k t) -> p k t", k=2)[:, :, 0:n]
                    S.dma("sp", lambda e, xb=xb, src=src, kg=kg: e.dma_start(out=xb, in_=src[:, kg * 2:kg * 2 + 2, :]),
                          ("ld", "xb%d" % b), writes=[("xb", b)])
                    for kk in range(2):
                        k = kg * 2 + kk
                        sb = k % 2
                        sq = f32v(o_sq[sb], 512)[:, 0:n]
                        S.op("act", lambda e, sq=sq, xb=xb, kk=kk: e.activation(out=sq, in_=xb[:, kk, :], func=AF.Square), [("xb", b)], [("sq", sb)])
                        S.op("pe", lambda e, sq=sq, k=k, n=n: e.matmul(PS[7][:, 0:n], lhsT=ones, rhs=sq, start=(k == 0), stop=(k == 15)),
                             [("sq", sb), "cst"], [("ps", 7)])
                rstd_from_ps(PS[7][:, 0:n], n, 1.0 / 2048.0, rs, rs, [("ps", 7)], "rs")
                for kg in range(8):
                    b = cnt[0] % 2
                    cnt[0] += 1
                    xb = f32v(o_xb[b], 1024).rearrange("p (k t) -> p k t", k=2)[:, :, 0:n]
                    S.dma("sp", lambda e, xb=xb, src=src, kg=kg: e.dma_start(out=xb, in_=src[:, kg * 2:kg * 2 + 2, :]),
                          ("ld", "xb%d" % b), writes=[("xb", b)])
                    for kk in range(2):
                        k = kg * 2 + kk
                        tb_ = k % 2
                        for (c0, cn, r) in rsel:
                            tp = f32v(o_tp[tb_], 512)[:, c0:c0 + cn]
                            S.op("dve", lambda e, tp=tp, xb=xb, kk=kk, k=k, r=r, c0=c0, cn=cn, rs=rs: e.scalar_tensor_tensor(
                                out=tp, in0=xb[:, kk, c0:c0 + cn], scalar=G[:, k, r:r + 1], in1=rs[:, c0:c0 + cn],
                                op0=ALU.mult, op1=ALU.mult), [("xb", b), "G", "rs"], [("tp", tb_)])
                            S.op("act", lambda e, tp=tp, k=k, r=r, c0=c0, cn=cn, t0=t0: e.activation(
                                out=dst[:, k, t0 + c0:t0 + c0 + cn], in_=tp, func=AF.Identity, bias=SHm[:, k, r:r + 1], scale=1.0),
                                [("tp", tb_), "modT"], [name])

        PBLK = [(0, 512), (512, 512)]
        SAMPLE_RSEL = [(4 * bi, 4, 1 + bi) for bi in range(4)]

        o_kT = R_X.alloc(4 * 2064 * 2)
        DBG["kT"] = o_kT
        kT = bfv(o_kT, 4 * 2064).rearrange("p (h t) -> p h t", h=4)
        o_v = R_X.alloc(16 * 512 * 2)
        DBG["vT"] = o_v
        vT = bfv(o_v, 16 * 512).rearrange("p (t c) -> p t c", c=512)
        o_vs = R_X.alloc(4 * 512 * 2)
        vS = bfv(o_vs, 4 * 512).rearrange("p (b c) -> p b c", c=512)
        o_qT = R_X.alloc(8 * NT * 2)
        DBG["qT"] = o_qT
        qT = bfv(o_qT, 8 * NT).rearrange("p (h t) -> p h t", h=8)
        x_mark = R_X.cur

        psc = [0]

        def next_ps(lo=0, hi=6):
            i = lo + psc[0] % (hi - lo)
            psc[0] += 1
            return i

        def proj_fm(wv, wk, blk, src, t0, n, post):
            pi = next_ps()
            for k in range(16):
                S.op("pe", lambda e, k=k, pi=pi: e.matmul(PS[pi][:, 0:n], lhsT=wv[:, blk, k, :], rhs=src[:, k, t0:t0 + n],
                                                          start=(k == 0), stop=(k == 15)), [wk, "hT"], [("ps", pi)])
            post(pi)

        def proj_tm(wv, wk, src, t0, m, post):
            pi = next_ps()
            for k in range(16):
                S.op("pe", lambda e, k=k, pi=pi: e.matmul(PS[pi][0:m, :], lhsT=src[:, k, t0:t0 + m], rhs=wv[:, k, :],
                                                          start=(k == 0), stop=(k == 15)), [wk, "hT"], [("ps", pi)])
            post(pi)

        tmp_off = {}

        def tmpf(name, n=512, region=None):
            if name not in tmp_off:
                tmp_off[name] = (region or R_M).alloc(n * 4)
            return f32v(tmp_off[name], n)

        def tmpb(name, n=512, region=None):
            if name not in tmp_off:
                tmp_off[name] = (region or R_M).alloc(n * 2)
            return bfv(tmp_off[name], n)

        def qk_post(pi, n, gcol, out_bf, out_f32_key=None, out_f32=None, tag="qk"):
            sq = tmpf(tag + "sq")[:, 0:n]
            rs = tmpf(tag + "rs")[:, 0:n]
            S.op("act", lambda e: e.activation(out=sq, in_=PS[pi][:, 0:n], func=AF.Square), [("ps", pi)], [tag + "sq"])
            S.op("pe", lambda e: e.matmul(PS[6][:, 0:n], lhsT=bones, rhs=sq, start=True, stop=True), [tag + "sq", "cst"], [("ps", 6)])
            rstd_from_ps(PS[6][:, 0:n], n, 1.0 / 64.0, rs, rs, [("ps", 6)], tag + "rs")
            if out_f32 is not None:
                S.op("dve", lambda e: e.scalar_tensor_tensor(out=out_f32, in0=PS[pi][:, 0:n], scalar=gcol, in1=rs, op0=ALU.mult, op1=ALU.mult),
                     [("ps", pi), tag + "rs", "sc", "prm"], [out_f32_key])
                S.op("act", lambda e: e.activation(out=out_bf[0], in_=out_f32, func=AF.Copy), [out_f32_key], [out_bf[1]])
            else:
                S.op("dve", lambda e: e.scalar_tensor_tensor(out=out_bf[0], in0=PS[pi][:, 0:n], scalar=gcol, in1=rs, op0=ALU.mult, op1=ALU.mult),
                     [("ps", pi), tag + "rs", "sc", "prm"], [out_bf[1]])

        gk = prm[:, P_GK:P_GK + 1]
        flag = prm[:, P_FLAG:P_FLAG + 1]
        hgg = prm[:, P_HGG:P_HGG + 1]

        def hgrn_bufs(region, ntok, ntile, with_q):
            d = {}
            d["kt"] = bfv(region.alloc(4 * ntok * 2), 4 * ntok).rearrange("p (h t) -> p h t", h=4)
            d["khat"] = bfv(region.alloc(ntile * 512 * 2), ntile * 512).rearrange("p (t c) -> p t c", c=512)
            d["hi"] = bfv(region.alloc(ntile * 512 * 2), ntile * 512).rearrange("p (t c) -> p t c", c=512)
            nch = ntok // 64 + 4
            d["d"] = f32v(region.alloc(4 * nch * 4), 4 * nch).rearrange("p (h c) -> p h c", h=4)
            if with_q:
                d["eg"] = bfv(region.alloc(4 * ntok * 2), 4 * ntok).rearrange("p (h t) -> p h t", h=4)
                d["qt"] = bfv(region.alloc(4 * ntok * 2), 4 * ntok).rearrange("p (h t) -> p h t", h=4)
                d["gate"] = bfv(region.alloc(4 * ntok * 2), 4 * ntok).rearrange("p (h t) -> p h t", h=4)
                d["khs"] = bfv(region.alloc(4 * 512 * 2), 4 * 512).rearrange("p (b c) -> p b c", c=512)
                d["his"] = bfv(region.alloc(4 * 512 * 2), 4 * 512).rearrange("p (b c) -> p b c", c=512)
            return d

        o_Sst = R_CONST.alloc(8 * 128 * 4)
        DBG["Sst"] = o_Sst
        Sst = f32v(o_Sst, 1024).rearrange("p (h v) -> p h v", h=8)

        def hf_post(pi, hb, hl, hglob, t0, n, with_q, is_sample, tag):
            sg = tmpf("hf_sg")[:, 0:n]
            lf = tmpf("hf_lf")[:, 0:n]
            hk = tmpf("hf_hk")[:, 0:n]
            G = tmpf("hf_G")[:, 0:n]
            eg = tmpf("hf_eg")[:, 0:n]
            kf = sg
            khT = tmpb("hf_khT")[:, 0:n]
            S.op("act", lambda e: e.activation(out=sg, in_=PS[pi][:, 0:n], func=AF.Sigmoid), [("ps", pi)], ["hf_sg"])
            S.op("dve", lambda e: e.tensor_scalar(out=lf, in0=sg, scalar1=omlh[:, hglob:hglob + 1], scalar2=lbh[:, hglob:hglob + 1],
                                                  op0=ALU.mult, op1=ALU.add), ["hf_sg", "sc"], ["hf_lf"])
            S.op("act", lambda e: e.activation(out=lf, in_=lf, func=AF.Ln), ["hf_lf"], ["hf_lf"])
            S.op("dve", lambda e: e.tensor_scalar(out=hk, in0=sg, scalar1=nomlh[:, hglob:hglob + 1], scalar2=omlh[:, hglob:hglob + 1],
                                                  op0=ALU.mult, op1=ALU.add), ["hf_sg", "sc"], ["hf_hk"])
            msk = scanms[:, 0:n] if is_sample else scanm[:, 0:n]
            S.op("dve", lambda e: e.tensor_tensor_scan(out=G, data0=msk, data1=lf, initial=0.0, op0=ALU.mult, op1=ALU.add),
                 ["hf_lf", "cst"], ["hf_G"])
            S.op("act", lambda e: e.activation(out=eg, in_=G, func=AF.Exp), ["hf_G"], ["hf_eg"])
            S.op("act", lambda e: e.activation(out=G, in_=G, func=AF.Exp, scale=-1.0), ["hf_G"], ["hf_G"])
            S.op("dve", lambda e: e.tensor_tensor(out=kf, in0=hk, in1=G, op=ALU.mult), ["hf_hk", "hf_G", "hf_sg"], ["hf_sg"])
            csz = 4 if is_sample else 64
            ncn = n // csz
            c0 = t0 // 64 if not is_sample else 16
            dv = hb["d"][:, hl, c0:c0 + ncn]
            S.op("dve", lambda e: e.tensor_copy(out=dv, in_=eg.rearrange("p (c s) -> p c s", s=csz)[:, :, csz - 1]), ["hf_eg"], [tag + "d"])
            S.op("act", lambda e: e.activation(out=hb["kt"][:, hl, t0:t0 + n], in_=kf, func=AF.Copy), ["hf_sg"], [tag + "kt"])
            if with_q:
                S.op("act", lambda e: e.activation(out=hb["eg"][:, hl, t0:t0 + n], in_=eg, func=AF.Copy), ["hf_eg"], [tag + "eg"])
            S.op("dve", lambda e: e.tensor_tensor(out=khT.rearrange("p (c s) -> p c s", s=csz), in0=kf.rearrange("p (c s) -> p c s", s=csz),
                                                  in1=dv.unsqueeze(2).to_broadcast([128, ncn, csz]), op=ALU.mult),
                 ["hf_sg", tag + "d"], ["hf_khT"])
            PSb = PS[6].bitcast(BF16)
            if not is_sample:
                for j in range(n // 128):
                    S.op("pe", lambda e, j=j: e.transpose(out=PSb[:, j * 128:(j + 1) * 128], in_=khT[:, j * 128:(j + 1) * 128], identity=identb),
                         ["hf_khT", "bfc"], [("ps", 6)])
                tl0 = t0 // 128
                S.op("act", lambda e: e.activation(out=hb["khat"][:, tl0:tl0 + n // 128, hl * 128:(hl + 1) * 128],
                                                   in_=PSb[:, 0:n].rearrange("p (j c) -> p j c", c=128), func=AF.Copy),
                     [("ps", 6)], [tag + "khat"])
            else:
                for bi in range(4):
                    S.op("pe", lambda e, bi=bi: e.transpose(out=PSb[0:4, bi * 128:(bi + 1) * 128], in_=khT[:, bi * 4:bi * 4 + 4], identity=identb),
                         ["hf_khT", "bfc"], [("ps", 6)])
                S.op("act", lambda e: e.activation(out=hb["khs"][0:4, :, hl * 128:(hl + 1) * 128],
                                                   in_=PSb[0:4, 0:512].rearrange("p (j c) -> p j c", c=128), func=AF.Copy),
                     [("ps", 6)], [tag + "khs"])

        def state_chain(hb, hl, hglob, tile0, nchunk, ch0, store_bf, tag):
            for c in range(nchunk):
                tl_, e2 = tile0 + c // 2, c % 2
                pu = next_ps()
                if store_bf is not None:
                    S.op("act", lambda e, c=c: e.activation(out=store_bf[:, c, :], in_=Sst[:, hglob, :], func=AF.Copy), [("S", hglob)], [tag + "Sbf"])
                S.op("pe", lambda e, c=c, tl_=tl_, e2=e2, pu=pu: e.matmul(
                    PS[pu][:, 0:128],
                    lhsT=hb["khat"][64 * e2:64 * e2 + 64, tl_, hl * 128:(hl + 1) * 128],
                    rhs=hb["hi"][64 * e2:64 * e2 + 64, tl_, hl * 128:(hl + 1) * 128], start=True, stop=True),
                    [tag + "khat", tag + "hi"], [("ps", pu)])
                S.op("dve", lambda e, c=c, pu=pu: e.scalar_tensor_tensor(
                    out=Sst[:, hglob, :], in0=Sst[:, hglob, :], scalar=hb["d"][:, hl, ch0 + c:ch0 + c + 1],
                    in1=PS[pu][:, 0:128], op0=ALU.mult, op1=ALU.add),
                    [("ps", pu), tag + "d", ("S", hglob)], [("S", hglob)])

        make_hT(d_xp, [(d_xp[:, :, t0:t0 + n], t0, n, [(0, n, 0)]) for (t0, n) in PBLK], G1, SH1, hT, G1, SH1, "hT")
        S.barrier()
        R_M.reset()
        for h in range(8):
            S.op("dve", lambda e, h=h: e.memset(Sst[:, h, :], 0.0), [], [("S", h)])
        R_AM_mark = R_AM.cur
        g_i = 0
        w, wk = load_w16(d_win[g_i]); g_i += 1
        wv = w.rearrange("p (b k c) -> p b k c", b=4, k=16)
        for blk in range(4):
            for (t0, n) in PBLK:
                proj_fm(wv, wk, blk, hT, t0, n, lambda pi, blk=blk, t0=t0, n=n: qk_post(
                    pi, n, gk, (kT[:, blk, t0:t0 + n], "kT"), tag="qk"))
        w, wk = load_w16(d_win[g_i]); g_i += 1
        wv = w.rearrange("p (k c) -> p k c", k=16)
        for tl_ in range(8):
            proj_tm(wv, wk, hT, tl_ * 128, 128, lambda pi, tl_=tl_: S.op(
                "act", lambda e: e.activation(out=vT[:, tl_, :], in_=PS[pi][:, :], func=AF.Copy), [("ps", pi)], ["vT"]))
        for hgp in range(2):
            hb = hgrn_bufs(R_AM, 1024, 8, False)
            w, wk = load_w16(d_win[g_i]); g_i += 1
            wv = w.rearrange("p (b k c) -> p b k c", b=4, k=16)
            for hl in range(4):
                for (t0, n) in PBLK:
                    proj_fm(wv, wk, hl, hT, t0, n, lambda pi, hl=hl, t0=t0, n=n, hb=hb: hf_post(
                        pi, hb, hl, hgp * 4 + hl, t0, n, False, False, "p"))
            w, wk = load_w16(d_win[g_i]); g_i += 1
            wv = w.rearrange("p (k c) -> p k c", k=16)
            for tl_ in range(8):
                proj_tm(wv, wk, hT, tl_ * 128, 128, lambda pi, tl_=tl_, hb=hb: S.op(
                    "act", lambda e: e.activation(out=hb["hi"][:, tl_, :], in_=PS[pi][:, :], func=AF.Copy), [("ps", pi)], ["phi"]))
            for hl in range(4):
                state_chain(hb, hl, hgp * 4 + hl, 0, 16, 0, None, "p")
            S.barrier()
            R_AM.cur = R_AM_mark
        for h in range(8):
            S.op("dve", lambda e, h=h: e.tensor_scalar(out=Sst[:, h, :], in0=Sst[:, h, :], scalar1=flag, scalar2=None, op0=ALU.mult),
                 [("S", h), "prm"], [("S", h)])
        S.barrier()
        chk("p2")
        R_M.reset()
        tmp_off.clear()

        OWN_BLOCKS = [(d_xo[:, :, t0:t0 + n], t0, n, [(0, n, 0)]) for (t0, n) in PBLK] + [(d_xs, 1024, 16, SAMPLE_RSEL)]
        make_hT(None, OWN_BLOCKS, G1, SH1, hT, G1, SH1, "hT")
        S.barrier()
        chk("p3a0")
        R_M.reset()
        TBS = [(0, 512), (512, 512), (1024, 16)]
        o_am = R_AM.alloc(16 * NT * 2)
        DBG["amT"] = o_am
        amT = bfv(o_am, 16 * NT).rearrange("p (f t) -> p f t", f=16)
        kst = [tmpf("kst0"), tmpf("kst1")]
        vst = [tmpf("vst0"), tmpf("vst1")]
        stc = [0]
        for qg in range(2):
            w, wk = load_w16(d_win[g_i]); g_i += 1
            wv = w.rearrange("p (b k c) -> p b k c", b=4, k=16)
            for blk in range(4):
                h = qg * 4 + blk
                for (t0, n) in TBS:
                    proj_fm(wv, wk, blk, hT, t0, n, lambda pi, h=h, t0=t0, n=n: qk_post(pi, n, gq8, (qT[:, h, t0:t0 + n], "qT"), tag="qk"))
        S.barrier()
        chk("p3a1")
        w, wk = load_w16(d_win[g_i]); g_i += 1
        wv = w.rearrange("p (b k c) -> p b k c", b=4, k=16)
        for blk in range(4):
            for (t0, n) in TBS:
                def kpost(pi, blk=blk, t0=t0, n=n):
                    b = stc[0] % 2
                    stc[0] += 1
                    st = kst[b][:, 0:n]
                    qk_post(pi, n, gk, (kT[:, blk, 1024 + t0:1024 + t0 + n], "kT"), out_f32_key=("kst", b), out_f32=st, tag="qk")
                    S.dma("sp", lambda e: e.dma_start(out=out_kT[:, blk * NT + t0:blk * NT + t0 + n], in_=st), ("out", "k%d" % b), reads=[("kst", b)])
                proj_fm(wv, wk, blk, hT, t0, n, kpost)
        S.barrier()
        chk("p3a2")
        w, wk = load_w16(d_win[g_i]); g_i += 1
        wv = w.rearrange("p (k c) -> p k c", k=16)
        for tl_ in range(8):
            def vpost(pi, tl_=tl_):
                b = stc[0] % 2
                stc[0] += 1
                S.op("act", lambda e: e.activation(out=vT[:, 8 + tl_, :], in_=PS[pi][:, :], func=AF.Copy), [("ps", pi)], ["vT"])
                S.op("dve", lambda e: e.tensor_copy(out=vst[b], in_=PS[pi][:, :]), [("ps", pi)], [("vst", b)])
                S.dma("sp", lambda e: e.dma_start(out=out_v[tl_ * 128:(tl_ + 1) * 128, :], in_=vst[b]), ("out", "v%d" % b), reads=[("vst", b)])
            proj_tm(wv, wk, hT, tl_ * 128, 128, vpost)
        S.barrier()
        chk("p3a3")
        for bi in range(4):
            def vspost(pi, bi=bi):
                b = stc[0] % 2
                stc[0] += 1
                S.op("act", lambda e: e.activation(out=vS[0:4, bi, :], in_=PS[pi][0:4, :], func=AF.Copy), [("ps", pi)], ["vS"])
                S.op("dve", lambda e: e.tensor_copy(out=vst[b][0:4, :], in_=PS[pi][0:4, :]), [("ps", pi)], [("vst", b)])
                S.dma("sp", lambda e: e.dma_start(out=out_v[1024 + bi * 4:1028 + bi * 4, :], in_=vst[b][0:4, :]), ("out", "v%d" % b), reads=[("vst", b)])
            proj_tm(wv, wk, hT, 1024 + bi * 4, 4, vspost)
        S.barrier()
        chk("p3a")
        R_M.reset()
        tmp_off.clear()

        att_tmp = [tmpf("att_t0"), tmpf("att_t1")]
        att_P = [tmpb("att_P0"), tmpb("att_P1"), tmpb("att_P2"), tmpb("att_P3")]
        att_o = tmpf("att_o")
        att_r = [tmpf("att_r0"), tmpf("att_r1")]
        pc = [0]

        def subln_finish(o_ap, n, out_ap, tag, gcol, extra=None, a3=None):
            sq = tmpf("sl_sq")[:, 0:n]
            rs = tmpf("sl_rs")[:, 0:n]
            o3, rs3 = o_ap, rs
            if a3 is not None:
                o3 = o_ap.rearrange("p (a b) -> p a b", a=a3)
                rs3 = rs.rearrange("p (a b) -> p a b", a=a3)
            S.op("act", lambda e: e.activation(out=sq, in_=o_ap, func=AF.Square), [tag], ["sl_sq"])
            S.op("pe", lambda e: e.matmul(PS[6][:, 0:n], lhsT=ones, rhs=sq, start=True, stop=True), ["sl_sq", "cst"], [("ps", 6)])
            rstd_from_ps(PS[6][:, 0:n], n, 1.0 / 128.0, rs, rs, [("ps", 6)], "sl_rs")
            if extra is None:
                S.op("dve", lambda e: e.scalar_tensor_tensor(out=out_ap, in0=o3, scalar=gcol, in1=rs3, op0=ALU.mult, op1=ALU.mult),
                     [tag, "sl_rs", "sc", "prm"], ["amT"])
            else:
                S.op("dve", lambda e: e.scalar_tensor_tensor(out=rs, in0=o_ap, scalar=gcol, in1=rs, op0=ALU.mult, op1=ALU.mult),
                     [tag, "sl_rs", "sc", "prm"], ["sl_rs"])
                S.op("dve", lambda e: e.tensor_tensor(out=out_ap, in0=rs, in1=extra[0], op=ALU.mult), ["sl_rs", extra[1]], ["amT"])

        att_tmp.append(tmpf("att_t2"))
        SBK = [4, 5, 7]
        units = []
        for h in range(8):
            for qb in range(2):
                tiles = _tile_list(qb)
                for ti, kt in enumerate(tiles):
                    for m in range(2):
                        units.append((h, qb, ti, kt, m, len(tiles)))

        def stage_a(i, u):
            h, qb, ti, kt, m, nt = u
            kvh = h // 2
            sb, pb = i % 3, i % 4
            d0 = 1024 + 512 * qb - 128 * kt
            ws = 512 if d0 >= 128 else d0 + 384
            ndv = cst[:, C_ND + ws:C_ND + ws + 512]
            S.op("pe", lambda e: e.matmul(
                PS[SBK[sb]][:, :], lhsT=kT[64 * m:64 * m + 64, kvh, kt * 128:(kt + 1) * 128],
                rhs=qT[64 * m:64 * m + 64, h, qb * 512:(qb + 1) * 512], start=True, stop=True),
                ["kT", "qT"], [("ps", SBK[sb])])
            S.op("dve", lambda e: e.scalar_tensor_tensor(
                out=att_tmp[sb], in0=ndv, scalar=SLOPES[h], in1=PS[SBK[sb]][:, :], op0=ALU.mult, op1=ALU.add),
                [("ps", SBK[sb]), "cst"], [("att_t", sb)])
            bc = biasc[:, (h * 2 + qb) * 16 + kt:(h * 2 + qb) * 16 + kt + 1]
            S.op("act", lambda e: e.activation(out=att_P[pb], in_=att_tmp[sb], func=AF.Exp, bias=bc, scale=1.0),
                 [("att_t", sb), "cst"], [("att_P", pb)])

        def fin1(h, qb):
            for m in range(2):
                S.op("act", lambda e, m=m: e.activation(out=att_r[m], in_=PS[2 + m][:, :], func=AF.Ln), [("ps", 2 + m)], [("att_r", m)])
                S.op("act", lambda e, m=m: e.activation(out=att_r[m], in_=att_r[m], func=AF.Exp, scale=-1.0), [("att_r", m)], [("att_r", m)])
                S.op("dve", lambda e, m=m: e.tensor_tensor(out=att_r[m], in0=PS[m][:, :], in1=att_r[m], op=ALU.mult),
                     [("ps", m), ("att_r", m)], [("att_r", m)])
            S.op("dve", lambda e: e.scalar_tensor_tensor(out=att_o, in0=att_r[1], scalar=neglam, in1=att_r[0], op0=ALU.mult, op1=ALU.add),
                 [("att_r", 0), ("att_r", 1), "sc"], ["att_o"])
            sq = tmpf("sl_sq")
            S.op("act", lambda e: e.activation(out=sq, in_=att_o, func=AF.Square), ["att_o"], ["sl_sq"])

        def fin2(h, qb):
            sq = tmpf("sl_sq")
            rs = tmpf("sl_rs")
            S.op("pe", lambda e: e.matmul(PS[6][:, :], lhsT=ones, rhs=sq, start=True, stop=True), ["sl_sq", "cst"], [("ps", 6)])
            rstd_from_ps(PS[6][:, :], 512, 1.0 / 128.0, rs, rs, [("ps", 6)], "sl_rs")
            S.op("dve", lambda e: e.scalar_tensor_tensor(out=amT[:, h, qb * 512:(qb + 1) * 512], in0=att_o, scalar=sg8, in1=rs,
                                                         op0=ALU.mult, op1=ALU.mult), ["att_o", "sl_rs", "sc", "prm"], ["amT"])

        def stage_b(i, u):
            h, qb, ti, kt, m, nt = u
            kvh = h // 2
            pb = i % 4
            S.op("pe", lambda e: e.matmul(PS[m][:, :], lhsT=vT[:, kt, kvh * 128:(kvh + 1) * 128], rhs=att_P[pb],
                                          start=(ti == 0), stop=(ti == nt - 1)), [("att_P", pb), "vT"], [("ps", m)])
            S.op("pe", lambda e: e.matmul(PS[2 + m][:, :], lhsT=onesb, rhs=att_P[pb],
                                          start=(ti == 0), stop=(ti == nt - 1)), [("att_P", pb), "bfc"], [("ps", 2 + m)])

        pend = []
        NU = len(units)
        for i in range(NU + 2):
            if i < NU:
                stage_a(i, units[i])
            if i >= 2:
                u = units[i - 2]
                stage_b(i - 2, u)
                for p_ in pend:
                    p_[2] -= 1
                while pend and pend[0][2] <= 0:
                    hh, qq, _ = pend.pop(0)
                    fin2(hh, qq)
                if u[2] == u[5] - 1 and u[4] == 1:
                    fin1(u[0], u[1])
                    pend.append([u[0], u[1], 4])
        for hh, qq, _ in pend:
            fin2(hh, qq)
        S.barrier()
        chk("p3b")
        R_M.reset()
        tmp_off.clear()

        o_pos = R_X.alloc(2048 * 4)
        posT = f32v(o_pos, 2048, 3).rearrange("p (b i) -> p b i", b=16)
        o_R3 = R_X.alloc(512 * 4)
        R3 = f32v(o_R3, 512, 3)
        o_nb = R_X.alloc(64 * 4)
        newb = f32v(o_nb, 64, 4)
        o_idx = R_X.alloc(512 * 4)
        idx = i32v(o_idx, 512)
        S.dma("sp", lambda e: e.dma_start(out=posT.rearrange("p b i -> p (b i)"), in_=d_posT), ("ld", "c0"), writes=["posT"])
        S.dma("sp", lambda e: e.dma_start(out=R3, in_=d_R3), ("ld", "c1"), writes=["R3"])
        S.dma("sp", lambda e: e.dma_start(out=newb, in_=d_newb), ("ld", "c2"), writes=["newb"])
        tmpf("sl_sq", 64)
        tmpf("sl_rs", 64)
        o_pt_ = R_M.alloc(512 * 4)
        pti = i32v(o_pt_, 512)
        ptf = f32v(o_pt_, 512)
        pid = f32v(R_M.alloc(4), 1)
        S.dma("sp", lambda e: e.dma_start(out=pti, in_=d_pt.rearrange("b n -> (b n)").partition_broadcast(128)), ("ld", "c3"), writes=["pti"])
        S.op("pool", lambda e: e.iota(pid, pattern=[[0, 1]], base=0, channel_multiplier=1, allow_small_or_imprecise_dtypes=True), [], ["pid"])
        S.op("dve", lambda e: e.tensor_copy(out=ptf, in_=pti), ["pti"], ["ptf"])
        S.op("dve", lambda e: e.tensor_scalar(out=ptf, in0=ptf, scalar1=128.0, scalar2=pid, op0=ALU.mult, op1=ALU.add), ["ptf", "pid"], ["ptf"])
        S.op("dve", lambda e: e.tensor_copy(out=idx, in_=ptf), ["ptf"], ["idx"])
        NSL = 8
        kvr = [bfv(R_M.alloc(2048), 1024) for _ in range(NSL)]
        kr = [t[:, 0:512] for t in kvr]
        vr = [t[:, 512:1024] for t in kvr]
        qbd = bfv(R_M.alloc(64 * 2), 64).rearrange("p (k c) -> p k c", k=4)
        Ps = [bfv(R_M.alloc(1024), 512) for _ in range(2)]
        Pn = bfv(R_M.alloc(128), 64, 4)
        sa_t = f32v(R_M.alloc(64 * 4), 64)
        sa_r = f32v(R_M.alloc(64 * 4), 64)
        sa_o = f32v(R_M.alloc(32 * 4), 32)
        pgc = [0]
        for bi in range(4):
            S.op("dve", lambda e: e.memset(qbd.rearrange("p k c -> p (k c)"), 0.0), [], ["qbd"])
            for kvh in range(4):
                for g in range(2):
                    for m in range(2):
                        S.op("dve", lambda e, kvh=kvh, g=g, m=m, bi=bi: e.tensor_copy(
                            out=qbd[64 * m:64 * m + 64, kvh, m * 8 + g * 4:m * 8 + g * 4 + 4],
                            in_=qT[64 * m:64 * m + 64, kvh * 2 + g, 1024 + bi * 4:1028 + bi * 4]), ["qT"], ["qbd"])
            S.op("pe", lambda e: e.matmul(PS[2][:, 0:128], lhsT=zerosb[:, 0:128], rhs=zerosb[:, 0:128], start=True, stop=False),
                 ["bfc"], [("ps", 2)])
            def sa_stage_a(B4):
                sb = B4 % 2
                B8, r0 = B4 // 2, (B4 % 2) * 4
                S.op("pe", lambda e: e.matmul(PS[sb][:, 0:256], lhsT=posT[:, B8, :], rhs=R3[:, r0 * 64:(r0 + 4) * 64], start=True, stop=False),
                     ["posT", "R3"], [("ps", sb)])
                for r in range(4):
                    pg = B4 * 4 + r
                    sl = sb * 4 + r
                    ia = idx[:, bi * 128 + pg:bi * 128 + pg + 1]
                    S.dma("pool", lambda e: e.indirect_dma_start(
                        out=kvr[sl], out_offset=None, in_=d_ckv, in_offset=bass.IndirectOffsetOnAxis(ap=ia, axis=0)),
                        ("kvr", sl), reads=["idx"], writes=[("kr", sl), ("vr", sl)])
                    for kvh in range(4):
                        S.op("pe", lambda e, kvh=kvh: e.matmul(
                            PS[sb][:, r * 64 + kvh * 16:r * 64 + kvh * 16 + 16], lhsT=kr[sl][:, kvh * 128:(kvh + 1) * 128],
                            rhs=qbd[:, kvh, :], start=False, stop=(r == 3 and kvh == 3)), [("kr", sl), "qbd"], [("ps", sb)])
                S.op("act", lambda e: e.activation(out=Ps[sb][:, 0:256], in_=PS[sb][:, 0:256], func=AF.Exp, bias=negM, scale=1.0),
                     [("ps", sb), "sc"], [("Ps", sb)])

            def sa_stage_b(B4):
                sb = B4 % 2
                for r in range(4):
                    sl = sb * 4 + r
                    for kvh in range(4):
                        S.op("pe", lambda e, kvh=kvh: e.matmul(
                            PS[2][:, kvh * 16:kvh * 16 + 16], lhsT=vr[sl][:, kvh * 128:(kvh + 1) * 128],
                            rhs=Ps[sb][:, r * 64 + kvh * 16:r * 64 + kvh * 16 + 16], start=False, stop=False),
                            [("vr", sl), ("Ps", sb)], [("ps", 2)])
                    S.op("pe", lambda e: e.matmul(PS[2][:, 64:128], lhsT=onesb, rhs=Ps[sb][:, r * 64:(r + 1) * 64],
                                                  start=False, stop=False), [("Ps", sb), "bfc"], [("ps", 2)])

            sa_stage_a(0)
            for B4 in range(32):
                if B4 + 1 < 32:
                    sa_stage_a(B4 + 1)
                sa_stage_b(B4)
            for kvh in range(4):
                S.op("pe", lambda e, kvh=kvh, bi=bi: e.matmul(PS[3][0:4, kvh * 16:kvh * 16 + 16], lhsT=kT[:, kvh, 2048 + bi * 4:2052 + bi * 4],
                                                              rhs=qbd[:, kvh, :], start=True, stop=True), ["kT", "qbd"], [("ps", 3)])
            S.op("dve", lambda e: e.tensor_tensor(out=sa_t[0:4, :], in0=PS[3][0:4, 0:64], in1=newb, op=ALU.add), [("ps", 3), "newb"], ["sa_t"])
            S.op("act", lambda e: e.activation(out=Pn, in_=sa_t[0:4, :], func=AF.Exp, bias=negM[0:4, :], scale=1.0), ["sa_t", "sc"], ["Pn"])
            for kvh in range(4):
                S.op("pe", lambda e, kvh=kvh, bi=bi: e.matmul(PS[2][:, kvh * 16:kvh * 16 + 16], lhsT=vS[0:4, bi, kvh * 128:(kvh + 1) * 128],
                                                              rhs=Pn[:, kvh * 16:kvh * 16 + 16], start=False, stop=False), ["vS", "Pn"], [("ps", 2)])
            S.op("pe", lambda e: e.matmul(PS[2][:, 64:128], lhsT=onesb[0:4, :], rhs=Pn, start=False, stop=True), ["Pn", "bfc"], [("ps", 2)])
            S.op("act", lambda e: e.activation(out=sa_r, in_=PS[2][:, 64:128], func=AF.Ln), [("ps", 2)], ["sa_r"])
            S.op("act", lambda e: e.activation(out=sa_r, in_=sa_r, func=AF.Exp, scale=-1.0), ["sa_r"], ["sa_r"])
            S.op("dve", lambda e: e.tensor_tensor(out=sa_r, in0=PS[2][:, 0:64], in1=sa_r, op=ALU.mult), [("ps", 2), "sa_r"], ["sa_r"])
            T4 = sa_r.rearrange("p (k m c) -> p k m c", k=4, m=2)
            S.op("dve", lambda e: e.scalar_tensor_tensor(out=sa_o.rearrange("p (k c) -> p k c", k=4), in0=T4[:, :, 1, :], scalar=neglam,
                                                         in1=T4[:, :, 0, :], op0=ALU.mult, op1=ALU.add), ["sa_r", "sc"], ["sa_o"])
            subln_finish(sa_o, 32, amT[:, 0:8, 1024 + bi * 4:1028 + bi * 4], "sa_o", sg8, a3=8)
        S.barrier()
        chk("p3c")
        R_M.reset()
        tmp_off.clear()
        R_X.reset()

        o_Sbf = R_X.alloc(17 * 128 * 2)
        Sbf = bfv(o_Sbf, 17 * 128).rearrange("p (c v) -> p c v", c=17)
        o_abf = R_X.alloc(256 * 2)
        abf = bfv(o_abf, 256).rearrange("p (j t) -> p j t", j=4)
        o_as = R_X.alloc(16 * 2)
        a_s = bfv(o_as, 16, 4)[:, 0:4]
        Ss = f32v(R_M.alloc(128 * 4), 128)
        Ssb = bfv(R_M.alloc(128 * 2), 128)
        Sso = [f32v(R_M.alloc(128 * 4), 128) for _ in range(2)]
        Spo = f32v(R_M.alloc(128 * 4), 128)
        m_mark = R_M.cur
        xm = R_X.cur
        ssc = [0]
        for hgp in range(2):
            R_X.cur = xm
            hb = hgrn_bufs(R_X, NT, 9, True)
            w, wk = load_w16(d_win[g_i]); g_i += 1
            wv = w.rearrange("p (b k c) -> p b k c", b=4, k=16)
            for hl in range(4):
                for (t0, n) in TBS:
                    proj_fm(wv, wk, hl, hT, t0, n, lambda pi, hl=hl, t0=t0, n=n, hb=hb: hf_post(
                        pi, hb, hl, hgp * 4 + hl, t0, n, True, n == 16, "o"))
            w, wk = load_w16(d_win[g_i]); g_i += 1
            wv = w.rearrange("p (b k c) -> p b k c", b=4, k=16)
            for hl in range(4):
                for (t0, n) in TBS:
                    def hqpost(pi, hl=hl, t0=t0, n=n, hb=hb):
                        sl_ = tmpf("hf_sg")[:, 0:n]
                        S.op("act", lambda e: e.activation(out=sl_, in_=PS[pi][:, 0:n], func=AF.Silu), [("ps", pi)], ["hf_sg"])
                        S.op("dve", lambda e: e.tensor_tensor(out=hb["qt"][:, hl, t0:t0 + n], in0=sl_, in1=hb["eg"][:, hl, t0:t0 + n], op=ALU.mult),
                             ["hf_sg", "oeg"], ["oqt"])
                    proj_fm(wv, wk, hl, hT, t0, n, hqpost)
            w, wk = load_w16(d_win[g_i]); g_i += 1
            wv = w.rearrange("p (k c) -> p k c", k=16)
            for tl_ in range(8):
                proj_tm(wv, wk, hT, tl_ * 128, 128, lambda pi, tl_=tl_, hb=hb: S.op(
                    "act", lambda e: e.activation(out=hb["hi"][:, tl_, :], in_=PS[pi][:, :], func=AF.Copy), [("ps", pi)], ["ohi"]))
            for bi in range(4):
                proj_tm(wv, wk, hT, 1024 + bi * 4, 4, lambda pi, bi=bi, hb=hb: S.op(
                    "act", lambda e: e.activation(out=hb["his"][0:4, bi, :], in_=PS[pi][0:4, :], func=AF.Copy), [("ps", pi)], ["ohis"]))
            w, wk = load_w16(d_win[g_i]); g_i += 1
            wv = w.rearrange("p (b k c) -> p b k c", b=4, k=16)
            for hl in range(4):
                for (t0, n) in TBS:
                    proj_fm(wv, wk, hl, hT, t0, n, lambda pi, hl=hl, t0=t0, n=n, hb=hb: S.op(
                        "act", lambda e: e.activation(out=hb["gate"][:, hl, t0:t0 + n], in_=PS[pi][:, 0:n], func=AF.Silu), [("ps", pi)], ["ogate"]))
            for hl in range(4):
                hglob = hgp * 4 + hl
                state_chain(hb, hl, hglob, 0, 16, 0, Sbf, "o")
                S.op("dve", lambda e, hglob=hglob: e.tensor_copy(out=Spo, in_=Sst[:, hglob, :]), [("S", hglob)], ["Spo"])
                S.dma("sp", lambda e, hglob=hglob: e.dma_start(out=out_s[:, hglob * 128:(hglob + 1) * 128], in_=Spo), ("out", "s"), reads=["Spo"])
                for qb in range(2):
                    pa = next_ps()
                    for c in range(8):
                        j, e2 = c // 2, c % 2
                        tk = qb * 512 + c * 64
                        S.op("pe", lambda e, j=j, e2=e2, tk=tk, pa=pa: e.matmul(
                            PS[pa][64 * e2:64 * e2 + 64, j * 64:(j + 1) * 64], lhsT=hb["kt"][:, hl, tk:tk + 64],
                            rhs=hb["qt"][:, hl, tk:tk + 64], start=True, stop=True), ["okt", "oqt"], [("ps", pa)])
                    S.op("dve", lambda e, pa=pa: e.tensor_tensor(out=abf, in0=PS[pa][:, 0:256].rearrange("p (j t) -> p j t", j=4),
                                                                 in1=tri.unsqueeze(1).to_broadcast([128, 4, 64]), op=ALU.mult),
                         [("ps", pa), "cst"], ["abf"])
                    po = next_ps()
                    for c in range(8):
                        j, e2 = c // 2, c % 2
                        tk = qb * 512 + c * 64
                        tl_ = qb * 4 + j
                        S.op("pe", lambda e, c=c, tk=tk, po=po: e.matmul(
                            PS[po][:, c * 64:(c + 1) * 64], lhsT=Sbf[:, qb * 8 + c, :], rhs=hb["qt"][:, hl, tk:tk + 64], start=True, stop=False),
                            ["oSbf", "oqt"], [("ps", po)])
                        S.op("pe", lambda e, c=c, j=j, e2=e2, tl_=tl_, po=po: e.matmul(
                            PS[po][:, c * 64:(c + 1) * 64], lhsT=hb["hi"][64 * e2:64 * e2 + 64, tl_, hl * 128:(hl + 1) * 128],
                            rhs=abf[64 * e2:64 * e2 + 64, j, :], start=False, stop=True), ["ohi", "abf"], [("ps", po)])
                    subln_finish(PS[po][:, :], 512, amT[:, 8 + hglob, qb * 512:(qb + 1) * 512], ("ps", po), hgg,
                                 extra=(hb["gate"][:, hl, qb * 512:(qb + 1) * 512], "ogate"))
                pos_ = next_ps()
                for bi in range(4):
                    tk = 1024 + bi * 4
                    b = ssc[0] % 2
                    ssc[0] += 1
                    S.dma("sp", lambda e, bi=bi, hglob=hglob: e.dma_start(out=Ss, in_=d_st0[bi, hglob]), ("ld", "ss"), writes=["Ss"])
                    S.op("act", lambda e: e.activation(out=Ssb, in_=Ss, func=AF.Copy), ["Ss"], ["Ssb"])
                    pa = next_ps()
                    S.op("pe", lambda e, tk=tk, pa=pa: e.matmul(PS[pa][0:4, 0:4], lhsT=hb["kt"][:, hl, tk:tk + 4], rhs=hb["qt"][:, hl, tk:tk + 4],
                                                                start=True, stop=True), ["okt", "oqt"], [("ps", pa)])
                    S.op("dve", lambda e, pa=pa: e.tensor_tensor(out=a_s, in0=PS[pa][0:4, 0:4], in1=tri[0:4, 0:4], op=ALU.mult), [("ps", pa), "cst"], ["a_s"])
                    S.op("pe", lambda e, tk=tk, bi=bi: e.matmul(PS[pos_][:, bi * 4:bi * 4 + 4], lhsT=Ssb, rhs=hb["qt"][:, hl, tk:tk + 4],
                                                                start=True, stop=False), ["Ssb", "oqt"], [("ps", pos_)])
                    S.op("pe", lambda e, bi=bi: e.matmul(PS[pos_][:, bi * 4:bi * 4 + 4], lhsT=hb["his"][0:4, bi, hl * 128:(hl + 1) * 128], rhs=a_s,
                                                         start=False, stop=True), ["ohis", "a_s"], [("ps", pos_)])
                    S.op("pe", lambda e, bi=bi, pa=pa: e.matmul(PS[pa][:, 128:256], lhsT=hb["khs"][0:4, bi, hl * 128:(hl + 1) * 128],
                                                                rhs=hb["his"][0:4, bi, hl * 128:(hl + 1) * 128], start=True, stop=True),
                         ["okhs", "ohis"], [("ps", pa)])
                    S.op("dve", lambda e, bi=bi, pa=pa, b=b: e.scalar_tensor_tensor(
                        out=Sso[b], in0=Ss, scalar=hb["d"][:, hl, 16 + bi:17 + bi], in1=PS[pa][:, 128:256], op0=ALU.mult, op1=ALU.add),
                        [("ps", pa), "od", "Ss"], [("Sso", b)])
                    S.dma("sp", lambda e, bi=bi, hglob=hglob, b=b: e.dma_start(out=out_ss[bi, :, hglob * 128:(hglob + 1) * 128], in_=Sso[b]),
                          ("out", "ss%d" % b), reads=[("Sso", b)])
                subln_finish(PS[pos_][:, 0:16], 16, amT[:, 8 + hglob, 1024:1040], ("ps", pos_), hgg, extra=(hb["gate"][:, hl, 1024:1040], "ogate"))
            S.barrier()
        S.barrier()
        chk("p3d")
        R_M.reset()
        tmp_off.clear()
        R_X.reset()

        o_x1 = R_X.alloc(16 * NT * 4)
        DBG["x1"] = o_x1
        x1 = f32v(o_x1, 16 * NT).rearrange("p (k t) -> p k t", k=16)
        xrb = [f32v(R_M.alloc(512 * 4), 512) for _ in range(2)]
        xrc = [0]
        o4t = f32v(R_M.alloc(16 * 4), 16)
        for og in range(4):
            w, wk = load_w16(d_wout[og])
            wv = w.rearrange("p (b f c) -> p b f c", b=4, f=16)
            for cbl in range(4):
                cb = og * 4 + cbl
                for (t0, n) in TBS:
                    pi = next_ps()
                    for f in range(16):
                        S.op("pe", lambda e, f=f, pi=pi, cbl=cbl, t0=t0, n=n: e.matmul(PS[pi][:, 0:n], lhsT=wv[:, cbl, f, :], rhs=amT[:, f, t0:t0 + n],
                                                                                       start=(f == 0), stop=(f == 15)), [wk, "amT"], [("ps", pi)])
                    b = xrc[0] % 2
                    xrc[0] += 1
                    src = d_xo[:, cb, t0:t0 + n] if n == 512 else d_xs[:, cb, :]
                    S.dma("sp", lambda e, b=b, src=src, n=n: e.dma_start(out=xrb[b][:, 0:n], in_=src), ("ld", "xr%d" % b), writes=[("xr", b)])
                    if n == 512:
                        S.op("dve", lambda e, pi=pi, b=b, cb=cb, t0=t0: e.scalar_tensor_tensor(
                            out=x1[:, cb, t0:t0 + 512], in0=PS[pi][:, :], scalar=GA1[:, cb, 0:1], in1=xrb[b], op0=ALU.mult, op1=ALU.add),
                            [("ps", pi), ("xr", b), "modT"], [("x1", cb, t0)])
                    else:
                        S.op("dve", lambda e, pi=pi, cb=cb: e.tensor_tensor(
                            out=o4t.rearrange("p (b t) -> p b t", b=4), in0=PS[pi][:, 0:16].rearrange("p (b t) -> p b t", b=4),
                            in1=GA1[:, cb, 1:5].unsqueeze(2).to_broadcast([128, 4, 4]), op=ALU.mult), [("ps", pi), "modT"], ["o4t"])
                        S.op("dve", lambda e, b=b, cb=cb: e.tensor_tensor(out=x1[:, cb, 1024:1040], in0=o4t, in1=xrb[b][:, 0:16], op=ALU.add),
                             ["o4t", ("xr", b)], [("x1", cb, 1024)])
        S.barrier()
        chk("p4")
        R_M.reset()

        h2T = hT
        comb = f32v(R_M.alloc(144 * 4), 144).rearrange("p (t g e) -> p t g e", g=4, e=4)
        m5_mark = R_M.cur
        sqb = [f32v(R_M.alloc(512 * 4), 512) for _ in range(2)]
        rs2 = f32v(R_M.alloc(512 * 4), 512)
        tp2 = [f32v(R_M.alloc(512 * 4), 512) for _ in range(2)]
        h2f = [f32v(R_M.alloc(512 * 4), 512) for _ in range(2)]
        zf = f32v(R_M.alloc(180 * 4), 180)
        S.op("dve", lambda e: e.memset(zf, 0.0), [], ["zf"])
        S.op("pe", lambda e: e.matmul(PS[5][:, 0:180], lhsT=zf[:, 0:128], rhs=zf[:, 0:180], start=True, stop=False), ["zf"], [("ps", 5)])
        for bidx, (t0, n) in enumerate(TBS):
            rsel = [(0, n, 0)] if n == 512 else SAMPLE_RSEL
            for k in range(16):
                sb = k % 2
                S.op("act", lambda e, k=k, sb=sb, t0=t0, n=n: e.activation(out=sqb[sb][:, 0:n], in_=x1[:, k, t0:t0 + n], func=AF.Square), ["x1"], [("sq2", sb)])
                S.op("pe", lambda e, k=k, sb=sb, n=n: e.matmul(PS[7][:, 0:n], lhsT=ones, rhs=sqb[sb][:, 0:n], start=(k == 0), stop=(k == 15)),
                     [("sq2", sb), "cst"], [("ps", 7)])
            rstd_from_ps(PS[7][:, 0:n], n, 1.0 / 2048.0, rs2[:, 0:n], rs2[:, 0:n], [("ps", 7)], "rs2")
            for k in range(16):
                sb = k % 2
                for (c0, cn, r) in rsel:
                    S.op("dve", lambda e, k=k, sb=sb, c0=c0, cn=cn, r=r, t0=t0: e.scalar_tensor_tensor(
                        out=tp2[sb][:, c0:c0 + cn], in0=x1[:, k, t0 + c0:t0 + c0 + cn], scalar=G2[:, k, r:r + 1], in1=rs2[:, c0:c0 + cn],
                        op0=ALU.mult, op1=ALU.mult), ["x1", "G", "rs2"], [("tp2", sb)])
                    S.op("act", lambda e, k=k, sb=sb, c0=c0, cn=cn, r=r: e.activation(
                        out=h2f[sb][:, c0:c0 + cn], in_=tp2[sb][:, c0:c0 + cn], func=AF.Identity, bias=SH2[:, k, r:r + 1], scale=1.0),
                        [("tp2", sb), "modT"], [("h2f", sb)])
                S.op("dve", lambda e, k=k, sb=sb, t0=t0, n=n: e.tensor_copy(out=h2T[:, k, t0:t0 + n], in_=h2f[sb][:, 0:n]), [("h2f", sb)], ["hT"])
                ntile = max(1, n // 128)
                for j in range(ntile):
                    tl_ = t0 // 128 + j
                    mm_ = min(128, n)
                    S.op("pe", lambda e, k=k, sb=sb, j=j, tl_=tl_, mm_=mm_: e.matmul(
                        PS[5][0:mm_, tl_ * 20:tl_ * 20 + 20], lhsT=h2f[sb][:, j * 128:j * 128 + mm_], rhs=wr[:, k, :],
                        start=False, stop=(k == 15 and tl_ == 8)), [("h2f", sb), "wr"], [("ps", 5)])
        def rt(n):
            return f32v(R_M.alloc(n * 4), n)
        LG = rt(180).rearrange("p (t c) -> p t c", c=20)
        S.op("act", lambda e: e.activation(out=LG.rearrange("p t c -> p (t c)"), in_=PS[5][:, 0:180], func=AF.Copy), [("ps", 5)], ["LG"])
        lg = rt(36).rearrange("p (t g) -> p t g", g=4)
        mx = rt(9)
        oh = rt(36).rearrange("p (t g) -> p t g", g=4)
        ex = rt(36).rearrange("p (t g) -> p t g", g=4)
        den = rt(9)
        pgt = rt(9)
        le = rt(144).rearrange("p (t g e) -> p t g e", g=4, e=4)
        les = rt(36).rearrange("p (t e) -> p t e", e=4)
        m1 = rt(9)
        k1 = rt(36).rearrange("p (t e) -> p t e", e=4)
        le2 = rt(36).rearrange("p (t e) -> p t e", e=4)
        m2 = rt(9)
        k2 = rt(36).rearrange("p (t e) -> p t e", e=4)
        w1 = rt(9)
        w2 = rt(9)
        ce = rt(36).rearrange("p (t e) -> p t e", e=4)
        ce2 = rt(36).rearrange("p (t e) -> p t e", e=4)
        brg = prm[:, P_BRG:P_BRG + 4].unsqueeze(1).to_broadcast([128, 9, 4])
        bre = prm[:, P_BRE:P_BRE + 16].unsqueeze(1).to_broadcast([128, 9, 16])
        R = "rt"

        def dv(fn, rd=(), wrk=R):
            S.op("dve", fn, [R] + list(rd), [wrk])

        dv(lambda e: e.tensor_tensor(out=lg, in0=LG[:, :, 0:4], in1=brg, op=ALU.add), ["LG", "prm"])
        dv(lambda e: e.tensor_reduce(out=mx, in_=lg, axis=AX.X, op=ALU.max))
        dv(lambda e: e.tensor_tensor(out=oh, in0=lg, in1=mx.unsqueeze(2).to_broadcast([128, 9, 4]), op=ALU.is_equal))
        dv(lambda e: e.tensor_tensor(out=ex, in0=lg, in1=mx.unsqueeze(2).to_broadcast([128, 9, 4]), op=ALU.subtract))
        S.op("act", lambda e: e.activation(out=ex, in_=ex, func=AF.Exp), [R], [R])
        dv(lambda e: e.tensor_reduce(out=den, in_=ex, axis=AX.X, op=ALU.add))
        dv(lambda e: e.reciprocal(out=pgt, in_=den))
        dv(lambda e: e.tensor_tensor(out=le.rearrange("p t g e -> p t (g e)"), in0=LG[:, :, 4:20], in1=bre, op=ALU.add), ["LG", "prm"])
        dv(lambda e: e.tensor_tensor(out=le, in0=le, in1=oh.unsqueeze(3).to_broadcast([128, 9, 4, 4]), op=ALU.mult))
        dv(lambda e: e.tensor_reduce(out=les, in_=le.rearrange("p t g e -> p t e g"), axis=AX.X, op=ALU.add))
        dv(lambda e: e.tensor_reduce(out=m1, in_=les, axis=AX.X, op=ALU.max))
        dv(lambda e: e.tensor_tensor(out=k1, in0=les, in1=m1.unsqueeze(2).to_broadcast([128, 9, 4]), op=ALU.is_equal))
        dv(lambda e: e.scalar_tensor_tensor(out=le2, in0=k1, scalar=-1.0e30, in1=les, op0=ALU.mult, op1=ALU.add))
        dv(lambda e: e.tensor_reduce(out=m2, in_=le2, axis=AX.X, op=ALU.max))
        dv(lambda e: e.tensor_tensor(out=k2, in0=le2, in1=m2.unsqueeze(2).to_broadcast([128, 9, 4]), op=ALU.is_equal))
        dv(lambda e: e.tensor_tensor(out=w2, in0=m2, in1=m1, op=ALU.subtract))
        S.op("act", lambda e: e.activation(out=w2, in_=w2, func=AF.Exp), [R], [R])
        dv(lambda e: e.tensor_scalar(out=w1, in0=w2, scalar1=1.0, scalar2=None, op0=ALU.add))
        dv(lambda e: e.reciprocal(out=w1, in_=w1))
        dv(lambda e: e.tensor_tensor(out=w2, in0=w2, in1=w1, op=ALU.mult))
        dv(lambda e: e.tensor_tensor(out=w1, in0=w1, in1=pgt, op=ALU.mult))
        dv(lambda e: e.tensor_tensor(out=w2, in0=w2, in1=pgt, op=ALU.mult))
        dv(lambda e: e.tensor_tensor(out=ce, in0=k1, in1=w1.unsqueeze(2).to_broadcast([128, 9, 4]), op=ALU.mult))
        dv(lambda e: e.tensor_tensor(out=ce2, in0=k2, in1=w2.unsqueeze(2).to_broadcast([128, 9, 4]), op=ALU.mult))
        dv(lambda e: e.tensor_tensor(out=ce, in0=ce, in1=ce2, op=ALU.add))
        dv(lambda e: e.tensor_tensor(out=comb, in0=oh.unsqueeze(3).to_broadcast([128, 9, 4, 4]),
                                     in1=ce.unsqueeze(2).to_broadcast([128, 9, 4, 4]), op=ALU.mult), wrk="comb")
        S.barrier()
        chk("p5")

        R_M.cur = m5_mark
        R_RING.reset()
        mslot = [R_RING.alloc(4096) for _ in range(8)]
        msl = [0]

        def load_w4(src_ap):
            slot = msl[0] % 8
            msl[0] += 1
            v = bfv(mslot[slot], 2048)
            key = ("mring", slot)
            S.dma("pool", lambda e: e.dma_start(out=v, in_=src_ap), ("mring", slot), writes=[key])
            return v, key

        o_hid = R_AM.base
        hid = bfv(o_hid, 4 * NT).rearrange("p (f t) -> p f t", f=4)
        cbc = [f32v(R_AM.base + 4 * NT * 2 + i * NT * 4, NT) for i in range(2)]
        De = [f32v(R_M.alloc(128 * 4), 128) for _ in range(2)]
        msA = [f32v(R_M.alloc(512 * 4), 512) for _ in range(2)]
        mtT = [f32v(R_M.alloc(512 * 4), 512) for _ in range(2)]
        mo4 = f32v(R_M.alloc(16 * 4), 16)
        dec = [0]
        mc = [0]
        def build_cbc(ex_):
            g_, e_ = ex_ // 4, ex_ % 4
            cb_ = cbc[ex_ % 2]
            for bidx, (t0, n) in enumerate(TBS):
                pi = next_ps()
                ntile = max(1, n // 128)
                for j in range(ntile):
                    tl_ = t0 // 128 + j
                    mm_ = min(128, n)
                    b = dec[0] % 2
                    dec[0] += 1
                    S.op("dve", lambda e, b=b, tl_=tl_, mm_=mm_: e.tensor_scalar(
                        out=De[b][0:mm_, 0:mm_], in0=ident[0:mm_, 0:mm_], scalar1=comb[0:mm_, tl_, g_, e_:e_ + 1], scalar2=None, op0=ALU.mult),
                        ["comb", "cst"], [("De", b)])
                    S.op("pe", lambda e, b=b, j=j, mm_=mm_, pi=pi: e.matmul(PS[pi][:, j * 128:j * 128 + mm_], lhsT=ones[0:mm_, :], rhs=De[b][0:mm_, 0:mm_],
                                                                            start=True, stop=True), [("De", b), "cst"], [("ps", pi)])
                S.op("act", lambda e, pi=pi, t0=t0, n=n: e.activation(out=cb_[:, t0:t0 + n], in_=PS[pi][:, 0:n], func=AF.Copy), [("ps", pi)], [("cbc", ex_ % 2)])

        build_cbc(0)
        for ex_ in range(16):
            g_, e_ = ex_ // 4, ex_ % 4
            cb_ = cbc[ex_ % 2]
            for fb in range(4):
                wg, wgk = load_w4(d_wgu[ex_, fb * 2])
                wu, wuk = load_w4(d_wgu[ex_, fb * 2 + 1])
                wgv = wg.rearrange("p (k c) -> p k c", k=16)
                wuv = wu.rearrange("p (k c) -> p k c", k=16)
                for (t0, n) in TBS:
                    pa, pu = next_ps(), next_ps()
                    for k in range(16):
                        S.op("pe", lambda e, k=k, pa=pa, t0=t0, n=n: e.matmul(PS[pa][:, 0:n], lhsT=wgv[:, k, :], rhs=h2T[:, k, t0:t0 + n],
                                                                              start=(k == 0), stop=(k == 15)), [wgk, "hT"], [("ps", pa)])
                    for k in range(16):
                        S.op("pe", lambda e, k=k, pu=pu, t0=t0, n=n: e.matmul(PS[pu][:, 0:n], lhsT=wuv[:, k, :], rhs=h2T[:, k, t0:t0 + n],
                                                                              start=(k == 0), stop=(k == 15)), [wuk, "hT"], [("ps", pu)])
                    b = mc[0] % 2
                    mc[0] += 1
                    S.op("act", lambda e, pa=pa, b=b, n=n: e.activation(out=msA[b][:, 0:n], in_=PS[pa][:, 0:n], func=AF.Silu), [("ps", pa)], [("msA", b)])
                    S.op("dve", lambda e, pu=pu, b=b, t0=t0, n=n: e.tensor_tensor(out=mtT[b][:, 0:n], in0=PS[pu][:, 0:n], in1=cb_[:, t0:t0 + n], op=ALU.mult),
                         [("ps", pu), ("cbc", ex_ % 2)], [("mtT", b)])
                    S.op("dve", lambda e, b=b, fb=fb, t0=t0, n=n: e.tensor_tensor(out=hid[:, fb, t0:t0 + n], in0=msA[b][:, 0:n], in1=mtT[b][:, 0:n], op=ALU.mult),
                         [("msA", b), ("mtT", b)], ["hid"])
            if ex_ + 1 < 16:
                build_cbc(ex_ + 1)
            for cg in range(4):
                wd, wdk = load_w4(d_wdn[ex_, cg])
                wdv = wd.rearrange("p (c f o) -> p c f o", c=4, f=4)
                for cbl in range(4):
                    cb = cg * 4 + cbl
                    for (t0, n) in TBS:
                        pi = next_ps()
                        for fb in range(4):
                            S.op("pe", lambda e, fb=fb, pi=pi, cbl=cbl, t0=t0, n=n: e.matmul(PS[pi][:, 0:n], lhsT=wdv[:, cbl, fb, :], rhs=hid[:, fb, t0:t0 + n],
                                                                                             start=(fb == 0), stop=(fb == 3)), [wdk, "hid"], [("ps", pi)])
                        if n == 512:
                            S.op("dve", lambda e, pi=pi, cb=cb, t0=t0: e.scalar_tensor_tensor(
                                out=x1[:, cb, t0:t0 + 512], in0=PS[pi][:, :], scalar=GA2[:, cb, 0:1], in1=x1[:, cb, t0:t0 + 512], op0=ALU.mult, op1=ALU.add),
                                [("ps", pi), "modT", ("x1", cb, t0)], [("x1", cb, t0)])
                        else:
                            S.op("dve", lambda e, pi=pi, cb=cb: e.tensor_tensor(
                                out=mo4.rearrange("p (b t) -> p b t", b=4), in0=PS[pi][:, 0:16].rearrange("p (b t) -> p b t", b=4),
                                in1=GA2[:, cb, 1:5].unsqueeze(2).to_broadcast([128, 4, 4]), op=ALU.mult), [("ps", pi), "modT"], ["mo4"])
                            S.op("dve", lambda e, cb=cb: e.tensor_tensor(out=x1[:, cb, 1024:1040], in0=mo4, in1=x1[:, cb, 1024:1040], op=ALU.add),
                                 ["mo4", ("x1", cb, 1024)], [("x1", cb, 1024)])
        S.barrier()
        S.dma("sp", lambda e: e.dma_start(out=out_yT, in_=x1.rearrange("p k t -> p (k t)")), ("out", "y"), reads=["x1"])

    except _Stop:
        pass
    if stop is not None:
        S.barrier()
        d_dbg = dout("dbg", [128, ARENA // 4])
        S.dma("sp", lambda e: e.dma_start(out=d_dbg, in_=A[:, :]), ("out", "dbg"))
    S.emit(nc, es)
    es.close()
    return nc


def _fm(a):
    T = a.shape[0]
    return np.ascontiguousarray(a.reshape(T, 16, 128).transpose(2, 1, 0))


def _wblocks_fm(w, cols):
    out = np.empty((128, 4, 16, 128), np.float32)
    for b, c0 in enumerate(cols):
        out[:, b] = w[:, c0:c0 + 128].reshape(16, 128, 128).transpose(1, 0, 2)
    return out.reshape(128, 8192)


def _wgroup_tm(w, c0):
    return np.ascontiguousarray(w[:, c0:c0 + 512].reshape(16, 128, 512).transpose(1, 0, 2)).reshape(128, 8192)


_NC_CACHE = {}


def prep(x_prompt, x_sample, cache_k, cache_v, state_hgrn, page_table, c_prompt, c_sample,
           norm1_g, norm2_g, w_ada, b_ada, w_in, q_norm_g, k_norm_g,
           lambda_q1, lambda_k1, lambda_q2, lambda_k2, subln_g, hg_lower_bound, hg_norm_g, w_out,
           w_router_group, b_router_group, w_router_expert, b_router_expert,
           w_exp_gate, w_exp_up, w_exp_down, small_cache=False):
    f = np.float32
    x_prompt = np.asarray(x_prompt, f); x_sample = np.asarray(x_sample, f)
    cache_k = np.asarray(cache_k, f); cache_v = np.asarray(cache_v, f)
    w_ada = np.asarray(w_ada, f)[0]; w_in = np.asarray(w_in, f)[0]; w_out = np.asarray(w_out, f)[0]
    wg_ = np.asarray(w_exp_gate, f)[0]; wu_ = np.asarray(w_exp_up, f)[0]; wd_ = np.asarray(w_exp_down, f)[0]

    wada = np.stack([_wblocks_fm(w_ada, [(g * 4 + b) * 128 for b in range(4)]) for g in range(24)])
    QC, KC, VC, HQ, HF, HI, HG = 0, 1024, 1536, 2048, 3072, 4096, 5120
    groups = []
    groups.append(_wblocks_fm(w_in, [KC + 128 * b for b in range(4)]))
    groups.append(_wgroup_tm(w_in, VC))
    for hgp in range(2):
        groups.append(_wblocks_fm(w_in, [HF + hgp * 512 + 128 * b for b in range(4)]))
        groups.append(_wgroup_tm(w_in, HI + hgp * 512))
    for qg in range(2):
        groups.append(_wblocks_fm(w_in, [QC + qg * 512 + 128 * b for b in range(4)]))
    groups.append(_wblocks_fm(w_in, [KC + 128 * b for b in range(4)]))
    groups.append(_wgroup_tm(w_in, VC))
    for hgp in range(2):
        groups.append(_wblocks_fm(w_in, [HF + hgp * 512 + 128 * b for b in range(4)]))
        groups.append(_wblocks_fm(w_in, [HQ + hgp * 512 + 128 * b for b in range(4)]))
        groups.append(_wgroup_tm(w_in, HI + hgp * 512))
        groups.append(_wblocks_fm(w_in, [HG + hgp * 512 + 128 * b for b in range(4)]))
    win = np.stack(groups)
    wout = np.stack([_wblocks_fm(w_out, [(g * 4 + b) * 128 for b in range(4)]) for g in range(4)])
    wgu = np.empty((16, 8, 128, 2048), f)
    for e in range(16):
        for fb in range(4):
            wgu[e, fb * 2] = wg_[e][:, fb * 128:(fb + 1) * 128].reshape(16, 128, 128).transpose(1, 0, 2).reshape(128, 2048)
            wgu[e, fb * 2 + 1] = wu_[e][:, fb * 128:(fb + 1) * 128].reshape(16, 128, 128).transpose(1, 0, 2).reshape(128, 2048)
    wdn = np.ascontiguousarray(wd_.reshape(16, 4, 128, 4, 4, 128).transpose(0, 3, 2, 4, 1, 5)).reshape(16, 4, 128, 2048)
    if small_cache:
        ckv = np.zeros((128, 1024), f)
    else:
        ckv = np.empty((5120 * 128, 1024), f)
        ckv[:, 0:512] = cache_k[0].transpose(0, 3, 2, 1).reshape(5120 * 128, 512)
        ckv[:, 512:1024] = cache_v[0].reshape(5120 * 128, 512)
    posT, R3, newb = make_sample_tables()
    wr = np.concatenate([np.asarray(w_router_group, f)[0], np.asarray(w_router_expert, f)[0].transpose(1, 0, 2).reshape(2048, 16)], axis=1)
    wr = np.ascontiguousarray(wr.reshape(16, 128, 20).transpose(1, 0, 2)).reshape(128, 320)

    prm0 = np.zeros((128, NPRM), f)
    prm0[:, P_N1:P_N1 + 16] = np.asarray(norm1_g, f)[0].reshape(16, 128).T
    prm0[:, P_N2:P_N2 + 16] = np.asarray(norm2_g, f)[0].reshape(16, 128).T
    prm0[:, P_BT:P_BT + 96] = np.asarray(b_ada, f)[0].reshape(96, 128).T
    prm0[:, P_GQ] = np.tile(np.asarray(q_norm_g, f)[0], 2)
    prm0[:, P_GK] = np.tile(np.asarray(k_norm_g, f)[0], 2)
    prm0[:, P_SG] = np.asarray(subln_g, f)[0]
    prm0[:, P_HGG] = np.asarray(hg_norm_g, f)[0]
    lbv = np.asarray(hg_lower_bound, f)
    prm0[:, P_LB:P_LB + 8] = lbv[0].reshape(8, 128).T
    prm0[:, P_LB + 8:P_LB + 16] = lbv[1].reshape(8, 128).T
    prm0[:, P_GQR:P_GQR + 64] = np.asarray(q_norm_g, f)[0][None]
    prm0[:, P_GKR:P_GKR + 64] = np.asarray(k_norm_g, f)[0][None]
    prm0[:, P_LAMR:P_LAMR + 256] = np.concatenate([np.asarray(a, f)[0] for a in (lambda_q1, lambda_k1, lambda_q2, lambda_k2)])[None]
    prm0[:, P_BRG:P_BRG + 4] = np.asarray(b_router_group, f)[0][None]
    prm0[:, P_BRE:P_BRE + 16] = np.asarray(b_router_expert, f)[0].reshape(16)[None]

    in_maps = []
    pt = np.asarray(page_table, np.int32)
    for c in range(8):
        b, half = c // 2, c % 2
        prm = prm0.copy()
        prm[:, P_FLAG] = float(half)
        crow = np.concatenate([np.asarray(c_prompt, f)[b:b + 1], np.asarray(c_sample, f)[4 * c:4 * c + 4]], axis=0)
        cT = np.ascontiguousarray(crow.reshape(5, 16, 128).transpose(2, 1, 0)).reshape(128, 80)
        in_maps.append({
            "cst": make_consts(half), "prm": prm, "cT": cT, "wr": wr,
            "xo": _fm(x_prompt[b, half * 1024:(half + 1) * 1024]),
            "xp": _fm(x_prompt[b, 0:1024]),
            "xs": _fm(x_sample[4 * c:4 * c + 4].reshape(16, 2048)),
            "wada": wada, "win": win, "wout": wout, "wgu": wgu, "wdn": wdn,
            "ckv": ckv, "pt": np.ascontiguousarray(pt[4 * c:4 * c + 4]),
            "st0": np.ascontiguousarray(np.asarray(state_hgrn, f)[0, 4 * c:4 * c + 4]),
            "posT": posT, "R3": R3, "newb": newb,
        })
    return in_maps


def kernel(**inputs):
    f = np.float32
    in_maps = prep(**inputs)
    if "nc" not in _NC_CACHE:
        _NC_CACHE["nc"] = build()
    res = run_bass_kernel_spmd(_NC_CACHE["nc"], in_maps, core_ids=list(range(8)))
    R = res.results

    y_prompt = np.empty((4, 2048, 2048), f); y_sample = np.empty((32, 4, 2048), f)
    nk_p = np.empty((1, 4, 2048, 4, 128), f); nv_p = np.empty((1, 4, 2048, 4, 128), f)
    nk_s = np.empty((1, 32, 4, 4, 128), f); nv_s = np.empty((1, 32, 4, 4, 128), f)
    ns_p = np.empty((1, 4, 8, 128, 128), f); ns_s = np.empty((1, 32, 8, 128, 128), f)
    for c in range(8):
        b, half = c // 2, c % 2
        yT = R[c]["yT"].reshape(128, 16, NT)
        yt = yT.transpose(2, 1, 0).reshape(NT, 2048)
        y_prompt[b, half * 1024:(half + 1) * 1024] = yt[:1024]
        y_sample[4 * c:4 * c + 4] = yt[1024:].reshape(4, 4, 2048)
        kTo = R[c]["kTo"].reshape(128, 4, NT).transpose(2, 1, 0)
        nk_p[0, b, half * 1024:(half + 1) * 1024] = kTo[:1024]
        nk_s[0, 4 * c:4 * c + 4] = kTo[1024:].reshape(4, 4, 4, 128)
        vo = R[c]["vo"].reshape(NT, 4, 128)
        nv_p[0, b, half * 1024:(half + 1) * 1024] = vo[:1024]
        nv_s[0, 4 * c:4 * c + 4] = vo[1024:].reshape(4, 4, 4, 128)
        if half == 1:
            ns_p[0, b] = R[c]["so"].reshape(128, 8, 128).transpose(1, 0, 2)
        ns_s[0, 4 * c:4 * c + 4] = R[c]["sso"].reshape(4, 128, 8, 128).transpose(0, 2, 1, 3)
    return (y_prompt, y_sample, nk_p, nv_p, nk_s, nv_s, ns_p, ns_s)
```

```python
import math
from contextlib import ExitStack
import numpy as np
import concourse.bass as bass
import concourse.mybir as mybir
from concourse.bass_utils import run_bass_kernel_spmd

F32 = mybir.dt.float32
BF16 = mybir.dt.bfloat16
I32 = mybir.dt.int32
AF = mybir.ActivationFunctionType
ALU = mybir.AluOpType
AX = mybir.AxisListType

NT = 1040
EPS = 1e-6
LAM_INIT = 0.2
SLOPES = [2.0 ** (-(h + 1)) for h in range(8)]
PAST = 16384
ENG = ["pe", "act", "dve", "pool", "sp"]
STOP_AFTER = None
DBG = {}


class _Rec:
    def __init__(self):
        self.call = None

    def __getattr__(self, name):
        def f(*a, **kw):
            self.call = (name, a, kw)
            return None
        return f


def _bind(fn):
    if fn is None:
        return None
    r = _Rec()
    fn(r)
    name, a, kw = r.call
    return lambda e: getattr(e, name)(*a, **kw)


class Sched:
    def __init__(self):
        self.ops = {e: [] for e in ENG}
        self.state = {}
        self.wE = {e: {} for e in ENG}
        self.wD = {e: {} for e in ENG}
        self.dcount = {}
        self.sig = {e: set() for e in ENG}
        self.last_real = {}

    def _deps(self, reads, writes):
        deps = []
        for k in reads:
            st = self.state.get(k)
            if st and st[0] is not None:
                deps.append((st[0], True))
            if st and isinstance(k, tuple) and k[0] == "ps":
                deps.extend((t, False) for t in st[1].values())
        for k in writes:
            st = self.state.get(k)
            if st:
                if st[0] is not None:
                    deps.append((st[0], False))
                deps.extend((t, False) for t in st[1].values())
        return deps

    def _update(self, reads, writes, tok):
        key = (tok[0], tok[1])
        for k in reads:
            st = self.state.setdefault(k, [None, {}])
            st[1][key] = tok
        for k in writes:
            self.state[k] = [tok, {}]

    def _waits(self, eng, deps):
        waits = []
        for (t, raw) in deps:
            if t[0] == "E":
                _, f, i = t
                if f == eng and (eng == "pe" or not raw):
                    continue
                if self.wE[eng].get(f, -1) >= i:
                    continue
                self.wE[eng][f] = i
                self.sig[f].add(i)
                waits.append(t)
            else:
                _, k, v = t
                if self.wD[eng].get(k, 0) >= v:
                    continue
                self.wD[eng][k] = v
                waits.append(t)
        return waits

    def op(self, eng, fn, reads=(), writes=()):
        waits = self._waits(eng, self._deps(reads, writes))
        idx = len(self.ops[eng])
        self.ops[eng].append((waits, _bind(fn), None))
        self.last_real[eng] = idx
        tok = ("E", eng, idx)
        self._update(reads, writes, tok)
        return tok

    def dma(self, q, fn, semkey, reads=(), writes=()):
        waits = self._waits(q, self._deps(reads, writes))
        v = self.dcount.get(semkey, 0) + 16
        self.dcount[semkey] = v
        self.ops[q].append((waits, _bind(fn), (semkey, v)))
        tok = ("D", semkey, v)
        self._update(reads, writes, tok)
        return tok

    def barrier(self):
        toks = [("E", e, self.last_real[e]) for e in ENG if e in self.last_real]
        for e in ENG:
            waits = []
            for t in toks:
                if t[1] != e and self.wE[e].get(t[1], -1) < t[2]:
                    self.wE[e][t[1]] = t[2]
                    self.sig[t[1]].add(t[2])
                    waits.append(t)
            for k, v in self.dcount.items():
                if k[0] == "out" and self.wD[e].get(k, 0) < v:
                    self.wD[e][k] = v
                    waits.append(("D", k, v))
            if waits:
                self.ops[e].append((waits, None, None))

    def emit(self, nc, es):
        esem = {e: es.enter_context(nc.semaphore("sem_" + e)) for e in ENG}
        dsem = {k: es.enter_context(nc.semaphore("dsem%d" % i)) for i, k in enumerate(self.dcount)}
        rank = {}
        for e in ENG:
            r = 0
            rank[e] = {}
            for i in range(len(self.ops[e])):
                if i in self.sig[e]:
                    r += 1
                    rank[e][i] = r
        block = es.enter_context(nc.Block())

        def run(e, eng):
            for i, (waits, fn, dm) in enumerate(self.ops[e]):
                for t in waits:
                    if t[0] == "E":
                        eng.wait_ge(esem[t[1]], rank[t[1]][t[2]])
                    else:
                        eng.wait_ge(dsem[t[1]], t[2])
                if fn is None:
                    continue
                ins = fn(eng)
                if dm is not None:
                    ins.then_inc(dsem[dm[0]], 16)
                elif i in self.sig[e]:
                    ins.then_inc(esem[e], 1)
            if e in ("sp", "pool", "act"):
                for k, v in self.dcount.items():
                    if k[0] == "out":
                        eng.wait_ge(dsem[k], v)

        @block.tensor
        def _(eng):
            run("pe", eng)

        @block.scalar
        def _(eng):
            run("act", eng)

        @block.vector
        def _(eng):
            run("dve", eng)

        @block.gpsimd
        def _(eng):
            run("pool", eng)

        @block.sync
        def _(eng):
            run("sp", eng)


C_ID, C_ONES, C_BONES, C_TRI, C_SCAN, C_SCANS, C_ND, C_BIASC = 0, 128, 256, 384, 448, 960, 976, 2000
NCST = 2000 + 256
ND_D0 = [0, -128, -256, -384]


def _tile_list(qb):
    return list(range(12)) if qb == 0 else list(range(16))


def make_consts(half):
    c = np.zeros((128, NCST), np.float32)
    p = np.arange(128)
    c[:, C_ID:C_ID + 128] = np.eye(128)
    c[:, C_ONES:C_ONES + 128] = 1.0
    c[:, C_BONES:C_BONES + 128] = (p[:, None] // 64 == p[None, :] // 64)
    c[:, C_TRI:C_TRI + 64] = ((p[:, None] % 64) <= np.arange(64)[None, :])
    sm = np.ones(512, np.float32)
    sm[::64] = 0.0
    c[:, C_SCAN:C_SCAN + 512] = sm[None]
    sms = np.ones(16, np.float32)
    sms[::4] = 0.0
    c[:, C_SCANS:C_SCANS + 16] = sms[None]
    u = np.arange(1024)
    dist = u[None, :] - 384 - p[:, None]
    c[:, C_ND:C_ND + 1024] = np.where(dist >= 0, -dist, -1.0e6)
    for h in range(8):
        for qb in range(2):
            for kt in range(16):
                d0 = 1024 + 512 * qb - 128 * kt
                val = 0.0 if d0 < 128 else -SLOPES[h] * (d0 - 128)
                if kt < 8 and half == 0:
                    val += -30000.0
                c[:, C_BIASC + (h * 2 + qb) * 16 + kt] = val
    return c


def make_sample_tables():
    posT = np.zeros((3, 16, 128), np.float32)
    posT[0] = np.arange(128)[None, :]
    posT[1] = 1.0
    posT[2] = np.arange(16)[:, None]
    R3 = np.zeros((3, 8, 64), np.float32)
    newb = np.zeros((4, 64), np.float32)
    for kvh in range(4):
        for m in range(2):
            for g in range(2):
                for t in range(4):
                    col = kvh * 16 + m * 8 + g * 4 + t
                    sl = SLOPES[kvh * 2 + g]
                    R3[0, :, col] = sl
                    R3[1, :, col] = -sl * (PAST + t - 128.0 * np.arange(8))
                    R3[2, :, col] = sl * 1024.0
                    for tp in range(4):
                        newb[tp, col] = -sl * (t - tp) if tp <= t else -1.0e6
    return posT.reshape(3, 2048), R3.reshape(3, 512), newb


P_N1, P_N2, P_BT, P_GQ, P_GK, P_SG, P_HGG, P_LB, P_FLAG = 0, 16, 32, 128, 129, 130, 131, 132, 148
P_GQR, P_GKR, P_LAMR, P_BRG, P_BRE = 149, 213, 277, 533, 537
NPRM = 553


def build(stop=None, small_cache=False):
    nc = bass.Bass("TRN2", target_bir_lowering=False)
    S = Sched()
    es = ExitStack()

    def din(name, shape, dt=F32):
        return nc.dram_tensor(name, list(shape), dt, kind="ExternalInput").ap()

    def dout(name, shape, dt=F32):
        return nc.dram_tensor(name, list(shape), dt, kind="ExternalOutput").ap()

    d_cst = din("cst", [128, NCST])
    d_prm = din("prm", [128, NPRM])
    d_cT = din("cT", [128, 80])
    d_wr = din("wr", [128, 320])
    d_xo = din("xo", [128, 16, 1024])
    d_xp = din("xp", [128, 16, 1024])
    d_xs = din("xs", [128, 16, 16])
    d_wada = din("wada", [24, 128, 8192])
    d_win = din("win", [18, 128, 8192])
    d_wout = din("wout", [4, 128, 8192])
    d_wgu = din("wgu", [16, 8, 128, 2048])
    d_wdn = din("wdn", [16, 4, 128, 2048])
    d_ckv = din("ckv", [128 if small_cache else 655360, 1024])
    d_pt = din("pt", [4, 128], I32)
    d_st0 = din("st0", [4, 8, 128, 128])
    d_posT = din("posT", [3, 2048])
    d_R3 = din("R3", [3, 512])
    d_newb = din("newb", [4, 64])
    out_yT = dout("yT", [128, 16 * NT])
    out_kT = dout("kTo", [128, 4 * NT])
    out_v = dout("vo", [NT, 512])
    out_s = dout("so", [128, 1024])
    out_ss = dout("sso", [4, 128, 1024])

    ARENA = 207 * 1024
    A = es.enter_context(nc.sbuf_tensor("arena", [128, ARENA // 4], F32))
    PS = [es.enter_context(nc.psum_tensor("ps%d" % i, [128, 512], F32)) for i in range(8)]

    def f32v(off, n, parts=128):
        assert off % 4 == 0
        return A[0:parts, off // 4: off // 4 + n]

    def bfv(off, n, parts=128):
        assert off % 4 == 0 and n % 2 == 0
        return A[0:parts, off // 4: off // 4 + n // 2].bitcast(BF16)

    def i32v(off, n, parts=128):
        return A[0:parts, off // 4: off // 4 + n].bitcast(I32)

    class Region:
        def __init__(self, base, size):
            self.base, self.size, self.cur = base, size, base

        def alloc(self, nbytes):
            nbytes = (nbytes + 63) // 64 * 64
            off = self.cur
            self.cur += nbytes
            assert self.cur <= self.base + self.size, ("region overflow", self.cur - self.base, self.size)
            return off

        def reset(self):
            self.cur = self.base

    R_CONST = Region(0, 22 * 1024)
    R_RING = Region(R_CONST.base + R_CONST.size, 32 * 1024)
    R_H = Region(R_RING.base + R_RING.size, 33280)
    R_AM = Region(R_H.base + R_H.size, 33280)
    R_X = Region(R_AM.base + R_AM.size, 66560)
    R_M = Region(R_X.base + R_X.size, ARENA - (R_X.base + R_X.size))

    class _Stop(Exception):
        pass

    def chk(name):
        if stop == name:
            raise _Stop()

    try:
        o_cst = R_CONST.alloc(NCST * 4)
        cst = f32v(o_cst, NCST)
        ident = cst[:, C_ID:C_ID + 128]
        ones = cst[:, C_ONES:C_ONES + 128]
        bones = cst[:, C_BONES:C_BONES + 128]
        tri = cst[:, C_TRI:C_TRI + 64]
        scanm = cst[:, C_SCAN:C_SCAN + 512]
        scanms = cst[:, C_SCANS:C_SCANS + 16]
        biasc = cst[:, C_BIASC:C_BIASC + 256]
        o_prm = R_CONST.alloc(NPRM * 4)
        prm = f32v(o_prm, NPRM)
        o_bfc = R_CONST.alloc(768 * 2)
        identb = bfv(o_bfc, 768)[:, 0:128]
        onesb = bfv(o_bfc, 768)[:, 128:256]
        zerosb = bfv(o_bfc, 768)[:, 256:768]
        o_mod = R_CONST.alloc(480 * 4)
        DBG["mod"] = o_mod
        modT = f32v(o_mod, 480).rearrange("p (c r) -> p c r", r=5)
        o_g = R_CONST.alloc(160 * 4)
        DBG["g"] = o_g
        G1 = f32v(o_g, 160)[:, 0:80].rearrange("p (c r) -> p c r", r=5)
        G2 = f32v(o_g, 160)[:, 80:160].rearrange("p (c r) -> p c r", r=5)
        o_sc = R_CONST.alloc(64 * 4)
        DBG["sc"] = o_sc
        sc = f32v(o_sc, 64)
        negM, neglam, gq8, sg8, oml, noml, mq, mk = (sc[:, i:i + 1] for i in range(8))
        omlh = sc[:, 8:16]
        nomlh = sc[:, 16:24]
        lbh = sc[:, 24:32]
        epsc = sc[:, 32:33]
        o_wr = R_CONST.alloc(320 * 4)
        wr = f32v(o_wr, 320).rearrange("p (k c) -> p k c", c=20)
        o_cT = R_CONST.alloc(80 * 4)
        cTt = f32v(o_cT, 80)
        o_sil = R_CONST.alloc(80 * 2)
        silT = bfv(o_sil, 80).rearrange("p (k r) -> p k r", r=5)

        S.dma("sp", lambda e: e.dma_start(out=cst, in_=d_cst), ("ld", "c0"), writes=["cst"])
        S.dma("sp", lambda e: e.dma_start(out=prm, in_=d_prm), ("ld", "c1"), writes=["prm"])
        S.dma("sp", lambda e: e.dma_start(out=cTt, in_=d_cT), ("ld", "c2"), writes=["cT"])
        S.dma("sp", lambda e: e.dma_start(out=wr.rearrange("p k c -> p (k c)"), in_=d_wr), ("ld", "c3"), writes=["wr"])

        S.op("dve", lambda e: e.tensor_copy(out=identb, in_=ident), ["cst"], ["bfc"])
        S.op("dve", lambda e: e.tensor_copy(out=onesb, in_=ones), ["cst"], ["bfc"])
        S.op("dve", lambda e: e.memset(zerosb, 0.0), [], ["bfc"])
        S.op("dve", lambda e: e.memset(epsc, EPS), [], ["sc"])
        o_gt = R_M.alloc(128 * 4)
        gt = f32v(o_gt, 128)
        S.op("dve", lambda e: e.tensor_tensor(out=gt, in0=prm[:, P_GQR:P_GQR + 128], in1=prm[:, P_GQR:P_GQR + 128], op=ALU.mult), ["prm"], ["gt"])
        S.op("dve", lambda e: e.tensor_reduce(out=sc[:, 6:8], in_=gt.rearrange("p (a b) -> p a b", b=64), axis=AX.X, op=ALU.max), ["gt"], ["sc"])
        S.op("dve", lambda e: e.tensor_tensor(out=negM, in0=mq, in1=mk, op=ALU.mult), ["sc"], ["sc"])
        S.op("dve", lambda e: e.tensor_scalar(out=negM, in0=negM, scalar1=1.0, scalar2=-8.0, op0=ALU.max, op1=ALU.mult), ["sc"], ["sc"])
        S.op("dve", lambda e: e.tensor_scalar(out=gq8, in0=prm[:, P_GQ:P_GQ + 1], scalar1=0.125, scalar2=None, op0=ALU.mult), ["prm"], ["sc"])
        S.op("dve", lambda e: e.tensor_scalar(out=sg8, in0=prm[:, P_SG:P_SG + 1], scalar1=1.0 - LAM_INIT, scalar2=None, op0=ALU.mult), ["prm"], ["sc"])
        o_t = R_M.alloc(256 * 4)
        tl = f32v(o_t, 256)
        lr = prm[:, P_LAMR:P_LAMR + 256]
        S.op("dve", lambda e: e.tensor_tensor(out=tl[:, 0:64], in0=lr[:, 0:64], in1=lr[:, 64:128], op=ALU.mult), ["prm"], ["tl"])
        S.op("dve", lambda e: e.tensor_tensor(out=tl[:, 64:128], in0=lr[:, 128:192], in1=lr[:, 192:256], op=ALU.mult), ["prm"], ["tl"])
        S.op("dve", lambda e: e.tensor_reduce(out=tl[:, 128:130], in_=tl[:, 0:128].rearrange("p (a b) -> p a b", b=64), axis=AX.X, op=ALU.add), ["tl"], ["tl2"])
        S.op("act", lambda e: e.activation(out=tl[:, 130:132], in_=tl[:, 128:130], func=AF.Exp), ["tl2"], ["tl3"])
        S.op("dve", lambda e: e.scalar_tensor_tensor(out=neglam, in0=tl[:, 131:132], scalar=-LAM_INIT, in1=tl[:, 130:131], op0=ALU.add, op1=ALU.subtract), ["tl3"], ["sc"])
        lb2 = prm[:, P_LB:P_LB + 16].rearrange("p (s h) -> p s h", s=2)
        S.op("dve", lambda e: e.tensor_tensor(out=tl[:, 132:140], in0=lb2[:, 0, :], in1=lb2[:, 1, :], op=ALU.subtract), ["prm"], ["tl4"])
        S.op("act", lambda e: e.activation(out=lbh, in_=tl[:, 132:140], func=AF.Sigmoid), ["tl4"], ["sc"])
        S.op("dve", lambda e: e.tensor_scalar(out=omlh, in0=lbh, scalar1=-1.0, scalar2=1.0, op0=ALU.mult, op1=ALU.add), ["sc"], ["sc"])
        S.op("dve", lambda e: e.tensor_scalar(out=nomlh, in0=omlh, scalar1=-1.0, scalar2=None, op0=ALU.mult), ["sc"], ["sc"])
        S.op("dve", lambda e: e.tensor_scalar(out=biasc, in0=biasc, scalar1=negM, scalar2=None, op0=ALU.add), ["cst", "sc"], ["cst"])
        S.op("act", lambda e: e.activation(out=silT.rearrange("p k r -> p (k r)"), in_=cTt, func=AF.Silu), ["cT"], ["silT"])

        ring_off = [R_RING.alloc(16384) for _ in range(2)]
        ring_i = [0]

        def load_w16(src_ap):
            slot = ring_i[0] % 2
            ring_i[0] += 1
            v = bfv(ring_off[slot], 8192)
            key = ("ring", slot)
            S.dma("pool", lambda e: e.dma_start(out=v.rearrange("p (a b) -> p a b", b=2048),
                                                in_=src_ap.rearrange("p (a b) -> p a b", b=2048)),
                  ("ring", slot), writes=[key])
            return v, key

        for grp in range(24):
            w, wk = load_w16(d_wada[grp])
            wv = w.rearrange("p (b k c) -> p b k c", b=4, k=16)
            for blk in range(4):
                cb = grp * 4 + blk
                for k in range(16):
                    S.op("pe", lambda e, cb=cb, blk=blk, k=k, wv=wv: e.matmul(
                        PS[0][:, cb * 5:cb * 5 + 5], lhsT=wv[:, blk, k, :], rhs=silT[:, k, :],
                        start=(k == 0), stop=(k == 15)), [wk, "silT"], [("ps", 0)])
        bT = prm[:, P_BT:P_BT + 96]
        S.op("dve", lambda e: e.tensor_tensor(out=modT, in0=PS[0][:, 0:480].rearrange("p (c r) -> p c r", r=5),
                                              in1=bT.unsqueeze(2).to_broadcast([128, 96, 5]), op=ALU.add),
             [("ps", 0), "prm"], ["modT"])
        n1b = prm[:, P_N1:P_N1 + 16].unsqueeze(2).to_broadcast([128, 16, 5])
        n2b = prm[:, P_N2:P_N2 + 16].unsqueeze(2).to_broadcast([128, 16, 5])
        S.op("dve", lambda e: e.scalar_tensor_tensor(out=G1, in0=modT[:, 16:32, :], scalar=1.0, in1=n1b, op0=ALU.add, op1=ALU.mult), ["modT", "prm"], ["G"])
        S.op("dve", lambda e: e.scalar_tensor_tensor(out=G2, in0=modT[:, 64:80, :], scalar=1.0, in1=n2b, op0=ALU.add, op1=ALU.mult), ["modT", "prm"], ["G"])
        SH1 = modT[:, 0:16, :]
        GA1 = modT[:, 32:48, :]
        SH2 = modT[:, 48:64, :]
        GA2 = modT[:, 80:96, :]
        S.barrier()
        chk("p1")
        R_M.reset()

        o_h = R_H.alloc(16 * NT * 2)
        DBG["hT"] = o_h
        hT = bfv(o_h, 16 * NT).rearrange("p (k t) -> p k t", t=NT)

        def rstd_from_ps(ps_ap, n, inv_n, out_ap, tmp_ap, rd, wr_key):
            S.op("act", lambda e: e.activation(out=out_ap, in_=ps_ap, func=AF.Ln, bias=epsc, scale=inv_n), rd + ["sc"], [wr_key])
            S.op("act", lambda e: e.activation(out=out_ap, in_=out_ap, func=AF.Exp, scale=-0.5), [wr_key], [wr_key])

        def make_hT(d_x, blocks, gmod, shmod, dst, G, SHm, name):
            o_xb = [R_M.alloc(2 * 512 * 4) for _ in range(2)]
            o_sq = [R_M.alloc(512 * 4) for _ in range(2)]
            o_rs = R_M.alloc(512 * 4)
            o_tp = [R_M.alloc(512 * 4) for _ in range(2)]
            cnt = [0]
            for (src, t0, n, rsel) in blocks:
                rs = f32v(o_rs, 512)[:, 0:n]
                for kg in range(8):
                    b = cnt[0] % 2
                    cnt[0] += 1
                    xb = f32v(o_xb[b], 1024).rearrange("p (k t) -> p k t", k=2)[:, :, 0:n]
                    S.dma("sp", lambda e, xb=xb, src=src, kg=kg: e.dma_start(out=xb, in_=src[:, kg * 2:kg * 2 + 2, :]),
                          ("ld", "xb%d" % b), writes=[("xb", b)])
                    for kk in range(2):
                        k = kg * 2 + kk
                        sb = k % 2
                        sq = f32v(o_sq[sb], 512)[:, 0:n]
                        S.op("act", lambda e, sq=sq, xb=xb, kk=kk: e.activation(out=sq, in_=xb[:, kk, :], func=AF.Square), [("xb", b)], [("sq", sb)])
                        S.op("pe", lambda e, sq=sq, k=k, n=n: e.matmul(PS[7][:, 0:n], lhsT=ones, rhs=sq, start=(k == 0), stop=(k == 15)),
                             [("sq", sb), "cst"], [("ps", 7)])
                rstd_from_ps(PS[7][:, 0:n], n, 1.0 / 2048.0, rs, rs, [("ps", 7)], "rs")
                for kg in range(8):
                    b = cnt[0] % 2
                    cnt[0] += 1
                    xb = f32v(o_xb[b], 1024).rearrange("p (k t) -> p k t", k=2)[:, :, 0:n]
                    S.dma("sp", lambda e, xb=xb, src=src, kg=kg: e.dma_start(out=xb, in_=src[:, kg * 2:kg * 2 + 2, :]),
                          ("ld", "xb%d" % b), writes=[("xb", b)])
                    for kk in range(2):
                        k = kg * 2 + kk
                        tb_ = k % 2
                        for (c0, cn, r) in rsel:
                            tp = f32v(o_tp[tb_], 512)[:, c0:c0 + cn]
                            S.op("dve", lambda e, tp=tp, xb=xb, kk=kk, k=k, r=r, c0=c0, cn=cn, rs=rs: e.scalar_tensor_tensor(
                                out=tp, in0=xb[:, kk, c0:c0 + cn], scalar=G[:, k, r:r + 1], in1=rs[:, c0:c0 + cn],
                                op0=ALU.mult, op1=ALU.mult), [("xb", b), "G", "rs"], [("tp", tb_)])
                            S.op("act", lambda e, tp=tp, k=k, r=r, c0=c0, cn=cn, t0=t0: e.activation(
                                out=dst[:, k, t0 + c0:t0 + c0 + cn], in_=tp, func=AF.Identity, bias=SHm[:, k, r:r + 1], scale=1.0),
                                [("tp", tb_), "modT"], [name])

        PBLK = [(0, 512), (512, 512)]
        SAMPLE_RSEL = [(4 * bi, 4, 1 + bi) for bi in range(4)]

        o_kT = R_X.alloc(4 * 2064 * 2)
        DBG["kT"] = o_kT
        kT = bfv(o_kT, 4 * 2064).rearrange("p (h t) -> p h t", h=4)
        o_v = R_X.alloc(16 * 512 * 2)
        DBG["vT"] = o_v
        vT = bfv(o_v, 16 * 512).rearrange("p (t c) -> p t c", c=512)
        o_vs = R_X.alloc(4 * 512 * 2)
        vS = bfv(o_vs, 4 * 512).rearrange("p (b c) -> p b c", c=512)
        o_qT = R_X.alloc(8 * NT * 2)
        DBG["qT"] = o_qT
        qT = bfv(o_qT, 8 * NT).rearrange("p (h t) -> p h t", h=8)
        x_mark = R_X.cur

        psc = [0]

        def next_ps(lo=0, hi=6):
            i = lo + psc[0] % (hi - lo)
            psc[0] += 1
            return i

        def proj_fm(wv, wk, blk, src, t0, n, post):
            pi = next_ps()
            for k in range(16):
                S.op("pe", lambda e, k=k, pi=pi: e.matmul(PS[pi][:, 0:n], lhsT=wv[:, blk, k, :], rhs=src[:, k, t0:t0 + n],
                                                          start=(k == 0), stop=(k == 15)), [wk, "hT"], [("ps", pi)])
            post(pi)

        def proj_tm(wv, wk, src, t0, m, post):
            pi = next_ps()
            for k in range(16):
                S.op("pe", lambda e, k=k, pi=pi: e.matmul(PS[pi][0:m, :], lhsT=src[:, k, t0:t0 + m], rhs=wv[:, k, :],
                                                          start=(k == 0), stop=(k == 15)), [wk, "hT"], [("ps", pi)])
            post(pi)

        tmp_off = {}

        def tmpf(name, n=512, region=None):
            if name not in tmp_off:
                tmp_off[name] = (region or R_M).alloc(n * 4)
            return f32v(tmp_off[name], n)

        def tmpb(name, n=512, region=None):
            if name not in tmp_off:
                tmp_off[name] = (region or R_M).alloc(n * 2)
            return bfv(tmp_off[name], n)

        def qk_post(pi, n, gcol, out_bf, out_f32_key=None, out_f32=None, tag="qk"):
            sq = tmpf(tag + "sq")[:, 0:n]
            rs = tmpf(tag + "rs")[:, 0:n]
            S.op("act", lambda e: e.activation(out=sq, in_=PS[pi][:, 0:n], func=AF.Square), [("ps", pi)], [tag + "sq"])
            S.op("pe", lambda e: e.matmul(PS[6][:, 0:n], lhsT=bones, rhs=sq, start=True, stop=True), [tag + "sq", "cst"], [("ps", 6)])
            rstd_from_ps(PS[6][:, 0:n], n, 1.0 / 64.0, rs, rs, [("ps", 6)], tag + "rs")
            if out_f32 is not None:
                S.op("dve", lambda e: e.scalar_tensor_tensor(out=out_f32, in0=PS[pi][:, 0:n], scalar=gcol, in1=rs, op0=ALU.mult, op1=ALU.mult),
                     [("ps", pi), tag + "rs", "sc", "prm"], [out_f32_key])
                S.op("act", lambda e: e.activation(out=out_bf[0], in_=out_f32, func=AF.Copy), [out_f32_key], [out_bf[1]])
            else:
                S.op("dve", lambda e: e.scalar_tensor_tensor(out=out_bf[0], in0=PS[pi][:, 0:n], scalar=gcol, in1=rs, op0=ALU.mult, op1=ALU.mult),
                     [("ps", pi), tag + "rs", "sc", "prm"], [out_bf[1]])

        gk = prm[:, P_GK:P_GK + 1]
        flag = prm[:, P_FLAG:P_FLAG + 1]
        hgg = prm[:, P_HGG:P_HGG + 1]

        def hgrn_bufs(region, ntok, ntile, with_q):
            d = {}
            d["kt"] = bfv(region.alloc(4 * ntok * 2), 4 * ntok).rearrange("p (h t) -> p h t", h=4)
            d["khat"] = bfv(region.alloc(ntile * 512 * 2), ntile * 512).rearrange("p (t c) -> p t c", c=512)
            d["hi"] = bfv(region.alloc(ntile * 512 * 2), ntile * 512).rearrange("p (t c) -> p t c", c=512)
            nch = ntok // 64 + 4
            d["d"] = f32v(region.alloc(4 * nch * 4), 4 * nch).rearrange("p (h c) -> p h c", h=4)
            if with_q:
                d["eg"] = bfv(region.alloc(4 * ntok * 2), 4 * ntok).rearrange("p (h t) -> p h t", h=4)
                d["qt"] = bfv(region.alloc(4 * ntok * 2), 4 * ntok).rearrange("p (h t) -> p h t", h=4)
                d["gate"] = bfv(region.alloc(4 * ntok * 2), 4 * ntok).rearrange("p (h t) -> p h t", h=4)
                d["khs"] = bfv(region.alloc(4 * 512 * 2), 4 * 512).rearrange("p (b c) -> p b c", c=512)
                d["his"] = bfv(region.alloc(4 * 512 * 2), 4 * 512).rearrange("p (b c) -> p b c", c=512)
            return d

        o_Sst = R_CONST.alloc(8 * 128 * 4)
        DBG["Sst"] = o_Sst
        Sst = f32v(o_Sst, 1024).rearrange("p (h v) -> p h v", h=8)

        def hf_post(pi, hb, hl, hglob, t0, n, with_q, is_sample, tag):
            sg = tmpf("hf_sg")[:, 0:n]
            lf = tmpf("hf_lf")[:, 0:n]
            hk = tmpf("hf_hk")[:, 0:n]
            G = tmpf("hf_G")[:, 0:n]
            eg = tmpf("hf_eg")[:, 0:n]
            kf = sg
            khT = tmpb("hf_khT")[:, 0:n]
            S.op("act", lambda e: e.activation(out=sg, in_=PS[pi][:, 0:n], func=AF.Sigmoid), [("ps", pi)], ["hf_sg"])
            S.op("dve", lambda e: e.tensor_scalar(out=lf, in0=sg, scalar1=omlh[:, hglob:hglob + 1], scalar2=lbh[:, hglob:hglob + 1],
                                                  op0=ALU.mult, op1=ALU.add), ["hf_sg", "sc"], ["hf_lf"])
            S.op("act", lambda e: e.activation(out=lf, in_=lf, func=AF.Ln), ["hf_lf"], ["hf_lf"])
            S.op("dve", lambda e: e.tensor_scalar(out=hk, in0=sg, scalar1=nomlh[:, hglob:hglob + 1], scalar2=omlh[:, hglob:hglob + 1],
                                                  op0=ALU.mult, op1=ALU.add), ["hf_sg", "sc"], ["hf_hk"])
            msk = scanms[:, 0:n] if is_sample else scanm[:, 0:n]
            S.op("dve", lambda e: e.tensor_tensor_scan(out=G, data0=msk, data1=lf, initial=0.0, op0=ALU.mult, op1=ALU.add),
                 ["hf_lf", "cst"], ["hf_G"])
            S.op("act", lambda e: e.activation(out=eg, in_=G, func=AF.Exp), ["hf_G"], ["hf_eg"])
            S.op("act", lambda e: e.activation(out=G, in_=G, func=AF.Exp, scale=-1.0), ["hf_G"], ["hf_G"])
            S.op("dve", lambda e: e.tensor_tensor(out=kf, in0=hk, in1=G, op=ALU.mult), ["hf_hk", "hf_G", "hf_sg"], ["hf_sg"])
            csz = 4 if is_sample else 64
            ncn = n // csz
            c0 = t0 // 64 if not is_sample else 16
            dv = hb["d"][:, hl, c0:c0 + ncn]
            S.op("dve", lambda e: e.tensor_copy(out=dv, in_=eg.rearrange("p (c s) -> p c s", s=csz)[:, :, csz - 1]), ["hf_eg"], [tag + "d"])
            S.op("act", lambda e: e.activation(out=hb["kt"][:, hl, t0:t0 + n], in_=kf, func=AF.Copy), ["hf_sg"], [tag + "kt"])
            if with_q:
                S.op("act", lambda e: e.activation(out=hb["eg"][:, hl, t0:t0 + n], in_=eg, func=AF.Copy), ["hf_eg"], [tag + "eg"])
            S.op("dve", lambda e: e.tensor_tensor(out=khT.rearrange("p (c s) -> p c s", s=csz), in0=kf.rearrange("p (c s) -> p c s", s=csz),
                                                  in1=dv.unsqueeze(2).to_broadcast([128, ncn, csz]), op=ALU.mult),
                 ["hf_sg", tag + "d"], ["hf_khT"])
            PSb = PS[6].bitcast(BF16)
            if not is_sample:
                for j in range(n // 128):
                    S.op("pe", lambda e, j=j: e.transpose(out=PSb[:, j * 128:(j + 1) * 128], in_=khT[:, j * 128:(j + 1) * 128], identity=identb),
                         ["hf_khT", "bfc"], [("ps", 6)])
                tl0 = t0 // 128
                S.op("act", lambda e: e.activation(out=hb["khat"][:, tl0:tl0 + n // 128, hl * 128:(hl + 1) * 128],
                                                   in_=PSb[:, 0:n].rearrange("p (j c) -> p j c", c=128), func=AF.Copy),
                     [("ps", 6)], [tag + "khat"])
            else:
                for bi in range(4):
                    S.op("pe", lambda e, bi=bi: e.transpose(out=PSb[0:4, bi * 128:(bi + 1) * 128], in_=khT[:, bi * 4:bi * 4 + 4], identity=identb),
                         ["hf_khT", "bfc"], [("ps", 6)])
                S.op("act", lambda e: e.activation(out=hb["khs"][0:4, :, hl * 128:(hl + 1) * 128],
                                                   in_=PSb[0:4, 0:512].rearrange("p (j c) -> p j c", c=128), func=AF.Copy),
                     [("ps", 6)], [tag + "khs"])

        def state_chain(hb, hl, hglob, tile0, nchunk, ch0, store_bf, tag):
            for c in range(nchunk):
                tl_, e2 = tile0 + c // 2, c % 2
                pu = next_ps()
                if store_bf is not None:
                    S.op("act", lambda e, c=c: e.activation(out=store_bf[:, c, :], in_=Sst[:, hglob, :], func=AF.Copy), [("S", hglob)], [tag + "Sbf"])
                S.op("pe", lambda e, c=c, tl_=tl_, e2=e2, pu=pu: e.matmul(
                    PS[pu][:, 0:128],
                    lhsT=hb["khat"][64 * e2:64 * e2 + 64, tl_, hl * 128:(hl + 1) * 128],
                    rhs=hb["hi"][64 * e2:64 * e2 + 64, tl_, hl * 128:(hl + 1) * 128], start=True, stop=True),
                    [tag + "khat", tag + "hi"], [("ps", pu)])
                S.op("dve", lambda e, c=c, pu=pu: e.scalar_tensor_tensor(
                    out=Sst[:, hglob, :], in0=Sst[:, hglob, :], scalar=hb["d"][:, hl, ch0 + c:ch0 + c + 1],
                    in1=PS[pu][:, 0:128], op0=ALU.mult, op1=ALU.add),
                    [("ps", pu), tag + "d", ("S", hglob)], [("S", hglob)])

        make_hT(d_xp, [(d_xp[:, :, t0:t0 + n], t0, n, [(0, n, 0)]) for (t0, n) in PBLK], G1, SH1, hT, G1, SH1, "hT")
        S.barrier()
        R_M.reset()
        for h in range(8):
            S.op("dve", lambda e, h=h: e.memset(Sst[:, h, :], 0.0), [], [("S", h)])
        R_AM_mark = R_AM.cur
        g_i = 0
        w, wk = load_w16(d_win[g_i]); g_i += 1
        wv = w.rearrange("p (b k c) -> p b k c", b=4, k=16)
        for blk in range(4):
            for (t0, n) in PBLK:
                proj_fm(wv, wk, blk, hT, t0, n, lambda pi, blk=blk, t0=t0, n=n: qk_post(
                    pi, n, gk, (kT[:, blk, t0:t0 + n], "kT"), tag="qk"))
        w, wk = load_w16(d_win[g_i]); g_i += 1
        wv = w.rearrange("p (k c) -> p k c", k=16)
        for tl_ in range(8):
            proj_tm(wv, wk, hT, tl_ * 128, 128, lambda pi, tl_=tl_: S.op(
                "act", lambda e: e.activation(out=vT[:, tl_, :], in_=PS[pi][:, :], func=AF.Copy), [("ps", pi)], ["vT"]))
        for hgp in range(2):
            hb = hgrn_bufs(R_AM, 1024, 8, False)
            w, wk = load_w16(d_win[g_i]); g_i += 1
            wv = w.rearrange("p (b k c) -> p b k c", b=4, k=16)
            for hl in range(4):
                for (t0, n) in PBLK:
                    proj_fm(wv, wk, hl, hT, t0, n, lambda pi, hl=hl, t0=t0, n=n, hb=hb: hf_post(
                        pi, hb, hl, hgp * 4 + hl, t0, n, False, False, "p"))
            w, wk = load_w16(d_win[g_i]); g_i += 1
            wv = w.rearrange("p (k c) -> p k c", k=16)
            for tl_ in range(8):
                proj_tm(wv, wk, hT, tl_ * 128, 128, lambda pi, tl_=tl_, hb=hb: S.op(
                    "act", lambda e: e.activation(out=hb["hi"][:, tl_, :], in_=PS[pi][:, :], func=AF.Copy), [("ps", pi)], ["phi"]))
            for hl in range(4):
                state_chain(hb, hl, hgp * 4 + hl, 0, 16, 0, None, "p")
            S.barrier()
            R_AM.cur = R_AM_mark
        for h in range(8):
            S.op("dve", lambda e, h=h: e.tensor_scalar(out=Sst[:, h, :], in0=Sst[:, h, :], scalar1=flag, scalar2=None, op0=ALU.mult),
                 [("S", h), "prm"], [("S", h)])
        S.barrier()
        chk("p2")
        R_M.reset()
        tmp_off.clear()

        OWN_BLOCKS = [(d_xo[:, :, t0:t0 + n], t0, n, [(0, n, 0)]) for (t0, n) in PBLK] + [(d_xs, 1024, 16, SAMPLE_RSEL)]
        make_hT(None, OWN_BLOCKS, G1, SH1, hT, G1, SH1, "hT")
        S.barrier()
        chk("p3a0")
        R_M.reset()
        TBS = [(0, 512), (512, 512), (1024, 16)]
        o_am = R_AM.alloc(16 * NT * 2)
        DBG["amT"] = o_am
        amT = bfv(o_am, 16 * NT).rearrange("p (f t) -> p f t", f=16)
        kst = [tmpf("kst0"), tmpf("kst1")]
        vst = [tmpf("vst0"), tmpf("vst1")]
        stc = [0]
        for qg in range(2):
            w, wk = load_w16(d_win[g_i]); g_i += 1
            wv = w.rearrange("p (b k c) -> p b k c", b=4, k=16)
            for blk in range(4):
                h = qg * 4 + blk
                for (t0, n) in TBS:
                    proj_fm(wv, wk, blk, hT, t0, n, lambda pi, h=h, t0=t0, n=n: qk_post(pi, n, gq8, (qT[:, h, t0:t0 + n], "qT"), tag="qk"))
        S.barrier()
        chk("p3a1")
        w, wk = load_w16(d_win[g_i]); g_i += 1
        wv = w.rearrange("p (b k c) -> p b k c", b=4, k=16)
        for blk in range(4):
            for (t0, n) in TBS:
                def kpost(pi, blk=blk, t0=t0, n=n):
                    b = stc[0] % 2
                    stc[0] += 1
                    st = kst[b][:, 0:n]
                    qk_post(pi, n, gk, (kT[:, blk, 1024 + t0:1024 + t0 + n], "kT"), out_f32_key=("kst", b), out_f32=st, tag="qk")
                    S.dma("sp", lambda e: e.dma_start(out=out_kT[:, blk * NT + t0:blk * NT + t0 + n], in_=st), ("out", "k%d" % b), reads=[("kst", b)])
                proj_fm(wv, wk, blk, hT, t0, n, kpost)
        S.barrier()
        chk("p3a2")
        w, wk = load_w16(d_win[g_i]); g_i += 1
        wv = w.rearrange("p (k c) -> p k c", k=16)
        for tl_ in range(8):
            def vpost(pi, tl_=tl_):
                b = stc[0] % 2
                stc[0] += 1
                S.op("act", lambda e: e.activation(out=vT[:, 8 + tl_, :], in_=PS[pi][:, :], func=AF.Copy), [("ps", pi)], ["vT"])
                S.op("dve", lambda e: e.tensor_copy(out=vst[b], in_=PS[pi][:, :]), [("ps", pi)], [("vst", b)])
                S.dma("sp", lambda e: e.dma_start(out=out_v[tl_ * 128:(tl_ + 1) * 128, :], in_=vst[b]), ("out", "v%d" % b), reads=[("vst", b)])
            proj_tm(wv, wk, hT, tl_ * 128, 128, vpost)
        S.barrier()
        chk("p3a3")
        for bi in range(4):
            def vspost(pi, bi=bi):
                b = stc[0] % 2
                stc[0] += 1
                S.op("act", lambda e: e.activation(out=vS[0:4, bi, :], in_=PS[pi][0:4, :], func=AF.Copy), [("ps", pi)], ["vS"])
                S.op("dve", lambda e: e.tensor_copy(out=vst[b][0:4, :], in_=PS[pi][0:4, :]), [("ps", pi)], [("vst", b)])
                S.dma("sp", lambda e: e.dma_start(out=out_v[1024 + bi * 4:1028 + bi * 4, :], in_=vst[b][0:4, :]), ("out", "v%d" % b), reads=[("vst", b)])
            proj_tm(wv, wk, hT, 1024 + bi * 4, 4, vspost)
        S.barrier()
        chk("p3a")
        R_M.reset()
        tmp_off.clear()

        att_tmp = [tmpf("att_t0"), tmpf("att_t1")]
        att_P = [tmpb("att_P0"), tmpb("att_P1"), tmpb("att_P2"), tmpb("att_P3")]
        att_o = tmpf("att_o")
        att_r = [tmpf("att_r0"), tmpf("att_r1")]
        pc = [0]

        def subln_finish(o_ap, n, out_ap, tag, gcol, extra=None, a3=None):
            sq = tmpf("sl_sq")[:, 0:n]
            rs = tmpf("sl_rs")[:, 0:n]
            o3, rs3 = o_ap, rs
            if a3 is not None:
                o3 = o_ap.rearrange("p (a b) -> p a b", a=a3)
                rs3 = rs.rearrange("p (a b) -> p a b", a=a3)
            S.op("act", lambda e: e.activation(out=sq, in_=o_ap, func=AF.Square), [tag], ["sl_sq"])
            S.op("pe", lambda e: e.matmul(PS[6][:, 0:n], lhsT=ones, rhs=sq, start=True, stop=True), ["sl_sq", "cst"], [("ps", 6)])
            rstd_from_ps(PS[6][:, 0:n], n, 1.0 / 128.0, rs, rs, [("ps", 6)], "sl_rs")
            if extra is None:
                S.op("dve", lambda e: e.scalar_tensor_tensor(out=out_ap, in0=o3, scalar=gcol, in1=rs3, op0=ALU.mult, op1=ALU.mult),
                     [tag, "sl_rs", "sc", "prm"], ["amT"])
            else:
                S.op("dve", lambda e: e.scalar_tensor_tensor(out=rs, in0=o_ap, scalar=gcol, in1=rs, op0=ALU.mult, op1=ALU.mult),
                     [tag, "sl_rs", "sc", "prm"], ["sl_rs"])
                S.op("dve", lambda e: e.tensor_tensor(out=out_ap, in0=rs, in1=extra[0], op=ALU.mult), ["sl_rs", extra[1]], ["amT"])

        att_tmp.append(tmpf("att_t2"))
        SBK = [4, 5, 7]
        units = []
        for h in range(8):
            for qb in range(2):
                tiles = _tile_list(qb)
                for ti, kt in enumerate(tiles):
                    for m in range(2):
                        units.append((h, qb, ti, kt, m, len(tiles)))

        def stage_a(i, u):
            h, qb, ti, kt, m, nt = u
            kvh = h // 2
            sb, pb = i % 3, i % 4
            d0 = 1024 + 512 * qb - 128 * kt
            ws = 512 if d0 >= 128 else d0 + 384
            ndv = cst[:, C_ND + ws:C_ND + ws + 512]
            S.op("pe", lambda e: e.matmul(
                PS[SBK[sb]][:, :], lhsT=kT[64 * m:64 * m + 64, kvh, kt * 128:(kt + 1) * 128],
                rhs=qT[64 * m:64 * m + 64, h, qb * 512:(qb + 1) * 512], start=True, stop=True),
                ["kT", "qT"], [("ps", SBK[sb])])
            S.op("dve", lambda e: e.scalar_tensor_tensor(
                out=att_tmp[sb], in0=ndv, scalar=SLOPES[h], in1=PS[SBK[sb]][:, :], op0=ALU.mult, op1=ALU.add),
                [("ps", SBK[sb]), "cst"], [("att_t", sb)])
            bc = biasc[:, (h * 2 + qb) * 16 + kt:(h * 2 + qb) * 16 + kt + 1]
            S.op("act", lambda e: e.activation(out=att_P[pb], in_=att_tmp[sb], func=AF.Exp, bias=bc, scale=1.0),
                 [("att_t", sb), "cst"], [("att_P", pb)])

        def fin1(h, qb):
            for m in range(2):
                S.op("act", lambda e, m=m: e.activation(out=att_r[m], in_=PS[2 + m][:, :], func=AF.Ln), [("ps", 2 + m)], [("att_r", m)])
                S.op("act", lambda e, m=m: e.activation(out=att_r[m], in_=att_r[m], func=AF.Exp, scale=-1.0), [("att_r", m)], [("att_r", m)])
                S.op("dve", lambda e, m=m: e.tensor_tensor(out=att_r[m], in0=PS[m][:, :], in1=att_r[m], op=ALU.mult),
                     [("ps", m), ("att_r", m)], [("att_r", m)])
            S.op("dve", lambda e: e.scalar_tensor_tensor(out=att_o, in0=att_r[1], scalar=neglam, in1=att_r[0], op0=ALU.mult, op1=ALU.add),
                 [("att_r", 0), ("att_r", 1), "sc"], ["att_o"])
            sq = tmpf("sl_sq")
            S.op("act", lambda e: e.activation(out=sq, in_=att_o, func=AF.Square), ["att_o"], ["sl_sq"])

        def fin2(h, qb):
            sq = tmpf("sl_sq")
            rs = tmpf("sl_rs")
            S.op("pe", lambda e: e.matmul(PS[6][:, :], lhsT=ones, rhs=sq, start=True, stop=True), ["sl_sq", "cst"], [("ps", 6)])
            rstd_from_ps(PS[6][:, :], 512, 1.0 / 128.0, rs, rs, [("ps", 6)], "sl_rs")
            S.op("dve", lambda e: e.scalar_tensor_tensor(out=amT[:, h, qb * 512:(qb + 1) * 512], in0=att_o, scalar=sg8, in1=rs,
                                                         op0=ALU.mult, op1=ALU.mult), ["att_o", "sl_rs", "sc", "prm"], ["amT"])

        def stage_b(i, u):
            h, qb, ti, kt, m, nt = u
            kvh = h // 2
            pb = i % 4
            S.op("pe", lambda e: e.matmul(PS[m][:, :], lhsT=vT[:, kt, kvh * 128:(kvh + 1) * 128], rhs=att_P[pb],
                                          start=(ti == 0), stop=(ti == nt - 1)), [("att_P", pb), "vT"], [("ps", m)])
            S.op("pe", lambda e: e.matmul(PS[2 + m][:, :], lhsT=onesb, rhs=att_P[pb],
                                          start=(ti == 0), stop=(ti == nt - 1)), [("att_P", pb), "bfc"], [("ps", 2 + m)])

        pend = []
        NU = len(units)
        for i in range(NU + 2):
            if i < NU:
                stage_a(i, units[i])
            if i >= 2:
                u = units[i - 2]
                stage_b(i - 2, u)
                for p_ in pend:
                    p_[2] -= 1
                while pend and pend[0][2] <= 0:
                    hh, qq, _ = pend.pop(0)
                    fin2(hh, qq)
                if u[2] == u[5] - 1 and u[4] == 1:
                    fin1(u[0], u[1])
                    pend.append([u[0], u[1], 4])
        for hh, qq, _ in pend:
            fin2(hh, qq)
        S.barrier()
        chk("p3b")
        R_M.reset()
        tmp_off.clear()

        o_pos = R_X.alloc(2048 * 4)
        posT = f32v(o_pos, 2048, 3).rearrange("p (b i) -> p b i", b=16)
        o_R3 = R_X.alloc(512 * 4)
        R3 = f32v(o_R3, 512, 3)
        o_nb = R_X.alloc(64 * 4)
        newb = f32v(o_nb, 64, 4)
        o_idx = R_X.alloc(512 * 4)
        idx = i32v(o_idx, 512)
        S.dma("sp", lambda e: e.dma_start(out=posT.rearrange("p b i -> p (b i)"), in_=d_posT), ("ld", "c0"), writes=["posT"])
        S.dma("sp", lambda e: e.dma_start(out=R3, in_=d_R3), ("ld", "c1"), writes=["R3"])
        S.dma("sp", lambda e: e.dma_start(out=newb, in_=d_newb), ("ld", "c2"), writes=["newb"])
        tmpf("sl_sq", 64)
        tmpf("sl_rs", 64)
        o_pt_ = R_M.alloc(512 * 4)
        pti = i32v(o_pt_, 512)
        ptf = f32v(o_pt_, 512)
        pid = f32v(R_M.alloc(4), 1)
        S.dma("sp", lambda e: e.dma_start(out=pti, in_=d_pt.rearrange("b n -> (b n)").partition_broadcast(128)), ("ld", "c3"), writes=["pti"])
        S.op("pool", lambda e: e.iota(pid, pattern=[[0, 1]], base=0, channel_multiplier=1, allow_small_or_imprecise_dtypes=True), [], ["pid"])
        S.op("dve", lambda e: e.tensor_copy(out=ptf, in_=pti), ["pti"], ["ptf"])
        S.op("dve", lambda e: e.tensor_scalar(out=ptf, in0=ptf, scalar1=128.0, scalar2=pid, op0=ALU.mult, op1=ALU.add), ["ptf", "pid"], ["ptf"])
        S.op("dve", lambda e: e.tensor_copy(out=idx, in_=ptf), ["ptf"], ["idx"])
        NSL = 8
        kvr = [bfv(R_M.alloc(2048), 1024) for _ in range(NSL)]
        kr = [t[:, 0:512] for t in kvr]
        vr = [t[:, 512:1024] for t in kvr]
        qbd = bfv(R_M.alloc(64 * 2), 64).rearrange("p (k c) -> p k c", k=4)
        Ps = [bfv(R_M.alloc(1024), 512) for _ in range(2)]
        Pn = bfv(R_M.alloc(128), 64, 4)
        sa_t = f32v(R_M.alloc(64 * 4), 64)
        sa_r = f32v(R_M.alloc(64 * 4), 64)
        sa_o = f32v(R_M.alloc(32 * 4), 32)
        pgc = [0]
        for bi in range(4):
            S.op("dve", lambda e: e.memset(qbd.rearrange("p k c -> p (k c)"), 0.0), [], ["qbd"])
            for kvh in range(4):
                for g in range(2):
                    for m in range(2):
                        S.op("dve", lambda e, kvh=kvh, g=g, m=m, bi=bi: e.tensor_copy(
                            out=qbd[64 * m:64 * m + 64, kvh, m * 8 + g * 4:m * 8 + g * 4 + 4],
                            in_=qT[64 * m:64 * m + 64, kvh * 2 + g, 1024 + bi * 4:1028 + bi * 4]), ["qT"], ["qbd"])
            S.op("pe", lambda e: e.matmul(PS[2][:, 0:128], lhsT=zerosb[:, 0:128], rhs=zerosb[:, 0:128], start=True, stop=False),
                 ["bfc"], [("ps", 2)])
            NPB, NRB = 2, 4
            NBT = 128 // NPB

            def sa_stage_a(Bq):
                sb = Bq % 2
                B8, r0 = (Bq * NPB) // 8, (Bq * NPB) % 8
                W_ = NPB * 64
                S.op("pe", lambda e: e.matmul(PS[sb][:, 0:W_], lhsT=posT[:, B8, :], rhs=R3[:, r0 * 64:(r0 + NPB) * 64], start=True, stop=False),
                     ["posT", "R3"], [("ps", sb)])
                for r in range(NPB):
                    pg = Bq * NPB + r
                    sl = (Bq % NRB) * NPB + r
                    ia = idx[:, bi * 128 + pg:bi * 128 + pg + 1]
                    S.dma("pool", lambda e: e.indirect_dma_start(
                        out=kvr[sl], out_offset=None, in_=d_ckv, in_offset=bass.IndirectOffsetOnAxis(ap=ia, axis=0)),
                        ("kvr", sl), reads=["idx"], writes=[("kr", sl), ("vr", sl)])
                    for kvh in range(4):
                        S.op("pe", lambda e, kvh=kvh: e.matmul(
                            PS[sb][:, r * 64 + kvh * 16:r * 64 + kvh * 16 + 16], lhsT=kr[sl][:, kvh * 128:(kvh + 1) * 128],
                            rhs=qbd[:, kvh, :], start=False, stop=(r == NPB - 1 and kvh == 3)), [("kr", sl), "qbd"], [("ps", sb)])
                S.op("act", lambda e: e.activation(out=Ps[sb][:, 0:W_], in_=PS[sb][:, 0:W_], func=AF.Exp, bias=negM, scale=1.0),
                     [("ps", sb), "sc"], [("Ps", sb)])

            def sa_stage_b(Bq):
                sb = Bq % 2
                for r in range(NPB):
                    sl = (Bq % NRB) * NPB + r
                    for kvh in range(4):
                        S.op("pe", lambda e, kvh=kvh: e.matmul(
                            PS[2][:, kvh * 16:kvh * 16 + 16], lhsT=vr[sl][:, kvh * 128:(kvh + 1) * 128],
                            rhs=Ps[sb][:, r * 64 + kvh * 16:r * 64 + kvh * 16 + 16], start=False, stop=False),
                            [("vr", sl), ("Ps", sb)], [("ps", 2)])
                    S.op("pe", lambda e: e.matmul(PS[2][:, 64:128], lhsT=onesb, rhs=Ps[sb][:, r * 64:(r + 1) * 64],
                                                  start=False, stop=False), [("Ps", sb), "bfc"], [("ps", 2)])

            sa_stage_a(0)
            for Bq in range(NBT):
                if Bq + 1 < NBT:
                    sa_stage_a(Bq + 1)
                sa_stage_b(Bq)
            for kvh in range(4):
                S.op("pe", lambda e, kvh=kvh, bi=bi: e.matmul(PS[3][0:4, kvh * 16:kvh * 16 + 16], lhsT=kT[:, kvh, 2048 + bi * 4:2052 + bi * 4],
                                                              rhs=qbd[:, kvh, :], start=True, stop=True), ["kT", "qbd"], [("ps", 3)])
            S.op("dve", lambda e: e.tensor_tensor(out=sa_t[0:4, :], in0=PS[3][0:4, 0:64], in1=newb, op=ALU.add), [("ps", 3), "newb"], ["sa_t"])
            S.op("act", lambda e: e.activation(out=Pn, in_=sa_t[0:4, :], func=AF.Exp, bias=negM[0:4, :], scale=1.0), ["sa_t", "sc"], ["Pn"])
            for kvh in range(4):
                S.op("pe", lambda e, kvh=kvh, bi=bi: e.matmul(PS[2][:, kvh * 16:kvh * 16 + 16], lhsT=vS[0:4, bi, kvh * 128:(kvh + 1) * 128],
                                                              rhs=Pn[:, kvh * 16:kvh * 16 + 16], start=False, stop=False), ["vS", "Pn"], [("ps", 2)])
            S.op("pe", lambda e: e.matmul(PS[2][:, 64:128], lhsT=onesb[0:4, :], rhs=Pn, start=False, stop=True), ["Pn", "bfc"], [("ps", 2)])
            S.op("act", lambda e: e.activation(out=sa_r, in_=PS[2][:, 64:128], func=AF.Ln), [("ps", 2)], ["sa_r"])
            S.op("act", lambda e: e.activation(out=sa_r, in_=sa_r, func=AF.Exp, scale=-1.0), ["sa_r"], ["sa_r"])
            S.op("dve", lambda e: e.tensor_tensor(out=sa_r, in0=PS[2][:, 0:64], in1=sa_r, op=ALU.mult), [("ps", 2), "sa_r"], ["sa_r"])
            T4 = sa_r.rearrange("p (k m c) -> p k m c", k=4, m=2)
            S.op("dve", lambda e: e.scalar_tensor_tensor(out=sa_o.rearrange("p (k c) -> p k c", k=4), in0=T4[:, :, 1, :], scalar=neglam,
                                                         in1=T4[:, :, 0, :], op0=ALU.mult, op1=ALU.add), ["sa_r", "sc"], ["sa_o"])
            subln_finish(sa_o, 32, amT[:, 0:8, 1024 + bi * 4:1028 + bi * 4], "sa_o", sg8, a3=8)
        S.barrier()
        chk("p3c")
        R_M.reset()
        tmp_off.clear()
        R_X.reset()

        o_Sbf = R_X.alloc(17 * 128 * 2)
        Sbf = bfv(o_Sbf, 17 * 128).rearrange("p (c v) -> p c v", c=17)
        o_abf = R_X.alloc(256 * 2)
        abf = bfv(o_abf, 256).rearrange("p (j t) -> p j t", j=4)
        o_as = R_X.alloc(16 * 2)
        a_s = bfv(o_as, 16, 4)[:, 0:4]
        Ss = f32v(R_M.alloc(128 * 4), 128)
        Ssb = bfv(R_M.alloc(128 * 2), 128)
        Sso = [f32v(R_M.alloc(128 * 4), 128) for _ in range(2)]
        Spo = f32v(R_M.alloc(128 * 4), 128)
        m_mark = R_M.cur
        xm = R_X.cur
        ssc = [0]
        for hgp in range(2):
            R_X.cur = xm
            hb = hgrn_bufs(R_X, NT, 9, True)
            w, wk = load_w16(d_win[g_i]); g_i += 1
            wv = w.rearrange("p (b k c) -> p b k c", b=4, k=16)
            for hl in range(4):
                for (t0, n) in TBS:
                    proj_fm(wv, wk, hl, hT, t0, n, lambda pi, hl=hl, t0=t0, n=n, hb=hb: hf_post(
                        pi, hb, hl, hgp * 4 + hl, t0, n, True, n == 16, "o"))
            w, wk = load_w16(d_win[g_i]); g_i += 1
            wv = w.rearrange("p (b k c) -> p b k c", b=4, k=16)
            for hl in range(4):
                for (t0, n) in TBS:
                    def hqpost(pi, hl=hl, t0=t0, n=n, hb=hb):
                        sl_ = tmpf("hf_sg")[:, 0:n]
                        S.op("act", lambda e: e.activation(out=sl_, in_=PS[pi][:, 0:n], func=AF.Silu), [("ps", pi)], ["hf_sg"])
                        S.op("dve", lambda e: e.tensor_tensor(out=hb["qt"][:, hl, t0:t0 + n], in0=sl_, in1=hb["eg"][:, hl, t0:t0 + n], op=ALU.mult),
                             ["hf_sg", "oeg"], ["oqt"])
                    proj_fm(wv, wk, hl, hT, t0, n, hqpost)
            w, wk = load_w16(d_win[g_i]); g_i += 1
            wv = w.rearrange("p (k c) -> p k c", k=16)
            for tl_ in range(8):
                proj_tm(wv, wk, hT, tl_ * 128, 128, lambda pi, tl_=tl_, hb=hb: S.op(
                    "act", lambda e: e.activation(out=hb["hi"][:, tl_, :], in_=PS[pi][:, :], func=AF.Copy), [("ps", pi)], ["ohi"]))
            for bi in range(4):
                proj_tm(wv, wk, hT, 1024 + bi * 4, 4, lambda pi, bi=bi, hb=hb: S.op(
                    "act", lambda e: e.activation(out=hb["his"][0:4, bi, :], in_=PS[pi][0:4, :], func=AF.Copy), [("ps", pi)], ["ohis"]))
            w, wk = load_w16(d_win[g_i]); g_i += 1
            wv = w.rearrange("p (b k c) -> p b k c", b=4, k=16)
            for hl in range(4):
                for (t0, n) in TBS:
                    proj_fm(wv, wk, hl, hT, t0, n, lambda pi, hl=hl, t0=t0, n=n, hb=hb: S.op(
                        "act", lambda e: e.activation(out=hb["gate"][:, hl, t0:t0 + n], in_=PS[pi][:, 0:n], func=AF.Silu), [("ps", pi)], ["ogate"]))
            for hl in range(4):
                hglob = hgp * 4 + hl
                state_chain(hb, hl, hglob, 0, 16, 0, Sbf, "o")
                S.op("dve", lambda e, hglob=hglob: e.tensor_copy(out=Spo, in_=Sst[:, hglob, :]), [("S", hglob)], ["Spo"])
                S.dma("sp", lambda e, hglob=hglob: e.dma_start(out=out_s[:, hglob * 128:(hglob + 1) * 128], in_=Spo), ("out", "s"), reads=["Spo"])
                for qb in range(2):
                    pa = next_ps()
                    for c in range(8):
                        j, e2 = c // 2, c % 2
                        tk = qb * 512 + c * 64
                        S.op("pe", lambda e, j=j, e2=e2, tk=tk, pa=pa: e.matmul(
                            PS[pa][64 * e2:64 * e2 + 64, j * 64:(j + 1) * 64], lhsT=hb["kt"][:, hl, tk:tk + 64],
                            rhs=hb["qt"][:, hl, tk:tk + 64], start=True, stop=True), ["okt", "oqt"], [("ps", pa)])
                    S.op("dve", lambda e, pa=pa: e.tensor_tensor(out=abf, in0=PS[pa][:, 0:256].rearrange("p (j t) -> p j t", j=4),
                                                                 in1=tri.unsqueeze(1).to_broadcast([128, 4, 64]), op=ALU.mult),
                         [("ps", pa), "cst"], ["abf"])
                    po = next_ps()
                    for c in range(8):
                        j, e2 = c // 2, c % 2
                        tk = qb * 512 + c * 64
                        tl_ = qb * 4 + j
                        S.op("pe", lambda e, c=c, tk=tk, po=po: e.matmul(
                            PS[po][:, c * 64:(c + 1) * 64], lhsT=Sbf[:, qb * 8 + c, :], rhs=hb["qt"][:, hl, tk:tk + 64], start=True, stop=False),
                            ["oSbf", "oqt"], [("ps", po)])
                        S.op("pe", lambda e, c=c, j=j, e2=e2, tl_=tl_, po=po: e.matmul(
                            PS[po][:, c * 64:(c + 1) * 64], lhsT=hb["hi"][64 * e2:64 * e2 + 64, tl_, hl * 128:(hl + 1) * 128],
                            rhs=abf[64 * e2:64 * e2 + 64, j, :], start=False, stop=True), ["ohi", "abf"], [("ps", po)])
                    subln_finish(PS[po][:, :], 512, amT[:, 8 + hglob, qb * 512:(qb + 1) * 512], ("ps", po), hgg,
                                 extra=(hb["gate"][:, hl, qb * 512:(qb + 1) * 512], "ogate"))
                pos_ = next_ps()
                for bi in range(4):
                    tk = 1024 + bi * 4
                    b = ssc[0] % 2
                    ssc[0] += 1
                    S.dma("sp", lambda e, bi=bi, hglob=hglob: e.dma_start(out=Ss, in_=d_st0[bi, hglob]), ("ld", "ss"), writes=["Ss"])
                    S.op("act", lambda e: e.activation(out=Ssb, in_=Ss, func=AF.Copy), ["Ss"], ["Ssb"])
                    pa = next_ps()
                    S.op("pe", lambda e, tk=tk, pa=pa: e.matmul(PS[pa][0:4, 0:4], lhsT=hb["kt"][:, hl, tk:tk + 4], rhs=hb["qt"][:, hl, tk:tk + 4],
                                                                start=True, stop=True), ["okt", "oqt"], [("ps", pa)])
                    S.op("dve", lambda e, pa=pa: e.tensor_tensor(out=a_s, in0=PS[pa][0:4, 0:4], in1=tri[0:4, 0:4], op=ALU.mult), [("ps", pa), "cst"], ["a_s"])
                    S.op("pe", lambda e, tk=tk, bi=bi: e.matmul(PS[pos_][:, bi * 4:bi * 4 + 4], lhsT=Ssb, rhs=hb["qt"][:, hl, tk:tk + 4],
                                                                start=True, stop=False), ["Ssb", "oqt"], [("ps", pos_)])
                    S.op("pe", lambda e, bi=bi: e.matmul(PS[pos_][:, bi * 4:bi * 4 + 4], lhsT=hb["his"][0:4, bi, hl * 128:(hl + 1) * 128], rhs=a_s,
                                                         start=False, stop=True), ["ohis", "a_s"], [("ps", pos_)])
                    S.op("pe", lambda e, bi=bi, pa=pa: e.matmul(PS[pa][:, 128:256], lhsT=hb["khs"][0:4, bi, hl * 128:(hl + 1) * 128],
                                                                rhs=hb["his"][0:4, bi, hl * 128:(hl + 1) * 128], start=True, stop=True),
                         ["okhs", "ohis"], [("ps", pa)])
                    S.op("dve", lambda e, bi=bi, pa=pa, b=b: e.scalar_tensor_tensor(
                        out=Sso[b], in0=Ss, scalar=hb["d"][:, hl, 16 + bi:17 + bi], in1=PS[pa][:, 128:256], op0=ALU.mult, op1=ALU.add),
                        [("ps", pa), "od", "Ss"], [("Sso", b)])
                    S.dma("sp", lambda e, bi=bi, hglob=hglob, b=b: e.dma_start(out=out_ss[bi, :, hglob * 128:(hglob + 1) * 128], in_=Sso[b]),
                          ("out", "ss%d" % b), reads=[("Sso", b)])
                subln_finish(PS[pos_][:, 0:16], 16, amT[:, 8 + hglob, 1024:1040], ("ps", pos_), hgg, extra=(hb["gate"][:, hl, 1024:1040], "ogate"))
            S.barrier()
        S.barrier()
        chk("p3d")
        R_M.reset()
        tmp_off.clear()
        R_X.reset()

        o_x1 = R_X.alloc(16 * NT * 4)
        DBG["x1"] = o_x1
        x1 = f32v(o_x1, 16 * NT).rearrange("p (k t) -> p k t", k=16)
        xrb = [f32v(R_M.alloc(512 * 4), 512) for _ in range(2)]
        xrc = [0]
        o4t = f32v(R_M.alloc(16 * 4), 16)
        for og in range(4):
            w, wk = load_w16(d_wout[og])
            wv = w.rearrange("p (b f c) -> p b f c", b=4, f=16)
            for cbl in range(4):
                cb = og * 4 + cbl
                for (t0, n) in TBS:
                    pi = next_ps()
                    for f in range(16):
                        S.op("pe", lambda e, f=f, pi=pi, cbl=cbl, t0=t0, n=n: e.matmul(PS[pi][:, 0:n], lhsT=wv[:, cbl, f, :], rhs=amT[:, f, t0:t0 + n],
                                                                                       start=(f == 0), stop=(f == 15)), [wk, "amT"], [("ps", pi)])
                    b = xrc[0] % 2
                    xrc[0] += 1
                    src = d_xo[:, cb, t0:t0 + n] if n == 512 else d_xs[:, cb, :]
                    S.dma("sp", lambda e, b=b, src=src, n=n: e.dma_start(out=xrb[b][:, 0:n], in_=src), ("ld", "xr%d" % b), writes=[("xr", b)])
                    if n == 512:
                        S.op("dve", lambda e, pi=pi, b=b, cb=cb, t0=t0: e.scalar_tensor_tensor(
                            out=x1[:, cb, t0:t0 + 512], in0=PS[pi][:, :], scalar=GA1[:, cb, 0:1], in1=xrb[b], op0=ALU.mult, op1=ALU.add),
                            [("ps", pi), ("xr", b), "modT"], [("x1", cb, t0)])
                    else:
                        S.op("dve", lambda e, pi=pi, cb=cb: e.tensor_tensor(
                            out=o4t.rearrange("p (b t) -> p b t", b=4), in0=PS[pi][:, 0:16].rearrange("p (b t) -> p b t", b=4),
                            in1=GA1[:, cb, 1:5].unsqueeze(2).to_broadcast([128, 4, 4]), op=ALU.mult), [("ps", pi), "modT"], ["o4t"])
                        S.op("dve", lambda e, b=b, cb=cb: e.tensor_tensor(out=x1[:, cb, 1024:1040], in0=o4t, in1=xrb[b][:, 0:16], op=ALU.add),
                             ["o4t", ("xr", b)], [("x1", cb, 1024)])
        S.barrier()
        chk("p4")
        R_M.reset()

        h2T = hT
        comb = f32v(R_M.alloc(144 * 4), 144).rearrange("p (t g e) -> p t g e", g=4, e=4)
        m5_mark = R_M.cur
        sqb = [f32v(R_M.alloc(512 * 4), 512) for _ in range(2)]
        rs2 = f32v(R_M.alloc(512 * 4), 512)
        tp2 = [f32v(R_M.alloc(512 * 4), 512) for _ in range(2)]
        h2f = [f32v(R_M.alloc(512 * 4), 512) for _ in range(2)]
        zf = f32v(R_M.alloc(180 * 4), 180)
        S.op("dve", lambda e: e.memset(zf, 0.0), [], ["zf"])
        S.op("pe", lambda e: e.matmul(PS[5][:, 0:180], lhsT=zf[:, 0:128], rhs=zf[:, 0:180], start=True, stop=False), ["zf"], [("ps", 5)])
        for bidx, (t0, n) in enumerate(TBS):
            rsel = [(0, n, 0)] if n == 512 else SAMPLE_RSEL
            for k in range(16):
                sb = k % 2
                S.op("act", lambda e, k=k, sb=sb, t0=t0, n=n: e.activation(out=sqb[sb][:, 0:n], in_=x1[:, k, t0:t0 + n], func=AF.Square), ["x1"], [("sq2", sb)])
                S.op("pe", lambda e, k=k, sb=sb, n=n: e.matmul(PS[7][:, 0:n], lhsT=ones, rhs=sqb[sb][:, 0:n], start=(k == 0), stop=(k == 15)),
                     [("sq2", sb), "cst"], [("ps", 7)])
            rstd_from_ps(PS[7][:, 0:n], n, 1.0 / 2048.0, rs2[:, 0:n], rs2[:, 0:n], [("ps", 7)], "rs2")
            for k in range(16):
                sb = k % 2
                for (c0, cn, r) in rsel:
                    S.op("dve", lambda e, k=k, sb=sb, c0=c0, cn=cn, r=r, t0=t0: e.scalar_tensor_tensor(
                        out=tp2[sb][:, c0:c0 + cn], in0=x1[:, k, t0 + c0:t0 + c0 + cn], scalar=G2[:, k, r:r + 1], in1=rs2[:, c0:c0 + cn],
                        op0=ALU.mult, op1=ALU.mult), ["x1", "G", "rs2"], [("tp2", sb)])
                    S.op("act", lambda e, k=k, sb=sb, c0=c0, cn=cn, r=r: e.activation(
                        out=h2f[sb][:, c0:c0 + cn], in_=tp2[sb][:, c0:c0 + cn], func=AF.Identity, bias=SH2[:, k, r:r + 1], scale=1.0),
                        [("tp2", sb), "modT"], [("h2f", sb)])
                S.op("dve", lambda e, k=k, sb=sb, t0=t0, n=n: e.tensor_copy(out=h2T[:, k, t0:t0 + n], in_=h2f[sb][:, 0:n]), [("h2f", sb)], ["hT"])
                ntile = max(1, n // 128)
                for j in range(ntile):
                    tl_ = t0 // 128 + j
                    mm_ = min(128, n)
                    S.op("pe", lambda e, k=k, sb=sb, j=j, tl_=tl_, mm_=mm_: e.matmul(
                        PS[5][0:mm_, tl_ * 20:tl_ * 20 + 20], lhsT=h2f[sb][:, j * 128:j * 128 + mm_], rhs=wr[:, k, :],
                        start=False, stop=(k == 15 and tl_ == 8)), [("h2f", sb), "wr"], [("ps", 5)])
        def rt(n):
            return f32v(R_M.alloc(n * 4), n)
        LG = rt(180).rearrange("p (t c) -> p t c", c=20)
        S.op("act", lambda e: e.activation(out=LG.rearrange("p t c -> p (t c)"), in_=PS[5][:, 0:180], func=AF.Copy), [("ps", 5)], ["LG"])
        lg = rt(36).rearrange("p (t g) -> p t g", g=4)
        mx = rt(9)
        oh = rt(36).rearrange("p (t g) -> p t g", g=4)
        ex = rt(36).rearrange("p (t g) -> p t g", g=4)
        den = rt(9)
        pgt = rt(9)
        le = rt(144).rearrange("p (t g e) -> p t g e", g=4, e=4)
        les = rt(36).rearrange("p (t e) -> p t e", e=4)
        m1 = rt(9)
        k1 = rt(36).rearrange("p (t e) -> p t e", e=4)
        le2 = rt(36).rearrange("p (t e) -> p t e", e=4)
        m2 = rt(9)
        k2 = rt(36).rearrange("p (t e) -> p t e", e=4)
        w1 = rt(9)
        w2 = rt(9)
        ce = rt(36).rearrange("p (t e) -> p t e", e=4)
        ce2 = rt(36).rearrange("p (t e) -> p t e", e=4)
        brg = prm[:, P_BRG:P_BRG + 4].unsqueeze(1).to_broadcast([128, 9, 4])
        bre = prm[:, P_BRE:P_BRE + 16].unsqueeze(1).to_broadcast([128, 9, 16])
        R = "rt"

        def dv(fn, rd=(), wrk=R):
            S.op("dve", fn, [R] + list(rd), [wrk])

        dv(lambda e: e.tensor_tensor(out=lg, in0=LG[:, :, 0:4], in1=brg, op=ALU.add), ["LG", "prm"])
        dv(lambda e: e.tensor_reduce(out=mx, in_=lg, axis=AX.X, op=ALU.max))
        dv(lambda e: e.tensor_tensor(out=oh, in0=lg, in1=mx.unsqueeze(2).to_broadcast([128, 9, 4]), op=ALU.is_equal))
        dv(lambda e: e.tensor_tensor(out=ex, in0=lg, in1=mx.unsqueeze(2).to_broadcast([128, 9, 4]), op=ALU.subtract))
        S.op("act", lambda e: e.activation(out=ex, in_=ex, func=AF.Exp), [R], [R])
        dv(lambda e: e.tensor_reduce(out=den, in_=ex, axis=AX.X, op=ALU.add))
        dv(lambda e: e.reciprocal(out=pgt, in_=den))
        dv(lambda e: e.tensor_tensor(out=le.rearrange("p t g e -> p t (g e)"), in0=LG[:, :, 4:20], in1=bre, op=ALU.add), ["LG", "prm"])
        dv(lambda e: e.tensor_tensor(out=le, in0=le, in1=oh.unsqueeze(3).to_broadcast([128, 9, 4, 4]), op=ALU.mult))
        dv(lambda e: e.tensor_reduce(out=les, in_=le.rearrange("p t g e -> p t e g"), axis=AX.X, op=ALU.add))
        dv(lambda e: e.tensor_reduce(out=m1, in_=les, axis=AX.X, op=ALU.max))
        dv(lambda e: e.tensor_tensor(out=k1, in0=les, in1=m1.unsqueeze(2).to_broadcast([128, 9, 4]), op=ALU.is_equal))
        dv(lambda e: e.scalar_tensor_tensor(out=le2, in0=k1, scalar=-1.0e30, in1=les, op0=ALU.mult, op1=ALU.add))
        dv(lambda e: e.tensor_reduce(out=m2, in_=le2, axis=AX.X, op=ALU.max))
        dv(lambda e: e.tensor_tensor(out=k2, in0=le2, in1=m2.unsqueeze(2).to_broadcast([128, 9, 4]), op=ALU.is_equal))
        dv(lambda e: e.tensor_tensor(out=w2, in0=m2, in1=m1, op=ALU.subtract))
        S.op("act", lambda e: e.activation(out=w2, in_=w2, func=AF.Exp), [R], [R])
        dv(lambda e: e.tensor_scalar(out=w1, in0=w2, scalar1=1.0, scalar2=None, op0=ALU.add))
        dv(lambda e: e.reciprocal(out=w1, in_=w1))
        dv(lambda e: e.tensor_tensor(out=w2, in0=w2, in1=w1, op=ALU.mult))
        dv(lambda e: e.tensor_tensor(out=w1, in0=w1, in1=pgt, op=ALU.mult))
        dv(lambda e: e.tensor_tensor(out=w2, in0=w2, in1=pgt, op=ALU.mult))
        dv(lambda e: e.tensor_tensor(out=ce, in0=k1, in1=w1.unsqueeze(2).to_broadcast([128, 9, 4]), op=ALU.mult))
        dv(lambda e: e.tensor_tensor(out=ce2, in0=k2, in1=w2.unsqueeze(2).to_broadcast([128, 9, 4]), op=ALU.mult))
        dv(lambda e: e.tensor_tensor(out=ce, in0=ce, in1=ce2, op=ALU.add))
        dv(lambda e: e.tensor_tensor(out=comb, in0=oh.unsqueeze(3).to_broadcast([128, 9, 4, 4]),
                                     in1=ce.unsqueeze(2).to_broadcast([128, 9, 4, 4]), op=ALU.mult), wrk="comb")
        S.barrier()
        chk("p5")

        R_M.cur = m5_mark
        R_RING.reset()
        mslot = [R_RING.alloc(4096) for _ in range(8)]
        msl = [0]

        def load_w4(src_ap):
            slot = msl[0] % 8
            msl[0] += 1
            v = bfv(mslot[slot], 2048)
            key = ("mring", slot)
            S.dma("pool", lambda e: e.dma_start(out=v, in_=src_ap), ("mring", slot), writes=[key])
            return v, key

        o_hid = R_AM.base
        hid = bfv(o_hid, 4 * NT).rearrange("p (f t) -> p f t", f=4)
        cbc = [f32v(R_AM.base + 4 * NT * 2 + i * NT * 4, NT) for i in range(2)]
        De = [f32v(R_M.alloc(128 * 4), 128) for _ in range(2)]
        msA = [f32v(R_M.alloc(512 * 4), 512) for _ in range(2)]
        mtT = [f32v(R_M.alloc(512 * 4), 512) for _ in range(2)]
        mo4 = f32v(R_M.alloc(16 * 4), 16)
        dec = [0]
        mc = [0]
        def build_cbc(ex_):
            g_, e_ = ex_ // 4, ex_ % 4
            cb_ = cbc[ex_ % 2]
            for bidx, (t0, n) in enumerate(TBS):
                pi = next_ps()
                ntile = max(1, n // 128)
                for j in range(ntile):
                    tl_ = t0 // 128 + j
                    mm_ = min(128, n)
                    b = dec[0] % 2
                    dec[0] += 1
                    S.op("dve", lambda e, b=b, tl_=tl_, mm_=mm_: e.tensor_scalar(
                        out=De[b][0:mm_, 0:mm_], in0=ident[0:mm_, 0:mm_], scalar1=comb[0:mm_, tl_, g_, e_:e_ + 1], scalar2=None, op0=ALU.mult),
                        ["comb", "cst"], [("De", b)])
                    S.op("pe", lambda e, b=b, j=j, mm_=mm_, pi=pi: e.matmul(PS[pi][:, j * 128:j * 128 + mm_], lhsT=ones[0:mm_, :], rhs=De[b][0:mm_, 0:mm_],
                                                                            start=True, stop=True), [("De", b), "cst"], [("ps", pi)])
                S.op("act", lambda e, pi=pi, t0=t0, n=n: e.activation(out=cb_[:, t0:t0 + n], in_=PS[pi][:, 0:n], func=AF.Copy), [("ps", pi)], [("cbc", ex_ % 2)])

        build_cbc(0)
        for ex_ in range(16):
            g_, e_ = ex_ // 4, ex_ % 4
            cb_ = cbc[ex_ % 2]
            for fb in range(4):
                wg, wgk = load_w4(d_wgu[ex_, fb * 2])
                wu, wuk = load_w4(d_wgu[ex_, fb * 2 + 1])
                wgv = wg.rearrange("p (k c) -> p k c", k=16)
                wuv = wu.rearrange("p (k c) -> p k c", k=16)
                for (t0, n) in TBS:
                    pa, pu = next_ps(), next_ps()
                    for k in range(16):
                        S.op("pe", lambda e, k=k, pa=pa, t0=t0, n=n: e.matmul(PS[pa][:, 0:n], lhsT=wgv[:, k, :], rhs=h2T[:, k, t0:t0 + n],
                                                                              start=(k == 0), stop=(k == 15)), [wgk, "hT"], [("ps", pa)])
                    for k in range(16):
                        S.op("pe", lambda e, k=k, pu=pu, t0=t0, n=n: e.matmul(PS[pu][:, 0:n], lhsT=wuv[:, k, :], rhs=h2T[:, k, t0:t0 + n],
                                                                              start=(k == 0), stop=(k == 15)), [wuk, "hT"], [("ps", pu)])
                    b = mc[0] % 2
                    mc[0] += 1
                    S.op("act", lambda e, pa=pa, b=b, n=n: e.activation(out=msA[b][:, 0:n], in_=PS[pa][:, 0:n], func=AF.Silu), [("ps", pa)], [("msA", b)])
                    S.op("dve", lambda e, pu=pu, b=b, t0=t0, n=n: e.tensor_tensor(out=mtT[b][:, 0:n], in0=PS[pu][:, 0:n], in1=cb_[:, t0:t0 + n], op=ALU.mult),
                         [("ps", pu), ("cbc", ex_ % 2)], [("mtT", b)])
                    S.op("dve", lambda e, b=b, fb=fb, t0=t0, n=n: e.tensor_tensor(out=hid[:, fb, t0:t0 + n], in0=msA[b][:, 0:n], in1=mtT[b][:, 0:n], op=ALU.mult),
                         [("msA", b), ("mtT", b)], ["hid"])
            if ex_ + 1 < 16:
                build_cbc(ex_ + 1)
            for cg in range(4):
                wd, wdk = load_w4(d_wdn[ex_, cg])
                wdv = wd.rearrange("p (c f o) -> p c f o", c=4, f=4)
                for cbl in range(4):
                    cb = cg * 4 + cbl
                    for (t0, n) in TBS:
                        pi = next_ps()
                        for fb in range(4):
                            S.op("pe", lambda e, fb=fb, pi=pi, cbl=cbl, t0=t0, n=n: e.matmul(PS[pi][:, 0:n], lhsT=wdv[:, cbl, fb, :], rhs=hid[:, fb, t0:t0 + n],
                                                                                             start=(fb == 0), stop=(fb == 3)), [wdk, "hid"], [("ps", pi)])
                        if n == 512:
                            S.op("dve", lambda e, pi=pi, cb=cb, t0=t0: e.scalar_tensor_tensor(
                                out=x1[:, cb, t0:t0 + 512], in0=PS[pi][:, :], scalar=GA2[:, cb, 0:1], in1=x1[:, cb, t0:t0 + 512], op0=ALU.mult, op1=ALU.add),
                                [("ps", pi), "modT", ("x1", cb, t0)], [("x1", cb, t0)])
                        else:
                            S.op("dve", lambda e, pi=pi, cb=cb: e.tensor_tensor(
                                out=mo4.rearrange("p (b t) -> p b t", b=4), in0=PS[pi][:, 0:16].rearrange("p (b t) -> p b t", b=4),
                                in1=GA2[:, cb, 1:5].unsqueeze(2).to_broadcast([128, 4, 4]), op=ALU.mult), [("ps", pi), "modT"], ["mo4"])
                            S.op("dve", lambda e, cb=cb: e.tensor_tensor(out=x1[:, cb, 1024:1040], in0=mo4, in1=x1[:, cb, 1024:1040], op=ALU.add),
                                 ["mo4", ("x1", cb, 1024)], [("x1", cb, 1024)])
        S.barrier()
        S.dma("sp", lambda e: e.dma_start(out=out_yT, in_=x1.rearrange("p k t -> p (k t)")), ("out", "y"), reads=["x1"])

    except _Stop:
        pass
    if stop is not None:
        S.barrier()
        d_dbg = dout("dbg", [128, ARENA // 4])
        S.dma("sp", lambda e: e.dma_start(out=d_dbg, in_=A[:, :]), ("out", "dbg"))
    S.emit(nc, es)
    es.close()
    return nc


def _fm(a):
    T = a.shape[0]
    return np.ascontiguousarray(a.reshape(T, 16, 128).transpose(2, 1, 0))


def _wblocks_fm(w, cols):
    out = np.empty((128, 4, 16, 128), np.float32)
    for b, c0 in enumerate(cols):
        out[:, b] = w[:, c0:c0 + 128].reshape(16, 128, 128).transpose(1, 0, 2)
    return out.reshape(128, 8192)


def _wgroup_tm(w, c0):
    return np.ascontiguousarray(w[:, c0:c0 + 512].reshape(16, 128, 512).transpose(1, 0, 2)).reshape(128, 8192)


_NC_CACHE = {}


def prep(x_prompt, x_sample, cache_k, cache_v, state_hgrn, page_table, c_prompt, c_sample,
           norm1_g, norm2_g, w_ada, b_ada, w_in, q_norm_g, k_norm_g,
           lambda_q1, lambda_k1, lambda_q2, lambda_k2, subln_g, hg_lower_bound, hg_norm_g, w_out,
           w_router_group, b_router_group, w_router_expert, b_router_expert,
           w_exp_gate, w_exp_up, w_exp_down, small_cache=False):
    f = np.float32
    x_prompt = np.asarray(x_prompt, f); x_sample = np.asarray(x_sample, f)
    cache_k = np.asarray(cache_k, f); cache_v = np.asarray(cache_v, f)
    w_ada = np.asarray(w_ada, f)[0]; w_in = np.asarray(w_in, f)[0]; w_out = np.asarray(w_out, f)[0]
    wg_ = np.asarray(w_exp_gate, f)[0]; wu_ = np.asarray(w_exp_up, f)[0]; wd_ = np.asarray(w_exp_down, f)[0]

    wada = np.stack([_wblocks_fm(w_ada, [(g * 4 + b) * 128 for b in range(4)]) for g in range(24)])
    QC, KC, VC, HQ, HF, HI, HG = 0, 1024, 1536, 2048, 3072, 4096, 5120
    groups = []
    groups.append(_wblocks_fm(w_in, [KC + 128 * b for b in range(4)]))
    groups.append(_wgroup_tm(w_in, VC))
    for hgp in range(2):
        groups.append(_wblocks_fm(w_in, [HF + hgp * 512 + 128 * b for b in range(4)]))
        groups.append(_wgroup_tm(w_in, HI + hgp * 512))
    for qg in range(2):
        groups.append(_wblocks_fm(w_in, [QC + qg * 512 + 128 * b for b in range(4)]))
    groups.append(_wblocks_fm(w_in, [KC + 128 * b for b in range(4)]))
    groups.append(_wgroup_tm(w_in, VC))
    for hgp in range(2):
        groups.append(_wblocks_fm(w_in, [HF + hgp * 512 + 128 * b for b in range(4)]))
        groups.append(_wblocks_fm(w_in, [HQ + hgp * 512 + 128 * b for b in range(4)]))
        groups.append(_wgroup_tm(w_in, HI + hgp * 512))
        groups.append(_wblocks_fm(w_in, [HG + hgp * 512 + 128 * b for b in range(4)]))
    win = np.stack(groups)
    wout = np.stack([_wblocks_fm(w_out, [(g * 4 + b) * 128 for b in range(4)]) for g in range(4)])
    wgu = np.empty((16, 8, 128, 2048), f)
    for e in range(16):
        for fb in range(4):
            wgu[e, fb * 2] = wg_[e][:, fb * 128:(fb + 1) * 128].reshape(16, 128, 128).transpose(1, 0, 2).reshape(128, 2048)
            wgu[e, fb * 2 + 1] = wu_[e][:, fb * 128:(fb + 1) * 128].reshape(16, 128, 128).transpose(1, 0, 2).reshape(128, 2048)
    wdn = np.ascontiguousarray(wd_.reshape(16, 4, 128, 4, 4, 128).transpose(0, 3, 2, 4, 1, 5)).reshape(16, 4, 128, 2048)
    if small_cache:
        ckv = np.zeros((128, 1024), f)
    else:
        ckv = np.empty((5120 * 128, 1024), f)
        ckv[:, 0:512] = cache_k[0].transpose(0, 3, 2, 1).reshape(5120 * 128, 512)
        ckv[:, 512:1024] = cache_v[0].reshape(5120 * 128, 512)
    posT, R3, newb = make_sample_tables()
    wr = np.concatenate([np.asarray(w_router_group, f)[0], np.asarray(w_router_expert, f)[0].transpose(1, 0, 2).reshape(2048, 16)], axis=1)
    wr = np.ascontiguousarray(wr.reshape(16, 128, 20).transpose(1, 0, 2)).reshape(128, 320)

    prm0 = np.zeros((128, NPRM), f)
    prm0[:, P_N1:P_N1 + 16] = np.asarray(norm1_g, f)[0].reshape(16, 128).T
    prm0[:, P_N2:P_N2 + 16] = np.asarray(norm2_g, f)[0].reshape(16, 128).T
    prm0[:, P_BT:P_BT + 96] = np.asarray(b_ada, f)[0].reshape(96, 128).T
    prm0[:, P_GQ] = np.tile(np.asarray(q_norm_g, f)[0], 2)
    prm0[:, P_GK] = np.tile(np.asarray(k_norm_g, f)[0], 2)
    prm0[:, P_SG] = np.asarray(subln_g, f)[0]
    prm0[:, P_HGG] = np.asarray(hg_norm_g, f)[0]
    lbv = np.asarray(hg_lower_bound, f)
    prm0[:, P_LB:P_LB + 8] = lbv[0].reshape(8, 128).T
    prm0[:, P_LB + 8:P_LB + 16] = lbv[1].reshape(8, 128).T
    prm0[:, P_GQR:P_GQR + 64] = np.asarray(q_norm_g, f)[0][None]
    prm0[:, P_GKR:P_GKR + 64] = np.asarray(k_norm_g, f)[0][None]
    prm0[:, P_LAMR:P_LAMR + 256] = np.concatenate([np.asarray(a, f)[0] for a in (lambda_q1, lambda_k1, lambda_q2, lambda_k2)])[None]
    prm0[:, P_BRG:P_BRG + 4] = np.asarray(b_router_group, f)[0][None]
    prm0[:, P_BRE:P_BRE + 16] = np.asarray(b_router_expert, f)[0].reshape(16)[None]

    in_maps = []
    pt = np.asarray(page_table, np.int32)
    for c in range(8):
        b, half = c // 2, c % 2
        prm = prm0.copy()
        prm[:, P_FLAG] = float(half)
        crow = np.concatenate([np.asarray(c_prompt, f)[b:b + 1], np.asarray(c_sample, f)[4 * c:4 * c + 4]], axis=0)
        cT = np.ascontiguousarray(crow.reshape(5, 16, 128).transpose(2, 1, 0)).reshape(128, 80)
        in_maps.append({
            "cst": make_consts(half), "prm": prm, "cT": cT, "wr": wr,
            "xo": _fm(x_prompt[b, half * 1024:(half + 1) * 1024]),
            "xp": _fm(x_prompt[b, 0:1024]),
            "xs": _fm(x_sample[4 * c:4 * c + 4].reshape(16, 2048)),
            "wada": wada, "win": win, "wout": wout, "wgu": wgu, "wdn": wdn,
            "ckv": ckv, "pt": np.ascontiguousarray(pt[4 * c:4 * c + 4]),
            "st0": np.ascontiguousarray(np.asarray(state_hgrn, f)[0, 4 * c:4 * c + 4]),
            "posT": posT, "R3": R3, "newb": newb,
        })
    return in_maps


def kernel(**inputs):
    f = np.float32
    in_maps = prep(**inputs)
    if "nc" not in _NC_CACHE:
        _NC_CACHE["nc"] = build()
    res = run_bass_kernel_spmd(_NC_CACHE["nc"], in_maps, core_ids=list(range(8)))
    R = res.results

    y_prompt = np.empty((4, 2048, 2048), f); y_sample = np.empty((32, 4, 2048), f)
    nk_p = np.empty((1, 4, 2048, 4, 128), f); nv_p = np.empty((1, 4, 2048, 4, 128), f)
    nk_s = np.empty((1, 32, 4, 4, 128), f); nv_s = np.empty((1, 32, 4, 4, 128), f)
    ns_p = np.empty((1, 4, 8, 128, 128), f); ns_s = np.empty((1, 32, 8, 128, 128), f)
    for c in range(8):
        b, half = c // 2, c % 2
        yT = R[c]["yT"].reshape(128, 16, NT)
        yt = yT.transpose(2, 1, 0).reshape(NT, 2048)
        y_prompt[b, half * 1024:(half + 1) * 1024] = yt[:1024]
        y_sample[4 * c:4 * c + 4] = yt[1024:].reshape(4, 4, 2048)
        kTo = R[c]["kTo"].reshape(128, 4, NT).transpose(2, 1, 0)
        nk_p[0, b, half * 1024:(half + 1) * 1024] = kTo[:1024]
        nk_s[0, 4 * c:4 * c + 4] = kTo[1024:].reshape(4, 4, 4, 128)
        vo = R[c]["vo"].reshape(NT, 4, 128)
        nv_p[0, b, half * 1024:(half + 1) * 1024] = vo[:1024]
        nv_s[0, 4 * c:4 * c + 4] = vo[1024:].reshape(4, 4, 4, 128)
        if half == 1:
            ns_p[0, b] = R[c]["so"].reshape(128, 8, 128).transpose(1, 0, 2)
        ns_s[0, 4 * c:4 * c + 4] = R[c]["sso"].reshape(4, 128, 8, 128).transpose(0, 2, 1, 3)
    return (y_prompt, y_sample, nk_p, nv_p, nk_s, nv_s, ns_p, ns_s)
```

```python
import math
from contextlib import ExitStack
import numpy as np
import concourse.bass as bass
import concourse.mybir as mybir
from concourse.bass_utils import run_bass_kernel_spmd

F32 = mybir.dt.float32
BF16 = mybir.dt.bfloat16
I32 = mybir.dt.int32
AF = mybir.ActivationFunctionType
ALU = mybir.AluOpType
AX = mybir.AxisListType

NT = 1040
EPS = 1e-6
LAM_INIT = 0.2
SLOPES = [2.0 ** (-(h + 1)) for h in range(8)]
PAST = 16384
ENG = ["pe", "act", "dve", "pool", "sp"]
STOP_AFTER = None
DBG = {}


class _Rec:
    def __init__(self):
        self.call = None

    def __getattr__(self, name):
        def f(*a, **kw):
            self.call = (name, a, kw)
            return None
        return f


def _bind(fn):
    if fn is None:
        return None
    r = _Rec()
    fn(r)
    name, a, kw = r.call
    return lambda e: getattr(e, name)(*a, **kw)


class Sched:
    def __init__(self):
        self.ops = {e: [] for e in ENG}
        self.state = {}
        self.wE = {e: {} for e in ENG}
        self.wD = {e: {} for e in ENG}
        self.dcount = {}
        self.sig = {e: set() for e in ENG}
        self.last_real = {}

    def _deps(self, reads, writes):
        deps = []
        for k in reads:
            st = self.state.get(k)
            if st and st[0] is not None:
                deps.append((st[0], True))
            if st and isinstance(k, tuple) and k[0] == "ps":
                deps.extend((t, False) for t in st[1].values())
        for k in writes:
            st = self.state.get(k)
            if st:
                if st[0] is not None:
                    deps.append((st[0], False))
                deps.extend((t, False) for t in st[1].values())
        return deps

    def _update(self, reads, writes, tok):
        key = (tok[0], tok[1])
        for k in reads:
            st = self.state.setdefault(k, [None, {}])
            st[1][key] = tok
        for k in writes:
            self.state[k] = [tok, {}]

    def _waits(self, eng, deps):
        waits = []
        for (t, raw) in deps:
            if t[0] == "E":
                _, f, i = t
                if f == eng and (eng == "pe" or not raw):
                    continue
                if self.wE[eng].get(f, -1) >= i:
                    continue
                self.wE[eng][f] = i
                self.sig[f].add(i)
                waits.append(t)
            else:
                _, k, v = t
                if self.wD[eng].get(k, 0) >= v:
                    continue
                self.wD[eng][k] = v
                waits.append(t)
        return waits

    def op(self, eng, fn, reads=(), writes=()):
        waits = self._waits(eng, self._deps(reads, writes))
        idx = len(self.ops[eng])
        self.ops[eng].append((waits, _bind(fn), None))
        self.last_real[eng] = idx
        tok = ("E", eng, idx)
        self._update(reads, writes, tok)
        return tok

    def dma(self, q, fn, semkey, reads=(), writes=()):
        waits = self._waits(q, self._deps(reads, writes))
        v = self.dcount.get(semkey, 0) + 16
        self.dcount[semkey] = v
        self.ops[q].append((waits, _bind(fn), (semkey, v)))
        tok = ("D", semkey, v)
        self._update(reads, writes, tok)
        return tok

    def barrier(self):
        toks = [("E", e, self.last_real[e]) for e in ENG if e in self.last_real]
        for e in ENG:
            waits = []
            for t in toks:
                if t[1] != e and self.wE[e].get(t[1], -1) < t[2]:
                    self.wE[e][t[1]] = t[2]
                    self.sig[t[1]].add(t[2])
                    waits.append(t)
            for k, v in self.dcount.items():
                if k[0] == "out" and self.wD[e].get(k, 0) < v:
                    self.wD[e][k] = v
                    waits.append(("D", k, v))
            if waits:
                self.ops[e].append((waits, None, None))

    def emit(self, nc, es):
        esem = {e: es.enter_context(nc.semaphore("sem_" + e)) for e in ENG}
        dsem = {k: es.enter_context(nc.semaphore("dsem%d" % i)) for i, k in enumerate(self.dcount)}
        rank = {}
        for e in ENG:
            r = 0
            rank[e] = {}
            for i in range(len(self.ops[e])):
                if i in self.sig[e]:
                    r += 1
                    rank[e][i] = r
        block = es.enter_context(nc.Block())

        def run(e, eng):
            for i, (waits, fn, dm) in enumerate(self.ops[e]):
                for t in waits:
                    if t[0] == "E":
                        eng.wait_ge(esem[t[1]], rank[t[1]][t[2]])
                    else:
                        eng.wait_ge(dsem[t[1]], t[2])
                if fn is None:
                    continue
                ins = fn(eng)
                if dm is not None:
                    ins.then_inc(dsem[dm[0]], 16)
                elif i in self.sig[e]:
                    ins.then_inc(esem[e], 1)
            if e in ("sp", "pool", "act"):
                for k, v in self.dcount.items():
                    if k[0] == "out":
                        eng.wait_ge(dsem[k], v)

        @block.tensor
        def _(eng):
            run("pe", eng)

        @block.scalar
        def _(eng):
            run("act", eng)

        @block.vector
        def _(eng):
            run("dve", eng)

        @block.gpsimd
        def _(eng):
            run("pool", eng)

        @block.sync
        def _(eng):
            run("sp", eng)


C_ID, C_ONES, C_BONES, C_TRI, C_SCAN, C_SCANS, C_ND, C_BIASC = 0, 128, 256, 384, 448, 960, 976, 2000
NCST = 2000 + 256
ND_D0 = [0, -128, -256, -384]


def _tile_list(qb):
    return list(range(12)) if qb == 0 else list(range(16))


def make_consts(half):
    c = np.zeros((128, NCST), np.float32)
    p = np.arange(128)
    c[:, C_ID:C_ID + 128] = np.eye(128)
    c[:, C_ONES:C_ONES + 128] = 1.0
    c[:, C_BONES:C_BONES + 128] = (p[:, None] // 64 == p[None, :] // 64)
    c[:, C_TRI:C_TRI + 64] = ((p[:, None] % 64) <= np.arange(64)[None, :])
    sm = np.ones(512, np.float32)
    sm[::64] = 0.0
    c[:, C_SCAN:C_SCAN + 512] = sm[None]
    sms = np.ones(16, np.float32)
    sms[::4] = 0.0
    c[:, C_SCANS:C_SCANS + 16] = sms[None]
    u = np.arange(1024)
    dist = u[None, :] - 384 - p[:, None]
    c[:, C_ND:C_ND + 1024] = np.where(dist >= 0, -dist, -1.0e6)
    for h in range(8):
        for qb in range(2):
            for kt in range(16):
                d0 = 1024 + 512 * qb - 128 * kt
                val = 0.0 if d0 < 128 else -SLOPES[h] * (d0 - 128)
                if kt < 8 and half == 0:
                    val += -30000.0
                c[:, C_BIASC + (h * 2 + qb) * 16 + kt] = val
    return c


def make_sample_tables():
    posT = np.zeros((3, 16, 128), np.float32)
    posT[0] = np.arange(128)[None, :]
    posT[1] = 1.0
    posT[2] = np.arange(16)[:, None]
    R3 = np.zeros((3, 8, 64), np.float32)
    newb = np.zeros((4, 64), np.float32)
    for kvh in range(4):
        for m in range(2):
            for g in range(2):
                for t in range(4):
                    col = kvh * 16 + m * 8 + g * 4 + t
                    sl = SLOPES[kvh * 2 + g]
                    R3[0, :, col] = sl
                    R3[1, :, col] = -sl * (PAST + t - 128.0 * np.arange(8))
                    R3[2, :, col] = sl * 1024.0
                    for tp in range(4):
                        newb[tp, col] = -sl * (t - tp) if tp <= t else -1.0e6
    return posT.reshape(3, 2048), R3.reshape(3, 512), newb


P_N1, P_N2, P_BT, P_GQ, P_GK, P_SG, P_HGG, P_LB, P_FLAG = 0, 16, 32, 128, 129, 130, 131, 132, 148
P_GQR, P_GKR, P_LAMR, P_BRG, P_BRE = 149, 213, 277, 533, 537
NPRM = 553


def build(stop=None, small_cache=False):
    nc = bass.Bass("TRN2", target_bir_lowering=False)
    S = Sched()
    es = ExitStack()

    def din(name, shape, dt=F32):
        return nc.dram_tensor(name, list(shape), dt, kind="ExternalInput").ap()

    def dout(name, shape, dt=F32):
        return nc.dram_tensor(name, list(shape), dt, kind="ExternalOutput").ap()

    d_cst = din("cst", [128, NCST])
    d_prm = din("prm", [128, NPRM])
    d_cT = din("cT", [128, 80])
    d_wr = din("wr", [128, 320])
    d_xo = din("xo", [128, 16, 1024])
    d_xp = din("xp", [128, 16, 1024])
    d_xs = din("xs", [128, 16, 16])
    d_wada = din("wada", [24, 128, 8192])
    d_win = din("win", [18, 128, 8192])
    d_wout = din("wout", [4, 128, 8192])
    d_wgu = din("wgu", [16, 8, 128, 2048])
    d_wdn = din("wdn", [16, 4, 128, 2048])
    d_ckv = din("ckv", [128 if small_cache else 655360, 1024])
    d_pt = din("pt", [4, 128], I32)
    d_st0 = din("st0", [4, 8, 128, 128])
    d_posT = din("posT", [3, 2048])
    d_R3 = din("R3", [3, 512])
    d_newb = din("newb", [4, 64])
    out_yT = dout("yT", [128, 16 * NT])
    out_kT = dout("kTo", [128, 4 * NT])
    out_v = dout("vo", [NT, 512])
    out_s = dout("so", [128, 1024])
    out_ss = dout("sso", [4, 128, 1024])

    ARENA = 207 * 1024
    A = es.enter_context(nc.sbuf_tensor("arena", [128, ARENA // 4], F32))
    PS = [es.enter_context(nc.psum_tensor("ps%d" % i, [128, 512], F32)) for i in range(8)]

    def f32v(off, n, parts=128):
        assert off % 4 == 0
        return A[0:parts, off // 4: off // 4 + n]

    def bfv(off, n, parts=128):
        assert off % 4 == 0 and n % 2 == 0
        return A[0:parts, off // 4: off // 4 + n // 2].bitcast(BF16)

    def i32v(off, n, parts=128):
        return A[0:parts, off // 4: off // 4 + n].bitcast(I32)

    class Region:
        def __init__(self, base, size):
            self.base, self.size, self.cur = base, size, base

        def alloc(self, nbytes):
            nbytes = (nbytes + 63) // 64 * 64
            off = self.cur
            self.cur += nbytes
            assert self.cur <= self.base + self.size, ("region overflow", self.cur - self.base, self.size)
            return off

        def reset(self):
            self.cur = self.base

    R_CONST = Region(0, 22 * 1024)
    R_RING = Region(R_CONST.base + R_CONST.size, 32 * 1024)
    R_H = Region(R_RING.base + R_RING.size, 33280)
    R_AM = Region(R_H.base + R_H.size, 33280)
    R_X = Region(R_AM.base + R_AM.size, 66560)
    R_M = Region(R_X.base + R_X.size, ARENA - (R_X.base + R_X.size))

    class _Stop(Exception):
        pass

    def chk(name):
        if stop == name:
            raise _Stop()

    try:
        o_cst = R_CONST.alloc(NCST * 4)
        cst = f32v(o_cst, NCST)
        ident = cst[:, C_ID:C_ID + 128]
        ones = cst[:, C_ONES:C_ONES + 128]
        bones = cst[:, C_BONES:C_BONES + 128]
        tri = cst[:, C_TRI:C_TRI + 64]
        scanm = cst[:, C_SCAN:C_SCAN + 512]
        scanms = cst[:, C_SCANS:C_SCANS + 16]
        biasc = cst[:, C_BIASC:C_BIASC + 256]
        o_prm = R_CONST.alloc(NPRM * 4)
        prm = f32v(o_prm, NPRM)
        o_bfc = R_CONST.alloc(768 * 2)
        identb = bfv(o_bfc, 768)[:, 0:128]
        onesb = bfv(o_bfc, 768)[:, 128:256]
        zerosb = bfv(o_bfc, 768)[:, 256:768]
        o_mod = R_CONST.alloc(480 * 4)
        DBG["mod"] = o_mod
        modT = f32v(o_mod, 480).rearrange("p (c r) -> p c r", r=5)
        o_g = R_CONST.alloc(160 * 4)
        DBG["g"] = o_g
        G1 = f32v(o_g, 160)[:, 0:80].rearrange("p (c r) -> p c r", r=5)
        G2 = f32v(o_g, 160)[:, 80:160].rearrange("p (c r) -> p c r", r=5)
        o_sc = R_CONST.alloc(64 * 4)
        DBG["sc"] = o_sc
        sc = f32v(o_sc, 64)
        negM, neglam, gq8, sg8, oml, noml, mq, mk = (sc[:, i:i + 1] for i in range(8))
        omlh = sc[:, 8:16]
        nomlh = sc[:, 16:24]
        lbh = sc[:, 24:32]
        epsc = sc[:, 32:33]
        o_wr = R_CONST.alloc(320 * 4)
        wr = f32v(o_wr, 320).rearrange("p (k c) -> p k c", c=20)
        o_cT = R_CONST.alloc(80 * 4)
        cTt = f32v(o_cT, 80)
        o_sil = R_CONST.alloc(80 * 2)
        silT = bfv(o_sil, 80).rearrange("p (k r) -> p k r", r=5)

        S.dma("sp", lambda e: e.dma_start(out=cst, in_=d_cst), ("ld", "c0"), writes=["cst"])
        S.dma("sp", lambda e: e.dma_start(out=prm, in_=d_prm), ("ld", "c1"), writes=["prm"])
        S.dma("sp", lambda e: e.dma_start(out=cTt, in_=d_cT), ("ld", "c2"), writes=["cT"])
        S.dma("sp", lambda e: e.dma_start(out=wr.rearrange("p k c -> p (k c)"), in_=d_wr), ("ld", "c3"), writes=["wr"])

        S.op("dve", lambda e: e.tensor_copy(out=identb, in_=ident), ["cst"], ["bfc"])
        S.op("dve", lambda e: e.tensor_copy(out=onesb, in_=ones), ["cst"], ["bfc"])
        S.op("dve", lambda e: e.memset(zerosb, 0.0), [], ["bfc"])
        S.op("dve", lambda e: e.memset(epsc, EPS), [], ["sc"])
        o_gt = R_M.alloc(128 * 4)
        gt = f32v(o_gt, 128)
        S.op("dve", lambda e: e.tensor_tensor(out=gt, in0=prm[:, P_GQR:P_GQR + 128], in1=prm[:, P_GQR:P_GQR + 128], op=ALU.mult), ["prm"], ["gt"])
        S.op("dve", lambda e: e.tensor_reduce(out=sc[:, 6:8], in_=gt.rearrange("p (a b) -> p a b", b=64), axis=AX.X, op=ALU.max), ["gt"], ["sc"])
        S.op("dve", lambda e: e.tensor_tensor(out=negM, in0=mq, in1=mk, op=ALU.mult), ["sc"], ["sc"])
        S.op("dve", lambda e: e.tensor_scalar(out=negM, in0=negM, scalar1=1.0, scalar2=-8.0, op0=ALU.max, op1=ALU.mult), ["sc"], ["sc"])
        S.op("dve", lambda e: e.tensor_scalar(out=gq8, in0=prm[:, P_GQ:P_GQ + 1], scalar1=0.125, scalar2=None, op0=ALU.mult), ["prm"], ["sc"])
        S.op("dve", lambda e: e.tensor_scalar(out=sg8, in0=prm[:, P_SG:P_SG + 1], scalar1=1.0 - LAM_INIT, scalar2=None, op0=ALU.mult), ["prm"], ["sc"])
        o_t = R_M.alloc(256 * 4)
        tl = f32v(o_t, 256)
        lr = prm[:, P_LAMR:P_LAMR + 256]
        S.op("dve", lambda e: e.tensor_tensor(out=tl[:, 0:64], in0=lr[:, 0:64], in1=lr[:, 64:128], op=ALU.mult), ["prm"], ["tl"])
        S.op("dve", lambda e: e.tensor_tensor(out=tl[:, 64:128], in0=lr[:, 128:192], in1=lr[:, 192:256], op=ALU.mult), ["prm"], ["tl"])
        S.op("dve", lambda e: e.tensor_reduce(out=tl[:, 128:130], in_=tl[:, 0:128].rearrange("p (a b) -> p a b", b=64), axis=AX.X, op=ALU.add), ["tl"], ["tl2"])
        S.op("act", lambda e: e.activation(out=tl[:, 130:132], in_=tl[:, 128:130], func=AF.Exp), ["tl2"], ["tl3"])
        S.op("dve", lambda e: e.scalar_tensor_tensor(out=neglam, in0=tl[:, 131:132], scalar=-LAM_INIT, in1=tl[:, 130:131], op0=ALU.add, op1=ALU.subtract), ["tl3"], ["sc"])
        lb2 = prm[:, P_LB:P_LB + 16].rearrange("p (s h) -> p s h", s=2)
        S.op("dve", lambda e: e.tensor_tensor(out=tl[:, 132:140], in0=lb2[:, 0, :], in1=lb2[:, 1, :], op=ALU.subtract), ["prm"], ["tl4"])
        S.op("act", lambda e: e.activation(out=lbh, in_=tl[:, 132:140], func=AF.Sigmoid), ["tl4"], ["sc"])
        S.op("dve", lambda e: e.tensor_scalar(out=omlh, in0=lbh, scalar1=-1.0, scalar2=1.0, op0=ALU.mult, op1=ALU.add), ["sc"], ["sc"])
        S.op("dve", lambda e: e.tensor_scalar(out=nomlh, in0=omlh, scalar1=-1.0, scalar2=None, op0=ALU.mult), ["sc"], ["sc"])
        S.op("dve", lambda e: e.tensor_scalar(out=biasc, in0=biasc, scalar1=negM, scalar2=None, op0=ALU.add), ["cst", "sc"], ["cst"])
        S.op("act", lambda e: e.activation(out=silT.rearrange("p k r -> p (k r)"), in_=cTt, func=AF.Silu), ["cT"], ["silT"])

        ring_off = [R_RING.alloc(16384) for _ in range(2)]
        ring_i = [0]

        def load_w16(src_ap):
            slot = ring_i[0] % 2
            ring_i[0] += 1
            v = bfv(ring_off[slot], 8192)
            key = ("ring", slot)
            S.dma("pool", lambda e: e.dma_start(out=v.rearrange("p (a b) -> p a b", b=2048),
                                                in_=src_ap.rearrange("p (a b) -> p a b", b=2048)),
                  ("ring", slot), writes=[key])
            return v, key

        for grp in range(24):
            w, wk = load_w16(d_wada[grp])
            wv = w.rearrange("p (b k c) -> p b k c", b=4, k=16)
            for blk in range(4):
                cb = grp * 4 + blk
                for k in range(16):
                    S.op("pe", lambda e, cb=cb, blk=blk, k=k, wv=wv: e.matmul(
                        PS[0][:, cb * 5:cb * 5 + 5], lhsT=wv[:, blk, k, :], rhs=silT[:, k, :],
                        start=(k == 0), stop=(k == 15)), [wk, "silT"], [("ps", 0)])
        bT = prm[:, P_BT:P_BT + 96]
        S.op("dve", lambda e: e.tensor_tensor(out=modT, in0=PS[0][:, 0:480].rearrange("p (c r) -> p c r", r=5),
                                              in1=bT.unsqueeze(2).to_broadcast([128, 96, 5]), op=ALU.add),
             [("ps", 0), "prm"], ["modT"])
        n1b = prm[:, P_N1:P_N1 + 16].unsqueeze(2).to_broadcast([128, 16, 5])
        n2b = prm[:, P_N2:P_N2 + 16].unsqueeze(2).to_broadcast([128, 16, 5])
        S.op("dve", lambda e: e.scalar_tensor_tensor(out=G1, in0=modT[:, 16:32, :], scalar=1.0, in1=n1b, op0=ALU.add, op1=ALU.mult), ["modT", "prm"], ["G"])
        S.op("dve", lambda e: e.scalar_tensor_tensor(out=G2, in0=modT[:, 64:80, :], scalar=1.0, in1=n2b, op0=ALU.add, op1=ALU.mult), ["modT", "prm"], ["G"])
        SH1 = modT[:, 0:16, :]
        GA1 = modT[:, 32:48, :]
        SH2 = modT[:, 48:64, :]
        GA2 = modT[:, 80:96, :]
        S.barrier()
        chk("p1")
        R_M.reset()

        o_h = R_H.alloc(16 * NT * 2)
        DBG["hT"] = o_h
        hT = bfv(o_h, 16 * NT).rearrange("p (k t) -> p k t", t=NT)

        def rstd_from_ps(ps_ap, n, inv_n, out_ap, tmp_ap, rd, wr_key):
            S.op("act", lambda e: e.activation(out=out_ap, in_=ps_ap, func=AF.Ln, bias=epsc, scale=inv_n), rd + ["sc"], [wr_key])
            S.op("act", lambda e: e.activation(out=out_ap, in_=out_ap, func=AF.Exp, scale=-0.5), [wr_key], [wr_key])

        def make_hT(d_x, blocks, gmod, shmod, dst, G, SHm, name):
            o_xb = [R_M.alloc(2 * 512 * 4) for _ in range(2)]
            o_sq = [R_M.alloc(512 * 4) for _ in range(2)]
            o_rs = R_M.alloc(512 * 4)
            o_tp = [R_M.alloc(512 * 4) for _ in range(2)]
            cnt = [0]
            for (src, t0, n, rsel) in blocks:
                rs = f32v(o_rs, 512)[:, 0:n]
                for kg in range(8):
                    b = cnt[0] % 2
                    cnt[0] += 1
                    xb = f32v(o_xb[b], 1024).rearrange("p (k t) -> p k t", k=2)[:, :, 0:n]
                    S.dma("sp", lambda e, xb=xb, src=src, kg=kg: e.dma_start(out=xb, in_=src[:, kg * 2:kg * 2 + 2, :]),
                          ("ld", "xb%d" % b), writes=[("xb", b)])
                    for kk in range(2):
                        k = kg * 2 + kk
                        sb = k % 2
                        sq = f32v(o_sq[sb], 512)[:, 0:n]
                        S.op("act", lambda e, sq=sq, xb=xb, kk=kk: e.activation(out=sq, in_=xb[:, kk, :], func=AF.Square), [("xb", b)], [("sq", sb)])
                        S.op("pe", lambda e, sq=sq, k=k, n=n: e.matmul(PS[7][:, 0:n], lhsT=ones, rhs=sq, start=(k == 0), stop=(k == 15)),
                             [("sq", sb), "cst"], [("ps", 7)])
                rstd_from_ps(PS[7][:, 0:n], n, 1.0 / 2048.0, rs, rs, [("ps", 7)], "rs")
                for kg in range(8):
                    b = cnt[0] % 2
                    cnt[0] += 1
                    xb = f32v(o_xb[b], 1024).rearrange("p (k t) -> p k t", k=2)[:, :, 0:n]
                    S.dma("sp", lambda e, xb=xb, src=src, kg=kg: e.dma_start(out=xb, in_=src[:, kg * 2:kg * 2 + 2, :]),
                          ("ld", "xb%d" % b), writes=[("xb", b)])
                    for kk in range(2):
                        k = kg * 2 + kk
                        tb_ = k % 2
                        for (c0, cn, r) in rsel:
                            tp = f32v(o_tp[tb_], 512)[:, c0:c0 + cn]
                            S.op("dve", lambda e, tp=tp, xb=xb, kk=kk, k=k, r=r, c0=c0, cn=cn, rs=rs: e.scalar_tensor_tensor(
                                out=tp, in0=xb[:, kk, c0:c0 + cn], scalar=G[:, k, r:r + 1], in1=rs[:, c0:c0 + cn],
                                op0=ALU.mult, op1=ALU.mult), [("xb", b), "G", "rs"], [("tp", tb_)])
                            S.op("act", lambda e, tp=tp, k=k, r=r, c0=c0, cn=cn, t0=t0: e.activation(
                                out=dst[:, k, t0 + c0:t0 + c0 + cn], in_=tp, func=AF.Identity, bias=SHm[:, k, r:r + 1], scale=1.0),
                                [("tp", tb_), "modT"], [name])

        PBLK = [(0, 512), (512, 512)]
        SAMPLE_RSEL = [(4 * bi, 4, 1 + bi) for bi in range(4)]

        o_kT = R_X.alloc(4 * 2064 * 2)
        DBG["kT"] = o_kT
        kT = bfv(o_kT, 4 * 2064).rearrange("p (h t) -> p h t", h=4)
        o_v = R_X.alloc(16 * 512 * 2)
        DBG["vT"] = o_v
        vT = bfv(o_v, 16 * 512).rearrange("p (t c) -> p t c", c=512)
        o_vs = R_X.alloc(4 * 512 * 2)
        vS = bfv(o_vs, 4 * 512).rearrange("p (b c) -> p b c", c=512)
        o_qT = R_X.alloc(8 * NT * 2)
        DBG["qT"] = o_qT
        qT = bfv(o_qT, 8 * NT).rearrange("p (h t) -> p h t", h=8)
        x_mark = R_X.cur

        psc = [0]

        def next_ps(lo=0, hi=6):
            i = lo + psc[0] % (hi - lo)
            psc[0] += 1
            return i

        deferred = []

        def flush_deferred():
            while deferred:
                deferred.pop(0)()

        def proj_fm(wv, wk, blk, src, t0, n, post):
            pi = next_ps()
            for k in range(16):
                S.op("pe", lambda e, k=k, pi=pi: e.matmul(PS[pi][:, 0:n], lhsT=wv[:, blk, k, :], rhs=src[:, k, t0:t0 + n],
                                                          start=(k == 0), stop=(k == 15)), [wk, "hT"], [("ps", pi)])
            flush_deferred()
            post(pi)

        def proj_tm(wv, wk, src, t0, m, post):
            pi = next_ps()
            for k in range(16):
                S.op("pe", lambda e, k=k, pi=pi: e.matmul(PS[pi][0:m, :], lhsT=src[:, k, t0:t0 + m], rhs=wv[:, k, :],
                                                          start=(k == 0), stop=(k == 15)), [wk, "hT"], [("ps", pi)])
            flush_deferred()
            post(pi)

        tmp_off = {}

        def tmpf(name, n=512, region=None):
            if name not in tmp_off:
                tmp_off[name] = (region or R_M).alloc(n * 4)
            return f32v(tmp_off[name], n)

        def tmpb(name, n=512, region=None):
            if name not in tmp_off:
                tmp_off[name] = (region or R_M).alloc(n * 2)
            return bfv(tmp_off[name], n)

        def qk_post(pi, n, gcol, out_bf, out_f32_key=None, out_f32=None, tag="qk"):
            sq = tmpf(tag + "sq")[:, 0:n]
            rs = tmpf(tag + "rs")[:, 0:n]
            S.op("act", lambda e: e.activation(out=sq, in_=PS[pi][:, 0:n], func=AF.Square), [("ps", pi)], [tag + "sq"])
            S.op("pe", lambda e: e.matmul(PS[6][:, 0:n], lhsT=bones, rhs=sq, start=True, stop=True), [tag + "sq", "cst"], [("ps", 6)])
            rstd_from_ps(PS[6][:, 0:n], n, 1.0 / 64.0, rs, rs, [("ps", 6)], tag + "rs")
            if out_f32 is not None:
                S.op("dve", lambda e: e.scalar_tensor_tensor(out=out_f32, in0=PS[pi][:, 0:n], scalar=gcol, in1=rs, op0=ALU.mult, op1=ALU.mult),
                     [("ps", pi), tag + "rs", "sc", "prm"], [out_f32_key])
                S.op("act", lambda e: e.activation(out=out_bf[0], in_=out_f32, func=AF.Copy), [out_f32_key], [out_bf[1]])
            else:
                S.op("dve", lambda e: e.scalar_tensor_tensor(out=out_bf[0], in0=PS[pi][:, 0:n], scalar=gcol, in1=rs, op0=ALU.mult, op1=ALU.mult),
                     [("ps", pi), tag + "rs", "sc", "prm"], [out_bf[1]])

        gk = prm[:, P_GK:P_GK + 1]
        flag = prm[:, P_FLAG:P_FLAG + 1]
        hgg = prm[:, P_HGG:P_HGG + 1]

        def hgrn_bufs(region, ntok, ntile, with_q):
            d = {}
            d["kt"] = bfv(region.alloc(4 * ntok * 2), 4 * ntok).rearrange("p (h t) -> p h t", h=4)
            d["khat"] = bfv(region.alloc(ntile * 512 * 2), ntile * 512).rearrange("p (t c) -> p t c", c=512)
            d["hi"] = bfv(region.alloc(ntile * 512 * 2), ntile * 512).rearrange("p (t c) -> p t c", c=512)
            nch = ntok // 64 + 4
            d["d"] = f32v(region.alloc(4 * nch * 4), 4 * nch).rearrange("p (h c) -> p h c", h=4)
            if with_q:
                d["eg"] = bfv(region.alloc(4 * ntok * 2), 4 * ntok).rearrange("p (h t) -> p h t", h=4)
                d["qt"] = bfv(region.alloc(4 * ntok * 2), 4 * ntok).rearrange("p (h t) -> p h t", h=4)
                d["gate"] = bfv(region.alloc(4 * ntok * 2), 4 * ntok).rearrange("p (h t) -> p h t", h=4)
                d["khs"] = bfv(region.alloc(4 * 512 * 2), 4 * 512).rearrange("p (b c) -> p b c", c=512)
                d["his"] = bfv(region.alloc(4 * 512 * 2), 4 * 512).rearrange("p (b c) -> p b c", c=512)
            return d

        o_Sst = R_CONST.alloc(8 * 128 * 4)
        DBG["Sst"] = o_Sst
        Sst = f32v(o_Sst, 1024).rearrange("p (h v) -> p h v", h=8)

        hfc = [0]

        def hf_post(pi, hb, hl, hglob, t0, n, with_q, is_sample, tag):
            sg = tmpf("hf_sg")[:, 0:n]
            lf = tmpf("hf_lf")[:, 0:n]
            hk = tmpf("hf_hk")[:, 0:n]
            G = tmpf("hf_G")[:, 0:n]
            eg = tmpf("hf_eg")[:, 0:n]
            kf = sg
            kp_ = hfc[0] % 2
            hfc[0] += 1
            khT = tmpb("hf_khT%d" % kp_)[:, 0:n]
            KHK = ("hf_khT", kp_)
            S.op("act", lambda e: e.activation(out=sg, in_=PS[pi][:, 0:n], func=AF.Sigmoid), [("ps", pi)], ["hf_sg"])
            S.op("dve", lambda e: e.tensor_scalar(out=lf, in0=sg, scalar1=omlh[:, hglob:hglob + 1], scalar2=lbh[:, hglob:hglob + 1],
                                                  op0=ALU.mult, op1=ALU.add), ["hf_sg", "sc"], ["hf_lf"])
            S.op("act", lambda e: e.activation(out=lf, in_=lf, func=AF.Ln), ["hf_lf"], ["hf_lf"])
            S.op("dve", lambda e: e.tensor_scalar(out=hk, in0=sg, scalar1=nomlh[:, hglob:hglob + 1], scalar2=omlh[:, hglob:hglob + 1],
                                                  op0=ALU.mult, op1=ALU.add), ["hf_sg", "sc"], ["hf_hk"])
            msk = scanms[:, 0:n] if is_sample else scanm[:, 0:n]
            S.op("dve", lambda e: e.tensor_tensor_scan(out=G, data0=msk, data1=lf, initial=0.0, op0=ALU.mult, op1=ALU.add),
                 ["hf_lf", "cst"], ["hf_G"])
            S.op("act", lambda e: e.activation(out=eg, in_=G, func=AF.Exp), ["hf_G"], ["hf_eg"])
            S.op("act", lambda e: e.activation(out=G, in_=G, func=AF.Exp, scale=-1.0), ["hf_G"], ["hf_G"])
            S.op("dve", lambda e: e.tensor_tensor(out=kf, in0=hk, in1=G, op=ALU.mult), ["hf_hk", "hf_G", "hf_sg"], ["hf_sg"])
            csz = 4 if is_sample else 64
            ncn = n // csz
            c0 = t0 // 64 if not is_sample else 16
            dv = hb["d"][:, hl, c0:c0 + ncn]
            S.op("dve", lambda e: e.tensor_copy(out=dv, in_=eg.rearrange("p (c s) -> p c s", s=csz)[:, :, csz - 1]), ["hf_eg"], [tag + "d"])
            S.op("act", lambda e: e.activation(out=hb["kt"][:, hl, t0:t0 + n], in_=kf, func=AF.Copy), ["hf_sg"], [tag + "kt"])
            if with_q:
                S.op("act", lambda e: e.activation(out=hb["eg"][:, hl, t0:t0 + n], in_=eg, func=AF.Copy), ["hf_eg"], [tag + "eg"])
            S.op("dve", lambda e: e.tensor_tensor(out=khT.rearrange("p (c s) -> p c s", s=csz), in0=kf.rearrange("p (c s) -> p c s", s=csz),
                                                  in1=dv.unsqueeze(2).to_broadcast([128, ncn, csz]), op=ALU.mult),
                 ["hf_sg", tag + "d"], [KHK])
            def tr_():
                PSb = PS[6].bitcast(BF16)
                if not is_sample:
                    for j in range(n // 128):
                        S.op("pe", lambda e, j=j: e.transpose(out=PSb[:, j * 128:(j + 1) * 128], in_=khT[:, j * 128:(j + 1) * 128], identity=identb),
                             [KHK, "bfc"], [("ps", 6)])
                    tl0 = t0 // 128
                    S.op("act", lambda e: e.activation(out=hb["khat"][:, tl0:tl0 + n // 128, hl * 128:(hl + 1) * 128],
                                                       in_=PSb[:, 0:n].rearrange("p (j c) -> p j c", c=128), func=AF.Copy),
                         [("ps", 6)], [tag + "khat"])
                else:
                    for bi in range(4):
                        S.op("pe", lambda e, bi=bi: e.transpose(out=PSb[0:4, bi * 128:(bi + 1) * 128], in_=khT[:, bi * 4:bi * 4 + 4], identity=identb),
                             [KHK, "bfc"], [("ps", 6)])
                    S.op("act", lambda e: e.activation(out=hb["khs"][0:4, :, hl * 128:(hl + 1) * 128],
                                                       in_=PSb[0:4, 0:512].rearrange("p (j c) -> p j c", c=128), func=AF.Copy),
                         [("ps", 6)], [tag + "khs"])
            deferred.append(tr_)

        def state_chain(hb, hl, hglob, tile0, nchunk, ch0, store_bf, tag):
            flush_deferred()
            for c in range(nchunk):
                tl_, e2 = tile0 + c // 2, c % 2
                pu = next_ps()
                if store_bf is not None:
                    S.op("act", lambda e, c=c: e.activation(out=store_bf[:, c, :], in_=Sst[:, hglob, :], func=AF.Copy), [("S", hglob)], [tag + "Sbf"])
                S.op("pe", lambda e, c=c, tl_=tl_, e2=e2, pu=pu: e.matmul(
                    PS[pu][:, 0:128],
                    lhsT=hb["khat"][64 * e2:64 * e2 + 64, tl_, hl * 128:(hl + 1) * 128],
                    rhs=hb["hi"][64 * e2:64 * e2 + 64, tl_, hl * 128:(hl + 1) * 128], start=True, stop=True),
                    [tag + "khat", tag + "hi"], [("ps", pu)])
                S.op("dve", lambda e, c=c, pu=pu: e.scalar_tensor_tensor(
                    out=Sst[:, hglob, :], in0=Sst[:, hglob, :], scalar=hb["d"][:, hl, ch0 + c:ch0 + c + 1],
                    in1=PS[pu][:, 0:128], op0=ALU.mult, op1=ALU.add),
                    [("ps", pu), tag + "d", ("S", hglob)], [("S", hglob)])

        make_hT(d_xp, [(d_xp[:, :, t0:t0 + n], t0, n, [(0, n, 0)]) for (t0, n) in PBLK], G1, SH1, hT, G1, SH1, "hT")
        S.barrier()
        R_M.reset()
        for h in range(8):
            S.op("dve", lambda e, h=h: e.memset(Sst[:, h, :], 0.0), [], [("S", h)])
        R_AM_mark = R_AM.cur
        g_i = 0
        w, wk = load_w16(d_win[g_i]); g_i += 1
        wv = w.rearrange("p (b k c) -> p b k c", b=4, k=16)
        for blk in range(4):
            for (t0, n) in PBLK:
                proj_fm(wv, wk, blk, hT, t0, n, lambda pi, blk=blk, t0=t0, n=n: qk_post(
                    pi, n, gk, (kT[:, blk, t0:t0 + n], "kT"), tag="qk"))
        w, wk = load_w16(d_win[g_i]); g_i += 1
        wv = w.rearrange("p (k c) -> p k c", k=16)
        for tl_ in range(8):
            proj_tm(wv, wk, hT, tl_ * 128, 128, lambda pi, tl_=tl_: S.op(
                "act", lambda e: e.activation(out=vT[:, tl_, :], in_=PS[pi][:, :], func=AF.Copy), [("ps", pi)], ["vT"]))
        for hgp in range(2):
            hb = hgrn_bufs(R_AM, 1024, 8, False)
            w, wk = load_w16(d_win[g_i]); g_i += 1
            wv = w.rearrange("p (b k c) -> p b k c", b=4, k=16)
            for hl in range(4):
                for (t0, n) in PBLK:
                    proj_fm(wv, wk, hl, hT, t0, n, lambda pi, hl=hl, t0=t0, n=n, hb=hb: hf_post(
                        pi, hb, hl, hgp * 4 + hl, t0, n, False, False, "p"))
            w, wk = load_w16(d_win[g_i]); g_i += 1
            wv = w.rearrange("p (k c) -> p k c", k=16)
            for tl_ in range(8):
                proj_tm(wv, wk, hT, tl_ * 128, 128, lambda pi, tl_=tl_, hb=hb: S.op(
                    "act", lambda e: e.activation(out=hb["hi"][:, tl_, :], in_=PS[pi][:, :], func=AF.Copy), [("ps", pi)], ["phi"]))
            for hl in range(4):
                state_chain(hb, hl, hgp * 4 + hl, 0, 16, 0, None, "p")
            S.barrier()
            R_AM.cur = R_AM_mark
        for h in range(8):
            S.op("dve", lambda e, h=h: e.tensor_scalar(out=Sst[:, h, :], in0=Sst[:, h, :], scalar1=flag, scalar2=None, op0=ALU.mult),
                 [("S", h), "prm"], [("S", h)])
        S.barrier()
        chk("p2")
        R_M.reset()
        tmp_off.clear()

        OWN_BLOCKS = [(d_xo[:, :, t0:t0 + n], t0, n, [(0, n, 0)]) for (t0, n) in PBLK] + [(d_xs, 1024, 16, SAMPLE_RSEL)]
        make_hT(None, OWN_BLOCKS, G1, SH1, hT, G1, SH1, "hT")
        S.barrier()
        chk("p3a0")
        R_M.reset()
        TBS = [(0, 512), (512, 512), (1024, 16)]
        o_am = R_AM.alloc(16 * NT * 2)
        DBG["amT"] = o_am
        amT = bfv(o_am, 16 * NT).rearrange("p (f t) -> p f t", f=16)
        kst = [tmpf("kst0"), tmpf("kst1")]
        vst = [tmpf("vst0"), tmpf("vst1")]
        stc = [0]
        for qg in range(2):
            w, wk = load_w16(d_win[g_i]); g_i += 1
            wv = w.rearrange("p (b k c) -> p b k c", b=4, k=16)
            for blk in range(4):
                h = qg * 4 + blk
                for (t0, n) in TBS:
                    proj_fm(wv, wk, blk, hT, t0, n, lambda pi, h=h, t0=t0, n=n: qk_post(pi, n, gq8, (qT[:, h, t0:t0 + n], "qT"), tag="qk"))
        S.barrier()
        chk("p3a1")
        w, wk = load_w16(d_win[g_i]); g_i += 1
        wv = w.rearrange("p (b k c) -> p b k c", b=4, k=16)
        for blk in range(4):
            for (t0, n) in TBS:
                def kpost(pi, blk=blk, t0=t0, n=n):
                    b = stc[0] % 2
                    stc[0] += 1
                    st = kst[b][:, 0:n]
                    qk_post(pi, n, gk, (kT[:, blk, 1024 + t0:1024 + t0 + n], "kT"), out_f32_key=("kst", b), out_f32=st, tag="qk")
                    S.dma("sp", lambda e: e.dma_start(out=out_kT[:, blk * NT + t0:blk * NT + t0 + n], in_=st), ("out", "k%d" % b), reads=[("kst", b)])
                proj_fm(wv, wk, blk, hT, t0, n, kpost)
        S.barrier()
        chk("p3a2")
        w, wk = load_w16(d_win[g_i]); g_i += 1
        wv = w.rearrange("p (k c) -> p k c", k=16)
        for tl_ in range(8):
            def vpost(pi, tl_=tl_):
                b = stc[0] % 2
                stc[0] += 1
                S.op("act", lambda e: e.activation(out=vT[:, 8 + tl_, :], in_=PS[pi][:, :], func=AF.Copy), [("ps", pi)], ["vT"])
                S.op("dve", lambda e: e.tensor_copy(out=vst[b], in_=PS[pi][:, :]), [("ps", pi)], [("vst", b)])
                S.dma("sp", lambda e: e.dma_start(out=out_v[tl_ * 128:(tl_ + 1) * 128, :], in_=vst[b]), ("out", "v%d" % b), reads=[("vst", b)])
            proj_tm(wv, wk, hT, tl_ * 128, 128, vpost)
        S.barrier()
        chk("p3a3")
        for bi in range(4):
            def vspost(pi, bi=bi):
                b = stc[0] % 2
                stc[0] += 1
                S.op("act", lambda e: e.activation(out=vS[0:4, bi, :], in_=PS[pi][0:4, :], func=AF.Copy), [("ps", pi)], ["vS"])
                S.op("dve", lambda e: e.tensor_copy(out=vst[b][0:4, :], in_=PS[pi][0:4, :]), [("ps", pi)], [("vst", b)])
                S.dma("sp", lambda e: e.dma_start(out=out_v[1024 + bi * 4:1028 + bi * 4, :], in_=vst[b][0:4, :]), ("out", "v%d" % b), reads=[("vst", b)])
            proj_tm(wv, wk, hT, 1024 + bi * 4, 4, vspost)
        S.barrier()
        chk("p3a")
        R_M.reset()
        tmp_off.clear()

        att_tmp = [tmpf("att_t0"), tmpf("att_t1")]
        att_P = [tmpb("att_P0"), tmpb("att_P1"), tmpb("att_P2"), tmpb("att_P3")]
        att_o = tmpf("att_o")
        att_r = [tmpf("att_r0"), tmpf("att_r1")]
        pc = [0]

        def subln_finish(o_ap, n, out_ap, tag, gcol, extra=None, a3=None):
            sq = tmpf("sl_sq")[:, 0:n]
            rs = tmpf("sl_rs")[:, 0:n]
            o3, rs3 = o_ap, rs
            if a3 is not None:
                o3 = o_ap.rearrange("p (a b) -> p a b", a=a3)
                rs3 = rs.rearrange("p (a b) -> p a b", a=a3)
            S.op("act", lambda e: e.activation(out=sq, in_=o_ap, func=AF.Square), [tag], ["sl_sq"])
            S.op("pe", lambda e: e.matmul(PS[6][:, 0:n], lhsT=ones, rhs=sq, start=True, stop=True), ["sl_sq", "cst"], [("ps", 6)])
            rstd_from_ps(PS[6][:, 0:n], n, 1.0 / 128.0, rs, rs, [("ps", 6)], "sl_rs")
            if extra is None:
                S.op("dve", lambda e: e.scalar_tensor_tensor(out=out_ap, in0=o3, scalar=gcol, in1=rs3, op0=ALU.mult, op1=ALU.mult),
                     [tag, "sl_rs", "sc", "prm"], ["amT"])
            else:
                S.op("dve", lambda e: e.scalar_tensor_tensor(out=rs, in0=o_ap, scalar=gcol, in1=rs, op0=ALU.mult, op1=ALU.mult),
                     [tag, "sl_rs", "sc", "prm"], ["sl_rs"])
                S.op("dve", lambda e: e.tensor_tensor(out=out_ap, in0=rs, in1=extra[0], op=ALU.mult), ["sl_rs", extra[1]], ["amT"])

        att_tmp.append(tmpf("att_t2"))
        SBK = [4, 5, 7]
        units = []
        for h in range(8):
            for qb in range(2):
                tiles = _tile_list(qb)
                for ti, kt in enumerate(tiles):
                    for m in range(2):
                        units.append((h, qb, ti, kt, m, len(tiles)))

        def stage_a(i, u):
            h, qb, ti, kt, m, nt = u
            kvh = h // 2
            sb, pb = i % 3, i % 4
            d0 = 1024 + 512 * qb - 128 * kt
            ws = 512 if d0 >= 128 else d0 + 384
            ndv = cst[:, C_ND + ws:C_ND + ws + 512]
            S.op("pe", lambda e: e.matmul(
                PS[SBK[sb]][:, :], lhsT=kT[64 * m:64 * m + 64, kvh, kt * 128:(kt + 1) * 128],
                rhs=qT[64 * m:64 * m + 64, h, qb * 512:(qb + 1) * 512], start=True, stop=True),
                ["kT", "qT"], [("ps", SBK[sb])])
            S.op("dve", lambda e: e.scalar_tensor_tensor(
                out=att_tmp[sb], in0=ndv, scalar=SLOPES[h], in1=PS[SBK[sb]][:, :], op0=ALU.mult, op1=ALU.add),
                [("ps", SBK[sb]), "cst"], [("att_t", sb)])
            bc = biasc[:, (h * 2 + qb) * 16 + kt:(h * 2 + qb) * 16 + kt + 1]
            S.op("act", lambda e: e.activation(out=att_P[pb], in_=att_tmp[sb], func=AF.Exp, bias=bc, scale=1.0),
                 [("att_t", sb), "cst"], [("att_P", pb)])

        def fin1(h, qb):
            for m in range(2):
                S.op("act", lambda e, m=m: e.activation(out=att_r[m], in_=PS[2 + m][:, :], func=AF.Ln), [("ps", 2 + m)], [("att_r", m)])
                S.op("act", lambda e, m=m: e.activation(out=att_r[m], in_=att_r[m], func=AF.Exp, scale=-1.0), [("att_r", m)], [("att_r", m)])
                S.op("dve", lambda e, m=m: e.tensor_tensor(out=att_r[m], in0=PS[m][:, :], in1=att_r[m], op=ALU.mult),
                     [("ps", m), ("att_r", m)], [("att_r", m)])
            S.op("dve", lambda e: e.scalar_tensor_tensor(out=att_o, in0=att_r[1], scalar=neglam, in1=att_r[0], op0=ALU.mult, op1=ALU.add),
                 [("att_r", 0), ("att_r", 1), "sc"], ["att_o"])
            sq = tmpf("sl_sq")
            S.op("act", lambda e: e.activation(out=sq, in_=att_o, func=AF.Square), ["att_o"], ["sl_sq"])

        def fin2(h, qb):
            sq = tmpf("sl_sq")
            rs = tmpf("sl_rs")
            S.op("pe", lambda e: e.matmul(PS[6][:, :], lhsT=ones, rhs=sq, start=True, stop=True), ["sl_sq", "cst"], [("ps", 6)])
            rstd_from_ps(PS[6][:, :], 512, 1.0 / 128.0, rs, rs, [("ps", 6)], "sl_rs")
            S.op("dve", lambda e: e.scalar_tensor_tensor(out=amT[:, h, qb * 512:(qb + 1) * 512], in0=att_o, scalar=sg8, in1=rs,
                                                         op0=ALU.mult, op1=ALU.mult), ["att_o", "sl_rs", "sc", "prm"], ["amT"])

        def stage_b(i, u):
            h, qb, ti, kt, m, nt = u
            kvh = h // 2
            pb = i % 4
            S.op("pe", lambda e: e.matmul(PS[m][:, :], lhsT=vT[:, kt, kvh * 128:(kvh + 1) * 128], rhs=att_P[pb],
                                          start=(ti == 0), stop=(ti == nt - 1)), [("att_P", pb), "vT"], [("ps", m)])
            S.op("pe", lambda e: e.matmul(PS[2 + m][:, :], lhsT=onesb, rhs=att_P[pb],
                                          start=(ti == 0), stop=(ti == nt - 1)), [("att_P", pb), "bfc"], [("ps", 2 + m)])

        pend = []
        NU = len(units)
        for i in range(NU + 2):
            if i < NU:
                stage_a(i, units[i])
            if i >= 2:
                u = units[i - 2]
                stage_b(i - 2, u)
                for p_ in pend:
                    p_[2] -= 1
                while pend and pend[0][2] <= 0:
                    hh, qq, _ = pend.pop(0)
                    fin2(hh, qq)
                if u[2] == u[5] - 1 and u[4] == 1:
                    fin1(u[0], u[1])
                    pend.append([u[0], u[1], 4])
        for hh, qq, _ in pend:
            fin2(hh, qq)
        S.barrier()
        chk("p3b")
        R_M.reset()
        tmp_off.clear()

        o_pos = R_X.alloc(2048 * 4)
        posT = f32v(o_pos, 2048, 3).rearrange("p (b i) -> p b i", b=16)
        o_R3 = R_X.alloc(512 * 4)
        R3 = f32v(o_R3, 512, 3)
        o_nb = R_X.alloc(64 * 4)
        newb = f32v(o_nb, 64, 4)
        o_idx = R_X.alloc(512 * 4)
        idx = i32v(o_idx, 512)
        S.dma("sp", lambda e: e.dma_start(out=posT.rearrange("p b i -> p (b i)"), in_=d_posT), ("ld", "c0"), writes=["posT"])
        S.dma("sp", lambda e: e.dma_start(out=R3, in_=d_R3), ("ld", "c1"), writes=["R3"])
        S.dma("sp", lambda e: e.dma_start(out=newb, in_=d_newb), ("ld", "c2"), writes=["newb"])
        tmpf("sl_sq", 64)
        tmpf("sl_rs", 64)
        o_pt_ = R_M.alloc(512 * 4)
        pti = i32v(o_pt_, 512)
        ptf = f32v(o_pt_, 512)
        pid = f32v(R_M.alloc(4), 1)
        S.dma("sp", lambda e: e.dma_start(out=pti, in_=d_pt.rearrange("b n -> (b n)").partition_broadcast(128)), ("ld", "c3"), writes=["pti"])
        S.op("pool", lambda e: e.iota(pid, pattern=[[0, 1]], base=0, channel_multiplier=1, allow_small_or_imprecise_dtypes=True), [], ["pid"])
        S.op("dve", lambda e: e.tensor_copy(out=ptf, in_=pti), ["pti"], ["ptf"])
        S.op("dve", lambda e: e.tensor_scalar(out=ptf, in0=ptf, scalar1=128.0, scalar2=pid, op0=ALU.mult, op1=ALU.add), ["ptf", "pid"], ["ptf"])
        S.op("dve", lambda e: e.tensor_copy(out=idx, in_=ptf), ["ptf"], ["idx"])
        NSL = 8
        kvr = [bfv(R_M.alloc(2048), 1024) for _ in range(NSL)]
        kr = [t[:, 0:512] for t in kvr]
        vr = [t[:, 512:1024] for t in kvr]
        qbd = bfv(R_M.alloc(64 * 2), 64).rearrange("p (k c) -> p k c", k=4)
        Ps = [bfv(R_M.alloc(1024), 512) for _ in range(2)]
        Pn = bfv(R_M.alloc(128), 64, 4)
        sa_t = f32v(R_M.alloc(64 * 4), 64)
        sa_r = f32v(R_M.alloc(64 * 4), 64)
        sa_o = f32v(R_M.alloc(32 * 4), 32)
        pgc = [0]
        for bi in range(4):
            S.op("dve", lambda e: e.memset(qbd.rearrange("p k c -> p (k c)"), 0.0), [], ["qbd"])
            for kvh in range(4):
                for g in range(2):
                    for m in range(2):
                        S.op("dve", lambda e, kvh=kvh, g=g, m=m, bi=bi: e.tensor_copy(
                            out=qbd[64 * m:64 * m + 64, kvh, m * 8 + g * 4:m * 8 + g * 4 + 4],
                            in_=qT[64 * m:64 * m + 64, kvh * 2 + g, 1024 + bi * 4:1028 + bi * 4]), ["qT"], ["qbd"])
            S.op("pe", lambda e: e.matmul(PS[2][:, 0:128], lhsT=zerosb[:, 0:128], rhs=zerosb[:, 0:128], start=True, stop=False),
                 ["bfc"], [("ps", 2)])
            NPB, NRB = 2, 4
            NBT = 128 // NPB

            def sa_stage_a(Bq):
                sb = Bq % 2
                B8, r0 = (Bq * NPB) // 8, (Bq * NPB) % 8
                W_ = NPB * 64
                S.op("pe", lambda e: e.matmul(PS[sb][:, 0:W_], lhsT=posT[:, B8, :], rhs=R3[:, r0 * 64:(r0 + NPB) * 64], start=True, stop=False),
                     ["posT", "R3"], [("ps", sb)])
                for r in range(NPB):
                    pg = Bq * NPB + r
                    sl = (Bq % NRB) * NPB + r
                    ia = idx[:, bi * 128 + pg:bi * 128 + pg + 1]
                    S.dma("pool", lambda e: e.indirect_dma_start(
                        out=kvr[sl], out_offset=None, in_=d_ckv, in_offset=bass.IndirectOffsetOnAxis(ap=ia, axis=0)),
                        ("kvr", sl), reads=["idx"], writes=[("kr", sl), ("vr", sl)])
                    for kvh in range(4):
                        S.op("pe", lambda e, kvh=kvh: e.matmul(
                            PS[sb][:, r * 64 + kvh * 16:r * 64 + kvh * 16 + 16], lhsT=kr[sl][:, kvh * 128:(kvh + 1) * 128],
                            rhs=qbd[:, kvh, :], start=False, stop=(r == NPB - 1 and kvh == 3)), [("kr", sl), "qbd"], [("ps", sb)])
                S.op("act", lambda e: e.activation(out=Ps[sb][:, 0:W_], in_=PS[sb][:, 0:W_], func=AF.Exp, bias=negM, scale=1.0),
                     [("ps", sb), "sc"], [("Ps", sb)])

            def sa_stage_b(Bq):
                sb = Bq % 2
                for r in range(NPB):
                    sl = (Bq % NRB) * NPB + r
                    for kvh in range(4):
                        S.op("pe", lambda e, kvh=kvh: e.matmul(
                            PS[2][:, kvh * 16:kvh * 16 + 16], lhsT=vr[sl][:, kvh * 128:(kvh + 1) * 128],
                            rhs=Ps[sb][:, r * 64 + kvh * 16:r * 64 + kvh * 16 + 16], start=False, stop=False),
                            [("vr", sl), ("Ps", sb)], [("ps", 2)])
                    S.op("pe", lambda e: e.matmul(PS[2][:, 64:128], lhsT=onesb, rhs=Ps[sb][:, r * 64:(r + 1) * 64],
                                                  start=False, stop=False), [("Ps", sb), "bfc"], [("ps", 2)])

            sa_stage_a(0)
            for Bq in range(NBT):
                if Bq + 1 < NBT:
                    sa_stage_a(Bq + 1)
                sa_stage_b(Bq)
            for kvh in range(4):
                S.op("pe", lambda e, kvh=kvh, bi=bi: e.matmul(PS[3][0:4, kvh * 16:kvh * 16 + 16], lhsT=kT[:, kvh, 2048 + bi * 4:2052 + bi * 4],
                                                              rhs=qbd[:, kvh, :], start=True, stop=True), ["kT", "qbd"], [("ps", 3)])
            S.op("dve", lambda e: e.tensor_tensor(out=sa_t[0:4, :], in0=PS[3][0:4, 0:64], in1=newb, op=ALU.add), [("ps", 3), "newb"], ["sa_t"])
            S.op("act", lambda e: e.activation(out=Pn, in_=sa_t[0:4, :], func=AF.Exp, bias=negM[0:4, :], scale=1.0), ["sa_t", "sc"], ["Pn"])
            for kvh in range(4):
                S.op("pe", lambda e, kvh=kvh, bi=bi: e.matmul(PS[2][:, kvh * 16:kvh * 16 + 16], lhsT=vS[0:4, bi, kvh * 128:(kvh + 1) * 128],
                                                              rhs=Pn[:, kvh * 16:kvh * 16 + 16], start=False, stop=False), ["vS", "Pn"], [("ps", 2)])
            S.op("pe", lambda e: e.matmul(PS[2][:, 64:128], lhsT=onesb[0:4, :], rhs=Pn, start=False, stop=True), ["Pn", "bfc"], [("ps", 2)])
            S.op("act", lambda e: e.activation(out=sa_r, in_=PS[2][:, 64:128], func=AF.Ln), [("ps", 2)], ["sa_r"])
            S.op("act", lambda e: e.activation(out=sa_r, in_=sa_r, func=AF.Exp, scale=-1.0), ["sa_r"], ["sa_r"])
            S.op("dve", lambda e: e.tensor_tensor(out=sa_r, in0=PS[2][:, 0:64], in1=sa_r, op=ALU.mult), [("ps", 2), "sa_r"], ["sa_r"])
            T4 = sa_r.rearrange("p (k m c) -> p k m c", k=4, m=2)
            S.op("dve", lambda e: e.scalar_tensor_tensor(out=sa_o.rearrange("p (k c) -> p k c", k=4), in0=T4[:, :, 1, :], scalar=neglam,
                                                         in1=T4[:, :, 0, :], op0=ALU.mult, op1=ALU.add), ["sa_r", "sc"], ["sa_o"])
            subln_finish(sa_o, 32, amT[:, 0:8, 1024 + bi * 4:1028 + bi * 4], "sa_o", sg8, a3=8)
        S.barrier()
        chk("p3c")
        R_M.reset()
        tmp_off.clear()
        R_X.reset()

        o_Sbf = R_X.alloc(17 * 128 * 2)
        Sbf = bfv(o_Sbf, 17 * 128).rearrange("p (c v) -> p c v", c=17)
        o_abf = R_X.alloc(256 * 2)
        abf = bfv(o_abf, 256).rearrange("p (j t) -> p j t", j=4)
        o_as = R_X.alloc(16 * 2)
        a_s = bfv(o_as, 16, 4)[:, 0:4]
        Ss2 = [f32v(R_M.alloc(128 * 4), 128) for _ in range(2)]
        Ssb2 = [bfv(R_M.alloc(128 * 2), 128) for _ in range(2)]
        Sso = [f32v(R_M.alloc(128 * 4), 128) for _ in range(2)]
        Spo = f32v(R_M.alloc(128 * 4), 128)
        m_mark = R_M.cur
        xm = R_X.cur
        ssc = [0]
        for hgp in range(2):
            R_X.cur = xm
            hb = hgrn_bufs(R_X, NT, 9, True)
            w, wk = load_w16(d_win[g_i]); g_i += 1
            wv = w.rearrange("p (b k c) -> p b k c", b=4, k=16)
            for hl in range(4):
                for (t0, n) in TBS:
                    proj_fm(wv, wk, hl, hT, t0, n, lambda pi, hl=hl, t0=t0, n=n, hb=hb: hf_post(
                        pi, hb, hl, hgp * 4 + hl, t0, n, True, n == 16, "o"))
            w, wk = load_w16(d_win[g_i]); g_i += 1
            wv = w.rearrange("p (b k c) -> p b k c", b=4, k=16)
            for hl in range(4):
                for (t0, n) in TBS:
                    def hqpost(pi, hl=hl, t0=t0, n=n, hb=hb):
                        sl_ = tmpf("hf_sg")[:, 0:n]
                        S.op("act", lambda e: e.activation(out=sl_, in_=PS[pi][:, 0:n], func=AF.Silu), [("ps", pi)], ["hf_sg"])
                        S.op("dve", lambda e: e.tensor_tensor(out=hb["qt"][:, hl, t0:t0 + n], in0=sl_, in1=hb["eg"][:, hl, t0:t0 + n], op=ALU.mult),
                             ["hf_sg", "oeg"], ["oqt"])
                    proj_fm(wv, wk, hl, hT, t0, n, hqpost)
            w, wk = load_w16(d_win[g_i]); g_i += 1
            wv = w.rearrange("p (k c) -> p k c", k=16)
            for tl_ in range(8):
                proj_tm(wv, wk, hT, tl_ * 128, 128, lambda pi, tl_=tl_, hb=hb: S.op(
                    "act", lambda e: e.activation(out=hb["hi"][:, tl_, :], in_=PS[pi][:, :], func=AF.Copy), [("ps", pi)], ["ohi"]))
            for bi in range(4):
                proj_tm(wv, wk, hT, 1024 + bi * 4, 4, lambda pi, bi=bi, hb=hb: S.op(
                    "act", lambda e: e.activation(out=hb["his"][0:4, bi, :], in_=PS[pi][0:4, :], func=AF.Copy), [("ps", pi)], ["ohis"]))
            w, wk = load_w16(d_win[g_i]); g_i += 1
            wv = w.rearrange("p (b k c) -> p b k c", b=4, k=16)
            for hl in range(4):
                for (t0, n) in TBS:
                    proj_fm(wv, wk, hl, hT, t0, n, lambda pi, hl=hl, t0=t0, n=n, hb=hb: S.op(
                        "act", lambda e: e.activation(out=hb["gate"][:, hl, t0:t0 + n], in_=PS[pi][:, 0:n], func=AF.Silu), [("ps", pi)], ["ogate"]))
            for hl in range(4):
                hglob = hgp * 4 + hl
                state_chain(hb, hl, hglob, 0, 16, 0, Sbf, "o")
                S.op("dve", lambda e, hglob=hglob: e.tensor_copy(out=Spo, in_=Sst[:, hglob, :]), [("S", hglob)], ["Spo"])
                S.dma("sp", lambda e, hglob=hglob: e.dma_start(out=out_s[:, hglob * 128:(hglob + 1) * 128], in_=Spo), ("out", "s"), reads=["Spo"])
                for qb in range(2):
                    pa = next_ps()
                    for c in range(8):
                        j, e2 = c // 2, c % 2
                        tk = qb * 512 + c * 64
                        S.op("pe", lambda e, j=j, e2=e2, tk=tk, pa=pa: e.matmul(
                            PS[pa][64 * e2:64 * e2 + 64, j * 64:(j + 1) * 64], lhsT=hb["kt"][:, hl, tk:tk + 64],
                            rhs=hb["qt"][:, hl, tk:tk + 64], start=True, stop=True), ["okt", "oqt"], [("ps", pa)])
                    S.op("dve", lambda e, pa=pa: e.tensor_tensor(out=abf, in0=PS[pa][:, 0:256].rearrange("p (j t) -> p j t", j=4),
                                                                 in1=tri.unsqueeze(1).to_broadcast([128, 4, 64]), op=ALU.mult),
                         [("ps", pa), "cst"], ["abf"])
                    po = next_ps()
                    for c in range(8):
                        j, e2 = c // 2, c % 2
                        tk = qb * 512 + c * 64
                        tl_ = qb * 4 + j
                        S.op("pe", lambda e, c=c, tk=tk, po=po: e.matmul(
                            PS[po][:, c * 64:(c + 1) * 64], lhsT=Sbf[:, qb * 8 + c, :], rhs=hb["qt"][:, hl, tk:tk + 64], start=True, stop=False),
                            ["oSbf", "oqt"], [("ps", po)])
                        S.op("pe", lambda e, c=c, j=j, e2=e2, tl_=tl_, po=po: e.matmul(
                            PS[po][:, c * 64:(c + 1) * 64], lhsT=hb["hi"][64 * e2:64 * e2 + 64, tl_, hl * 128:(hl + 1) * 128],
                            rhs=abf[64 * e2:64 * e2 + 64, j, :], start=False, stop=True), ["ohi", "abf"], [("ps", po)])
                    subln_finish(PS[po][:, :], 512, amT[:, 8 + hglob, qb * 512:(qb + 1) * 512], ("ps", po), hgg,
                                 extra=(hb["gate"][:, hl, qb * 512:(qb + 1) * 512], "ogate"))
                pos_ = next_ps()
                for bi in range(4):
                    tk = 1024 + bi * 4
                    b = ssc[0] % 2
                    ssc[0] += 1
                    Ss, Ssb = Ss2[b], Ssb2[b]
                    S.dma("sp", lambda e, bi=bi, hglob=hglob: e.dma_start(out=Ss, in_=d_st0[bi, hglob]), ("ld", "ss%d" % b), writes=[("Ss", b)])
                    S.op("act", lambda e: e.activation(out=Ssb, in_=Ss, func=AF.Copy), [("Ss", b)], [("Ssb", b)])
                    pa = next_ps()
                    S.op("pe", lambda e, tk=tk, pa=pa: e.matmul(PS[pa][0:4, 0:4], lhsT=hb["kt"][:, hl, tk:tk + 4], rhs=hb["qt"][:, hl, tk:tk + 4],
                                                                start=True, stop=True), ["okt", "oqt"], [("ps", pa)])
                    S.op("dve", lambda e, pa=pa: e.tensor_tensor(out=a_s, in0=PS[pa][0:4, 0:4], in1=tri[0:4, 0:4], op=ALU.mult), [("ps", pa), "cst"], ["a_s"])
                    S.op("pe", lambda e, tk=tk, bi=bi: e.matmul(PS[pos_][:, bi * 4:bi * 4 + 4], lhsT=Ssb, rhs=hb["qt"][:, hl, tk:tk + 4],
                                                                start=True, stop=False), [("Ssb", b), "oqt"], [("ps", pos_)])
                    S.op("pe", lambda e, bi=bi: e.matmul(PS[pos_][:, bi * 4:bi * 4 + 4], lhsT=hb["his"][0:4, bi, hl * 128:(hl + 1) * 128], rhs=a_s,
                                                         start=False, stop=True), ["ohis", "a_s"], [("ps", pos_)])
                    S.op("pe", lambda e, bi=bi, pa=pa: e.matmul(PS[pa][:, 128:256], lhsT=hb["khs"][0:4, bi, hl * 128:(hl + 1) * 128],
                                                                rhs=hb["his"][0:4, bi, hl * 128:(hl + 1) * 128], start=True, stop=True),
                         ["okhs", "ohis"], [("ps", pa)])
                    S.op("dve", lambda e, bi=bi, pa=pa, b=b: e.scalar_tensor_tensor(
                        out=Sso[b], in0=Ss, scalar=hb["d"][:, hl, 16 + bi:17 + bi], in1=PS[pa][:, 128:256], op0=ALU.mult, op1=ALU.add),
                        [("ps", pa), "od", ("Ss", b)], [("Sso", b)])
                    S.dma("sp", lambda e, bi=bi, hglob=hglob, b=b: e.dma_start(out=out_ss[bi, :, hglob * 128:(hglob + 1) * 128], in_=Sso[b]),
                          ("out", "ss%d" % b), reads=[("Sso", b)])
                subln_finish(PS[pos_][:, 0:16], 16, amT[:, 8 + hglob, 1024:1040], ("ps", pos_), hgg, extra=(hb["gate"][:, hl, 1024:1040], "ogate"))
            S.barrier()
        S.barrier()
        chk("p3d")
        R_M.reset()
        tmp_off.clear()
        R_X.reset()

        o_x1 = R_X.alloc(16 * NT * 4)
        DBG["x1"] = o_x1
        x1 = f32v(o_x1, 16 * NT).rearrange("p (k t) -> p k t", k=16)
        xrb = [f32v(R_M.alloc(512 * 4), 512) for _ in range(2)]
        xrc = [0]
        o4t = f32v(R_M.alloc(16 * 4), 16)
        for og in range(4):
            w, wk = load_w16(d_wout[og])
            wv = w.rearrange("p (b f c) -> p b f c", b=4, f=16)
            for cbl in range(4):
                cb = og * 4 + cbl
                for (t0, n) in TBS:
                    pi = next_ps()
                    for f in range(16):
                        S.op("pe", lambda e, f=f, pi=pi, cbl=cbl, t0=t0, n=n: e.matmul(PS[pi][:, 0:n], lhsT=wv[:, cbl, f, :], rhs=amT[:, f, t0:t0 + n],
                                                                                       start=(f == 0), stop=(f == 15)), [wk, "amT"], [("ps", pi)])
                    b = xrc[0] % 2
                    xrc[0] += 1
                    src = d_xo[:, cb, t0:t0 + n] if n == 512 else d_xs[:, cb, :]
                    S.dma("sp", lambda e, b=b, src=src, n=n: e.dma_start(out=xrb[b][:, 0:n], in_=src), ("ld", "xr%d" % b), writes=[("xr", b)])
                    if n == 512:
                        S.op("dve", lambda e, pi=pi, b=b, cb=cb, t0=t0: e.scalar_tensor_tensor(
                            out=x1[:, cb, t0:t0 + 512], in0=PS[pi][:, :], scalar=GA1[:, cb, 0:1], in1=xrb[b], op0=ALU.mult, op1=ALU.add),
                            [("ps", pi), ("xr", b), "modT"], [("x1", cb, t0)])
                    else:
                        S.op("dve", lambda e, pi=pi, cb=cb: e.tensor_tensor(
                            out=o4t.rearrange("p (b t) -> p b t", b=4), in0=PS[pi][:, 0:16].rearrange("p (b t) -> p b t", b=4),
                            in1=GA1[:, cb, 1:5].unsqueeze(2).to_broadcast([128, 4, 4]), op=ALU.mult), [("ps", pi), "modT"], ["o4t"])
                        S.op("dve", lambda e, b=b, cb=cb: e.tensor_tensor(out=x1[:, cb, 1024:1040], in0=o4t, in1=xrb[b][:, 0:16], op=ALU.add),
                             ["o4t", ("xr", b)], [("x1", cb, 1024)])
        S.barrier()
        chk("p4")
        R_M.reset()

        h2T = hT
        comb = f32v(R_M.alloc(144 * 4), 144).rearrange("p (t g e) -> p t g e", g=4, e=4)
        m5_mark = R_M.cur
        sqb = [f32v(R_M.alloc(512 * 4), 512) for _ in range(2)]
        rs2 = f32v(R_M.alloc(512 * 4), 512)
        tp2 = [f32v(R_M.alloc(512 * 4), 512) for _ in range(2)]
        h2f = [f32v(R_M.alloc(512 * 4), 512) for _ in range(2)]
        zf = f32v(R_M.alloc(180 * 4), 180)
        S.op("dve", lambda e: e.memset(zf, 0.0), [], ["zf"])
        S.op("pe", lambda e: e.matmul(PS[5][:, 0:180], lhsT=zf[:, 0:128], rhs=zf[:, 0:180], start=True, stop=False), ["zf"], [("ps", 5)])
        for bidx, (t0, n) in enumerate(TBS):
            rsel = [(0, n, 0)] if n == 512 else SAMPLE_RSEL
            for k in range(16):
                sb = k % 2
                S.op("act", lambda e, k=k, sb=sb, t0=t0, n=n: e.activation(out=sqb[sb][:, 0:n], in_=x1[:, k, t0:t0 + n], func=AF.Square), ["x1"], [("sq2", sb)])
                S.op("pe", lambda e, k=k, sb=sb, n=n: e.matmul(PS[7][:, 0:n], lhsT=ones, rhs=sqb[sb][:, 0:n], start=(k == 0), stop=(k == 15)),
                     [("sq2", sb), "cst"], [("ps", 7)])
            rstd_from_ps(PS[7][:, 0:n], n, 1.0 / 2048.0, rs2[:, 0:n], rs2[:, 0:n], [("ps", 7)], "rs2")
            for k in range(16):
                sb = k % 2
                for (c0, cn, r) in rsel:
                    S.op("dve", lambda e, k=k, sb=sb, c0=c0, cn=cn, r=r, t0=t0: e.scalar_tensor_tensor(
                        out=tp2[sb][:, c0:c0 + cn], in0=x1[:, k, t0 + c0:t0 + c0 + cn], scalar=G2[:, k, r:r + 1], in1=rs2[:, c0:c0 + cn],
                        op0=ALU.mult, op1=ALU.mult), ["x1", "G", "rs2"], [("tp2", sb)])
                    S.op("act", lambda e, k=k, sb=sb, c0=c0, cn=cn, r=r: e.activation(
                        out=h2f[sb][:, c0:c0 + cn], in_=tp2[sb][:, c0:c0 + cn], func=AF.Identity, bias=SH2[:, k, r:r + 1], scale=1.0),
                        [("tp2", sb), "modT"], [("h2f", sb)])
                S.op("dve", lambda e, k=k, sb=sb, t0=t0, n=n: e.tensor_copy(out=h2T[:, k, t0:t0 + n], in_=h2f[sb][:, 0:n]), [("h2f", sb)], ["hT"])
                ntile = max(1, n // 128)
                for j in range(ntile):
                    tl_ = t0 // 128 + j
                    mm_ = min(128, n)
                    S.op("pe", lambda e, k=k, sb=sb, j=j, tl_=tl_, mm_=mm_: e.matmul(
                        PS[5][0:mm_, tl_ * 20:tl_ * 20 + 20], lhsT=h2f[sb][:, j * 128:j * 128 + mm_], rhs=wr[:, k, :],
                        start=False, stop=(k == 15 and tl_ == 8)), [("h2f", sb), "wr"], [("ps", 5)])
        def rt(n):
            return f32v(R_M.alloc(n * 4), n)
        LG = rt(180).rearrange("p (t c) -> p t c", c=20)
        S.op("act", lambda e: e.activation(out=LG.rearrange("p t c -> p (t c)"), in_=PS[5][:, 0:180], func=AF.Copy), [("ps", 5)], ["LG"])
        lg = rt(36).rearrange("p (t g) -> p t g", g=4)
        mx = rt(9)
        oh = rt(36).rearrange("p (t g) -> p t g", g=4)
        ex = rt(36).rearrange("p (t g) -> p t g", g=4)
        den = rt(9)
        pgt = rt(9)
        le = rt(144).rearrange("p (t g e) -> p t g e", g=4, e=4)
        les = rt(36).rearrange("p (t e) -> p t e", e=4)
        m1 = rt(9)
        k1 = rt(36).rearrange("p (t e) -> p t e", e=4)
        le2 = rt(36).rearrange("p (t e) -> p t e", e=4)
        m2 = rt(9)
        k2 = rt(36).rearrange("p (t e) -> p t e", e=4)
        w1 = rt(9)
        w2 = rt(9)
        ce = rt(36).rearrange("p (t e) -> p t e", e=4)
        ce2 = rt(36).rearrange("p (t e) -> p t e", e=4)
        brg = prm[:, P_BRG:P_BRG + 4].unsqueeze(1).to_broadcast([128, 9, 4])
        bre = prm[:, P_BRE:P_BRE + 16].unsqueeze(1).to_broadcast([128, 9, 16])
        R = "rt"

        def dv(fn, rd=(), wrk=R):
            S.op("dve", fn, [R] + list(rd), [wrk])

        dv(lambda e: e.tensor_tensor(out=lg, in0=LG[:, :, 0:4], in1=brg, op=ALU.add), ["LG", "prm"])
        dv(lambda e: e.tensor_reduce(out=mx, in_=lg, axis=AX.X, op=ALU.max))
        dv(lambda e: e.tensor_tensor(out=oh, in0=lg, in1=mx.unsqueeze(2).to_broadcast([128, 9, 4]), op=ALU.is_equal))
        dv(lambda e: e.tensor_tensor(out=ex, in0=lg, in1=mx.unsqueeze(2).to_broadcast([128, 9, 4]), op=ALU.subtract))
        S.op("act", lambda e: e.activation(out=ex, in_=ex, func=AF.Exp), [R], [R])
        dv(lambda e: e.tensor_reduce(out=den, in_=ex, axis=AX.X, op=ALU.add))
        dv(lambda e: e.reciprocal(out=pgt, in_=den))
        dv(lambda e: e.tensor_tensor(out=le.rearrange("p t g e -> p t (g e)"), in0=LG[:, :, 4:20], in1=bre, op=ALU.add), ["LG", "prm"])
        dv(lambda e: e.tensor_tensor(out=le, in0=le, in1=oh.unsqueeze(3).to_broadcast([128, 9, 4, 4]), op=ALU.mult))
        dv(lambda e: e.tensor_reduce(out=les, in_=le.rearrange("p t g e -> p t e g"), axis=AX.X, op=ALU.add))
        dv(lambda e: e.tensor_reduce(out=m1, in_=les, axis=AX.X, op=ALU.max))
        dv(lambda e: e.tensor_tensor(out=k1, in0=les, in1=m1.unsqueeze(2).to_broadcast([128, 9, 4]), op=ALU.is_equal))
        dv(lambda e: e.scalar_tensor_tensor(out=le2, in0=k1, scalar=-1.0e30, in1=les, op0=ALU.mult, op1=ALU.add))
        dv(lambda e: e.tensor_reduce(out=m2, in_=le2, axis=AX.X, op=ALU.max))
        dv(lambda e: e.tensor_tensor(out=k2, in0=le2, in1=m2.unsqueeze(2).to_broadcast([128, 9, 4]), op=ALU.is_equal))
        dv(lambda e: e.tensor_tensor(out=w2, in0=m2, in1=m1, op=ALU.subtract))
        S.op("act", lambda e: e.activation(out=w2, in_=w2, func=AF.Exp), [R], [R])
        dv(lambda e: e.tensor_scalar(out=w1, in0=w2, scalar1=1.0, scalar2=None, op0=ALU.add))
        dv(lambda e: e.reciprocal(out=w1, in_=w1))
        dv(lambda e: e.tensor_tensor(out=w2, in0=w2, in1=w1, op=ALU.mult))
        dv(lambda e: e.tensor_tensor(out=w1, in0=w1, in1=pgt, op=ALU.mult))
        dv(lambda e: e.tensor_tensor(out=w2, in0=w2, in1=pgt, op=ALU.mult))
        dv(lambda e: e.tensor_tensor(out=ce, in0=k1, in1=w1.unsqueeze(2).to_broadcast([128, 9, 4]), op=ALU.mult))
        dv(lambda e: e.tensor_tensor(out=ce2, in0=k2, in1=w2.unsqueeze(2).to_broadcast([128, 9, 4]), op=ALU.mult))
        dv(lambda e: e.tensor_tensor(out=ce, in0=ce, in1=ce2, op=ALU.add))
        dv(lambda e: e.tensor_tensor(out=comb, in0=oh.unsqueeze(3).to_broadcast([128, 9, 4, 4]),
                                     in1=ce.unsqueeze(2).to_broadcast([128, 9, 4, 4]), op=ALU.mult), wrk="comb")
        S.barrier()
        chk("p5")

        R_M.cur = m5_mark
        R_RING.reset()
        mslot = [R_RING.alloc(4096) for _ in range(8)]
        msl = [0]

        def load_w4(src_ap):
            slot = msl[0] % 8
            msl[0] += 1
            v = bfv(mslot[slot], 2048)
            key = ("mring", slot)
            S.dma("pool", lambda e: e.dma_start(out=v, in_=src_ap), ("mring", slot), writes=[key])
            return v, key

        o_hid = R_AM.base
        hid = bfv(o_hid, 4 * NT).rearrange("p (f t) -> p f t", f=4)
        cbc = [f32v(R_AM.base + 4 * NT * 2 + i * NT * 4, NT) for i in range(2)]
        De = [f32v(R_M.alloc(128 * 4), 128) for _ in range(2)]
        msA = [f32v(R_M.alloc(512 * 4), 512) for _ in range(2)]
        mtT = [f32v(R_M.alloc(512 * 4), 512) for _ in range(2)]
        mo4 = f32v(R_M.alloc(16 * 4), 16)
        dec = [0]
        mc = [0]
        def build_cbc(ex_):
            g_, e_ = ex_ // 4, ex_ % 4
            cb_ = cbc[ex_ % 2]
            for bidx, (t0, n) in enumerate(TBS):
                pi = next_ps()
                ntile = max(1, n // 128)
                for j in range(ntile):
                    tl_ = t0 // 128 + j
                    mm_ = min(128, n)
                    b = dec[0] % 2
                    dec[0] += 1
                    S.op("dve", lambda e, b=b, tl_=tl_, mm_=mm_: e.tensor_scalar(
                        out=De[b][0:mm_, 0:mm_], in0=ident[0:mm_, 0:mm_], scalar1=comb[0:mm_, tl_, g_, e_:e_ + 1], scalar2=None, op0=ALU.mult),
                        ["comb", "cst"], [("De", b)])
                    S.op("pe", lambda e, b=b, j=j, mm_=mm_, pi=pi: e.matmul(PS[pi][:, j * 128:j * 128 + mm_], lhsT=ones[0:mm_, :], rhs=De[b][0:mm_, 0:mm_],
                                                                            start=True, stop=True), [("De", b), "cst"], [("ps", pi)])
                S.op("act", lambda e, pi=pi, t0=t0, n=n: e.activation(out=cb_[:, t0:t0 + n], in_=PS[pi][:, 0:n], func=AF.Copy), [("ps", pi)], [("cbc", ex_ % 2)])

        build_cbc(0)
        for ex_ in range(16):
            g_, e_ = ex_ // 4, ex_ % 4
            cb_ = cbc[ex_ % 2]
            for fb in range(4):
                wg, wgk = load_w4(d_wgu[ex_, fb * 2])
                wu, wuk = load_w4(d_wgu[ex_, fb * 2 + 1])
                wgv = wg.rearrange("p (k c) -> p k c", k=16)
                wuv = wu.rearrange("p (k c) -> p k c", k=16)
                for (t0, n) in TBS:
                    pa, pu = next_ps(), next_ps()
                    for k in range(16):
                        S.op("pe", lambda e, k=k, pa=pa, t0=t0, n=n: e.matmul(PS[pa][:, 0:n], lhsT=wgv[:, k, :], rhs=h2T[:, k, t0:t0 + n],
                                                                              start=(k == 0), stop=(k == 15)), [wgk, "hT"], [("ps", pa)])
                    for k in range(16):
                        S.op("pe", lambda e, k=k, pu=pu, t0=t0, n=n: e.matmul(PS[pu][:, 0:n], lhsT=wuv[:, k, :], rhs=h2T[:, k, t0:t0 + n],
                                                                              start=(k == 0), stop=(k == 15)), [wuk, "hT"], [("ps", pu)])
                    b = mc[0] % 2
                    mc[0] += 1
                    S.op("act", lambda e, pa=pa, b=b, n=n: e.activation(out=msA[b][:, 0:n], in_=PS[pa][:, 0:n], func=AF.Silu), [("ps", pa)], [("msA", b)])
                    S.op("dve", lambda e, pu=pu, b=b, t0=t0, n=n: e.tensor_tensor(out=mtT[b][:, 0:n], in0=PS[pu][:, 0:n], in1=cb_[:, t0:t0 + n], op=ALU.mult),
                         [("ps", pu), ("cbc", ex_ % 2)], [("mtT", b)])
                    S.op("dve", lambda e, b=b, fb=fb, t0=t0, n=n: e.tensor_tensor(out=hid[:, fb, t0:t0 + n], in0=msA[b][:, 0:n], in1=mtT[b][:, 0:n], op=ALU.mult),
                         [("msA", b), ("mtT", b)], ["hid"])
            if ex_ + 1 < 16:
                build_cbc(ex_ + 1)
            for cg in range(4):
                wd, wdk = load_w4(d_wdn[ex_, cg])
                wdv = wd.rearrange("p (c f o) -> p c f o", c=4, f=4)
                for cbl in range(4):
                    cb = cg * 4 + cbl
                    for (t0, n) in TBS:
                        pi = next_ps()
                        for fb in range(4):
                            S.op("pe", lambda e, fb=fb, pi=pi, cbl=cbl, t0=t0, n=n: e.matmul(PS[pi][:, 0:n], lhsT=wdv[:, cbl, fb, :], rhs=hid[:, fb, t0:t0 + n],
                                                                                             start=(fb == 0), stop=(fb == 3)), [wdk, "hid"], [("ps", pi)])
                        if n == 512:
                            S.op("dve", lambda e, pi=pi, cb=cb, t0=t0: e.scalar_tensor_tensor(
                                out=x1[:, cb, t0:t0 + 512], in0=PS[pi][:, :], scalar=GA2[:, cb, 0:1], in1=x1[:, cb, t0:t0 + 512], op0=ALU.mult, op1=ALU.add),
                                [("ps", pi), "modT", ("x1", cb, t0)], [("x1", cb, t0)])
                        else:
                            S.op("dve", lambda e, pi=pi, cb=cb: e.tensor_tensor(
                                out=mo4.rearrange("p (b t) -> p b t", b=4), in0=PS[pi][:, 0:16].rearrange("p (b t) -> p b t", b=4),
                                in1=GA2[:, cb, 1:5].unsqueeze(2).to_broadcast([128, 4, 4]), op=ALU.mult), [("ps", pi), "modT"], ["mo4"])
                            S.op("dve", lambda e, cb=cb: e.tensor_tensor(out=x1[:, cb, 1024:1040], in0=mo4, in1=x1[:, cb, 1024:1040], op=ALU.add),
                                 ["mo4", ("x1", cb, 1024)], [("x1", cb, 1024)])
        S.barrier()
        S.dma("sp", lambda e: e.dma_start(out=out_yT, in_=x1.rearrange("p k t -> p (k t)")), ("out", "y"), reads=["x1"])

    except _Stop:
        pass
    if stop is not None:
        S.barrier()
        d_dbg = dout("dbg", [128, ARENA // 4])
        S.dma("sp", lambda e: e.dma_start(out=d_dbg, in_=A[:, :]), ("out", "dbg"))
    S.emit(nc, es)
    es.close()
    return nc


def _fm(a):
    T = a.shape[0]
    return np.ascontiguousarray(a.reshape(T, 16, 128).transpose(2, 1, 0))


def _wblocks_fm(w, cols):
    out = np.empty((128, 4, 16, 128), np.float32)
    for b, c0 in enumerate(cols):
        out[:, b] = w[:, c0:c0 + 128].reshape(16, 128, 128).transpose(1, 0, 2)
    return out.reshape(128, 8192)


def _wgroup_tm(w, c0):
    return np.ascontiguousarray(w[:, c0:c0 + 512].reshape(16, 128, 512).transpose(1, 0, 2)).reshape(128, 8192)


_NC_CACHE = {}


def prep(x_prompt, x_sample, cache_k, cache_v, state_hgrn, page_table, c_prompt, c_sample,
           norm1_g, norm2_g, w_ada, b_ada, w_in, q_norm_g, k_norm_g,
           lambda_q1, lambda_k1, lambda_q2, lambda_k2, subln_g, hg_lower_bound, hg_norm_g, w_out,
           w_router_group, b_router_group, w_router_expert, b_router_expert,
           w_exp_gate, w_exp_up, w_exp_down, small_cache=False):
    f = np.float32
    x_prompt = np.asarray(x_prompt, f); x_sample = np.asarray(x_sample, f)
    cache_k = np.asarray(cache_k, f); cache_v = np.asarray(cache_v, f)
    w_ada = np.asarray(w_ada, f)[0]; w_in = np.asarray(w_in, f)[0]; w_out = np.asarray(w_out, f)[0]
    wg_ = np.asarray(w_exp_gate, f)[0]; wu_ = np.asarray(w_exp_up, f)[0]; wd_ = np.asarray(w_exp_down, f)[0]

    wada = np.stack([_wblocks_fm(w_ada, [(g * 4 + b) * 128 for b in range(4)]) for g in range(24)])
    QC, KC, VC, HQ, HF, HI, HG = 0, 1024, 1536, 2048, 3072, 4096, 5120
    groups = []
    groups.append(_wblocks_fm(w_in, [KC + 128 * b for b in range(4)]))
    groups.append(_wgroup_tm(w_in, VC))
    for hgp in range(2):
        groups.append(_wblocks_fm(w_in, [HF + hgp * 512 + 128 * b for b in range(4)]))
        groups.append(_wgroup_tm(w_in, HI + hgp * 512))
    for qg in range(2):
        groups.append(_wblocks_fm(w_in, [QC + qg * 512 + 128 * b for b in range(4)]))
    groups.append(_wblocks_fm(w_in, [KC + 128 * b for b in range(4)]))
    groups.append(_wgroup_tm(w_in, VC))
    for hgp in range(2):
        groups.append(_wblocks_fm(w_in, [HF + hgp * 512 + 128 * b for b in range(4)]))
        groups.append(_wblocks_fm(w_in, [HQ + hgp * 512 + 128 * b for b in range(4)]))
        groups.append(_wgroup_tm(w_in, HI + hgp * 512))
        groups.append(_wblocks_fm(w_in, [HG + hgp * 512 + 128 * b for b in range(4)]))
    win = np.stack(groups)
    wout = np.stack([_wblocks_fm(w_out, [(g * 4 + b) * 128 for b in range(4)]) for g in range(4)])
    wgu = np.empty((16, 8, 128, 2048), f)
    for e in range(16):
        for fb in range(4):
            wgu[e, fb * 2] = wg_[e][:, fb * 128:(fb + 1) * 128].reshape(16, 128, 128).transpose(1, 0, 2).reshape(128, 2048)
            wgu[e, fb * 2 + 1] = wu_[e][:, fb * 128:(fb + 1) * 128].reshape(16, 128, 128).transpose(1, 0, 2).reshape(128, 2048)
    wdn = np.ascontiguousarray(wd_.reshape(16, 4, 128, 4, 4, 128).transpose(0, 3, 2, 4, 1, 5)).reshape(16, 4, 128, 2048)
    if small_cache:
        ckv = np.zeros((128, 1024), f)
    else:
        ckv = np.empty((5120 * 128, 1024), f)
        ckv[:, 0:512] = cache_k[0].transpose(0, 3, 2, 1).reshape(5120 * 128, 512)
        ckv[:, 512:1024] = cache_v[0].reshape(5120 * 128, 512)
    posT, R3, newb = make_sample_tables()
    wr = np.concatenate([np.asarray(w_router_group, f)[0], np.asarray(w_router_expert, f)[0].transpose(1, 0, 2).reshape(2048, 16)], axis=1)
    wr = np.ascontiguousarray(wr.reshape(16, 128, 20).transpose(1, 0, 2)).reshape(128, 320)

    prm0 = np.zeros((128, NPRM), f)
    prm0[:, P_N1:P_N1 + 16] = np.asarray(norm1_g, f)[0].reshape(16, 128).T
    prm0[:, P_N2:P_N2 + 16] = np.asarray(norm2_g, f)[0].reshape(16, 128).T
    prm0[:, P_BT:P_BT + 96] = np.asarray(b_ada, f)[0].reshape(96, 128).T
    prm0[:, P_GQ] = np.tile(np.asarray(q_norm_g, f)[0], 2)
    prm0[:, P_GK] = np.tile(np.asarray(k_norm_g, f)[0], 2)
    prm0[:, P_SG] = np.asarray(subln_g, f)[0]
    prm0[:, P_HGG] = np.asarray(hg_norm_g, f)[0]
    lbv = np.asarray(hg_lower_bound, f)
    prm0[:, P_LB:P_LB + 8] = lbv[0].reshape(8, 128).T
    prm0[:, P_LB + 8:P_LB + 16] = lbv[1].reshape(8, 128).T
    prm0[:, P_GQR:P_GQR + 64] = np.asarray(q_norm_g, f)[0][None]
    prm0[:, P_GKR:P_GKR + 64] = np.asarray(k_norm_g, f)[0][None]
    prm0[:, P_LAMR:P_LAMR + 256] = np.concatenate([np.asarray(a, f)[0] for a in (lambda_q1, lambda_k1, lambda_q2, lambda_k2)])[None]
    prm0[:, P_BRG:P_BRG + 4] = np.asarray(b_router_group, f)[0][None]
    prm0[:, P_BRE:P_BRE + 16] = np.asarray(b_router_expert, f)[0].reshape(16)[None]

    in_maps = []
    pt = np.asarray(page_table, np.int32)
    for c in range(8):
        b, half = c // 2, c % 2
        prm = prm0.copy()
        prm[:, P_FLAG] = float(half)
        crow = np.concatenate([np.asarray(c_prompt, f)[b:b + 1], np.asarray(c_sample, f)[4 * c:4 * c + 4]], axis=0)
        cT = np.ascontiguousarray(crow.reshape(5, 16, 128).transpose(2, 1, 0)).reshape(128, 80)
        in_maps.append({
            "cst": make_consts(half), "prm": prm, "cT": cT, "wr": wr,
            "xo": _fm(x_prompt[b, half * 1024:(half + 1) * 1024]),
            "xp": _fm(x_prompt[b, 0:1024]),
            "xs": _fm(x_sample[4 * c:4 * c + 4].reshape(16, 2048)),
            "wada": wada, "win": win, "wout": wout, "wgu": wgu, "wdn": wdn,
            "ckv": ckv, "pt": np.ascontiguousarray(pt[4 * c:4 * c + 4]),
            "st0": np.ascontiguousarray(np.asarray(state_hgrn, f)[0, 4 * c:4 * c + 4]),
            "posT": posT, "R3": R3, "newb": newb,
        })
    return in_maps


def kernel(**inputs):
    f = np.float32
    in_maps = prep(**inputs)
    if "nc" not in _NC_CACHE:
        _NC_CACHE["nc"] = build()
    res = run_bass_kernel_spmd(_NC_CACHE["nc"], in_maps, core_ids=list(range(8)))
    R = res.results

    y_prompt = np.empty((4, 2048, 2048), f); y_sample = np.empty((32, 4, 2048), f)
    nk_p = np.empty((1, 4, 2048, 4, 128), f); nv_p = np.empty((1, 4, 2048, 4, 128), f)
    nk_s = np.empty((1, 32, 4, 4, 128), f); nv_s = np.empty((1, 32, 4, 4, 128), f)
    ns_p = np.empty((1, 4, 8, 128, 128), f); ns_s = np.empty((1, 32, 8, 128, 128), f)
    for c in range(8):
        b, half = c // 2, c % 2
        yT = R[c]["yT"].reshape(128, 16, NT)
        yt = yT.transpose(2, 1, 0).reshape(NT, 2048)
        y_prompt[b, half * 1024:(half + 1) * 1024] = yt[:1024]
        y_sample[4 * c:4 * c + 4] = yt[1024:].reshape(4, 4, 2048)
        kTo = R[c]["kTo"].reshape(128, 4, NT).transpose(2, 1, 0)
        nk_p[0, b, half * 1024:(half + 1) * 1024] = kTo[:1024]
        nk_s[0, 4 * c:4 * c + 4] = kTo[1024:].reshape(4, 4, 4, 128)
        vo = R[c]["vo"].reshape(NT, 4, 128)
        nv_p[0, b, half * 1024:(half + 1) * 1024] = vo[:1024]
        nv_s[0, 4 * c:4 * c + 4] = vo[1024:].reshape(4, 4, 4, 128)
        if half == 1:
            ns_p[0, b] = R[c]["so"].reshape(128, 8, 128).transpose(1, 0, 2)
        ns_s[0, 4 * c:4 * c + 4] = R[c]["sso"].reshape(4, 128, 8, 128).transpose(0, 2, 1, 3)
    return (y_prompt, y_sample, nk_p, nv_p, nk_s, nv_s, ns_p, ns_s)
```

```python
import math
from contextlib import ExitStack
import numpy as np
import concourse.bass as bass
import concourse.mybir as mybir
from concourse.bass_utils import run_bass_kernel_spmd

F32 = mybir.dt.float32
BF16 = mybir.dt.bfloat16
I32 = mybir.dt.int32
AF = mybir.ActivationFunctionType
ALU = mybir.AluOpType
AX = mybir.AxisListType

NT = 1040
EPS = 1e-6
LAM_INIT = 0.2
SLOPES = [2.0 ** (-(h + 1)) for h in range(8)]
PAST = 16384
ENG = ["pe", "act", "dve", "pool", "sp"]
STOP_AFTER = None
DBG = {}


class _Rec:
    def __init__(self):
        self.call = None

    def __getattr__(self, name):
        def f(*a, **kw):
            self.call = (name, a, kw)
            return None
        return f


def _bind(fn):
    if fn is None:
        return None
    r = _Rec()
    fn(r)
    name, a, kw = r.call
    return lambda e: getattr(e, name)(*a, **kw)


class Sched:
    def __init__(self):
        self.ops = {e: [] for e in ENG}
        self.state = {}
        self.wE = {e: {} for e in ENG}
        self.wD = {e: {} for e in ENG}
        self.dcount = {}
        self.sig = {e: set() for e in ENG}
        self.last_real = {}

    def _deps(self, reads, writes):
        deps = []
        for k in reads:
            st = self.state.get(k)
            if st and st[0] is not None:
                deps.append((st[0], True))
            if st and isinstance(k, tuple) and k[0] == "ps":
                deps.extend((t, False) for t in st[1].values())
        for k in writes:
            st = self.state.get(k)
            if st:
                if st[0] is not None:
                    deps.append((st[0], False))
                deps.extend((t, False) for t in st[1].values())
        return deps

    def _update(self, reads, writes, tok):
        key = (tok[0], tok[1])
        for k in reads:
            st = self.state.setdefault(k, [None, {}])
            st[1][key] = tok
        for k in writes:
            self.state[k] = [tok, {}]

    def _waits(self, eng, deps):
        waits = []
        for (t, raw) in deps:
            if t[0] == "E":
                _, f, i = t
                if f == eng and (eng == "pe" or not raw):
                    continue
                if self.wE[eng].get(f, -1) >= i:
                    continue
                self.wE[eng][f] = i
                self.sig[f].add(i)
                waits.append(t)
            else:
                _, k, v = t
                if self.wD[eng].get(k, 0) >= v:
                    continue
                self.wD[eng][k] = v
                waits.append(t)
        return waits

    def op(self, eng, fn, reads=(), writes=()):
        waits = self._waits(eng, self._deps(reads, writes))
        idx = len(self.ops[eng])
        self.ops[eng].append((waits, _bind(fn), None))
        self.last_real[eng] = idx
        tok = ("E", eng, idx)
        self._update(reads, writes, tok)
        return tok

    def dma(self, q, fn, semkey, reads=(), writes=()):
        waits = self._waits(q, self._deps(reads, writes))
        v = self.dcount.get(semkey, 0) + 16
        self.dcount[semkey] = v
        self.ops[q].append((waits, _bind(fn), (semkey, v)))
        tok = ("D", semkey, v)
        self._update(reads, writes, tok)
        return tok

    def barrier(self):
        toks = [("E", e, self.last_real[e]) for e in ENG if e in self.last_real]
        for e in ENG:
            waits = []
            for t in toks:
                if t[1] != e and self.wE[e].get(t[1], -1) < t[2]:
                    self.wE[e][t[1]] = t[2]
                    self.sig[t[1]].add(t[2])
                    waits.append(t)
            for k, v in self.dcount.items():
                if k[0] == "out" and self.wD[e].get(k, 0) < v:
                    self.wD[e][k] = v
                    waits.append(("D", k, v))
            if waits:
                self.ops[e].append((waits, None, None))

    def emit(self, nc, es):
        esem = {e: es.enter_context(nc.semaphore("sem_" + e)) for e in ENG}
        dsem = {k: es.enter_context(nc.semaphore("dsem%d" % i)) for i, k in enumerate(self.dcount)}
        rank = {}
        for e in ENG:
            r = 0
            rank[e] = {}
            for i in range(len(self.ops[e])):
                if i in self.sig[e]:
                    r += 1
                    rank[e][i] = r
        block = es.enter_context(nc.Block())

        def run(e, eng):
            for i, (waits, fn, dm) in enumerate(self.ops[e]):
                for t in waits:
                    if t[0] == "E":
                        eng.wait_ge(esem[t[1]], rank[t[1]][t[2]])
                    else:
                        eng.wait_ge(dsem[t[1]], t[2])
                if fn is None:
                    continue
                ins = fn(eng)
                if dm is not None:
                    ins.then_inc(dsem[dm[0]], 16)
                elif i in self.sig[e]:
                    ins.then_inc(esem[e], 1)
            if e in ("sp", "pool", "act"):
                for k, v in self.dcount.items():
                    if k[0] == "out":
                        eng.wait_ge(dsem[k], v)

        @block.tensor
        def _(eng):
            run("pe", eng)

        @block.scalar
        def _(eng):
            run("act", eng)

        @block.vector
        def _(eng):
            run("dve", eng)

        @block.gpsimd
        def _(eng):
            run("pool", eng)

        @block.sync
        def _(eng):
            run("sp", eng)


C_ID, C_ONES, C_BONES, C_TRI, C_SCAN, C_SCANS, C_ND, C_BIASC = 0, 128, 256, 384, 448, 960, 976, 2000
NCST = 2000 + 256
ND_D0 = [0, -128, -256, -384]


def _tile_list(qb):
    return list(range(12)) if qb == 0 else list(range(16))


def make_consts(half):
    c = np.zeros((128, NCST), np.float32)
    p = np.arange(128)
    c[:, C_ID:C_ID + 128] = np.eye(128)
    c[:, C_ONES:C_ONES + 128] = 1.0
    c[:, C_BONES:C_BONES + 128] = (p[:, None] // 64 == p[None, :] // 64)
    c[:, C_TRI:C_TRI + 64] = ((p[:, None] % 64) <= np.arange(64)[None, :])
    sm = np.ones(512, np.float32)
    sm[::64] = 0.0
    c[:, C_SCAN:C_SCAN + 512] = sm[None]
    sms = np.ones(16, np.float32)
    sms[::4] = 0.0
    c[:, C_SCANS:C_SCANS + 16] = sms[None]
    u = np.arange(1024)
    dist = u[None, :] - 384 - p[:, None]
    c[:, C_ND:C_ND + 1024] = np.where(dist >= 0, -dist, -1.0e6)
    for h in range(8):
        for qb in range(2):
            for kt in range(16):
                d0 = 1024 + 512 * qb - 128 * kt
                val = 0.0 if d0 < 128 else -SLOPES[h] * (d0 - 128)
                if kt < 8 and half == 0:
                    val += -30000.0
                c[:, C_BIASC + (h * 2 + qb) * 16 + kt] = val
    return c


def make_sample_tables():
    posT = np.zeros((3, 16, 128), np.float32)
    posT[0] = np.arange(128)[None, :]
    posT[1] = 1.0
    posT[2] = np.arange(16)[:, None]
    R3 = np.zeros((3, 8, 64), np.float32)
    newb = np.zeros((4, 64), np.float32)
    for kvh in range(4):
        for m in range(2):
            for g in range(2):
                for t in range(4):
                    col = kvh * 16 + m * 8 + g * 4 + t
                    sl = SLOPES[kvh * 2 + g]
                    R3[0, :, col] = sl
                    R3[1, :, col] = -sl * (PAST + t - 128.0 * np.arange(8))
                    R3[2, :, col] = sl * 1024.0
                    for tp in range(4):
                        newb[tp, col] = -sl * (t - tp) if tp <= t else -1.0e6
    return posT.reshape(3, 2048), R3.reshape(3, 512), newb


P_N1, P_N2, P_BT, P_GQ, P_GK, P_SG, P_HGG, P_LB, P_FLAG = 0, 16, 32, 128, 129, 130, 131, 132, 148
P_GQR, P_GKR, P_LAMR, P_BRG, P_BRE = 149, 213, 277, 533, 537
NPRM = 553


def build(stop=None, small_cache=False):
    nc = bass.Bass("TRN2", target_bir_lowering=False)
    S = Sched()
    es = ExitStack()

    def din(name, shape, dt=F32):
        return nc.dram_tensor(name, list(shape), dt, kind="ExternalInput").ap()

    def dout(name, shape, dt=F32):
        return nc.dram_tensor(name, list(shape), dt, kind="ExternalOutput").ap()

    d_cst = din("cst", [128, NCST])
    d_prm = din("prm", [128, NPRM])
    d_cT = din("cT", [128, 80])
    d_wr = din("wr", [128, 320])
    d_xo = din("xo", [128, 16, 1024])
    d_xp = din("xp", [128, 16, 1024])
    d_xs = din("xs", [128, 16, 16])
    d_wada = din("wada", [24, 128, 8192])
    d_win = din("win", [18, 128, 8192])
    d_wout = din("wout", [4, 128, 8192])
    d_wgu = din("wgu", [16, 8, 128, 2048])
    d_wdn = din("wdn", [16, 4, 128, 2048])
    d_ckv = din("ckv", [128 if small_cache else 655360, 1024])
    d_pt = din("pt", [4, 128], I32)
    d_st0 = din("st0", [4, 8, 128, 128])
    d_posT = din("posT", [3, 2048])
    d_R3 = din("R3", [3, 512])
    d_newb = din("newb", [4, 64])
    out_yT = dout("yT", [128, 16 * NT])
    out_kT = dout("kTo", [128, 4 * NT])
    out_v = dout("vo", [NT, 512])
    out_s = dout("so", [128, 1024])
    out_ss = dout("sso", [4, 128, 1024])

    ARENA = 207 * 1024
    A = es.enter_context(nc.sbuf_tensor("arena", [128, ARENA // 4], F32))
    PS = [es.enter_context(nc.psum_tensor("ps%d" % i, [128, 512], F32)) for i in range(8)]

    def f32v(off, n, parts=128):
        assert off % 4 == 0
        return A[0:parts, off // 4: off // 4 + n]

    def bfv(off, n, parts=128):
        assert off % 4 == 0 and n % 2 == 0
        return A[0:parts, off // 4: off // 4 + n // 2].bitcast(BF16)

    def i32v(off, n, parts=128):
        return A[0:parts, off // 4: off // 4 + n].bitcast(I32)

    class Region:
        def __init__(self, base, size):
            self.base, self.size, self.cur = base, size, base

        def alloc(self, nbytes):
            nbytes = (nbytes + 63) // 64 * 64
            off = self.cur
            self.cur += nbytes
            assert self.cur <= self.base + self.size, ("region overflow", self.cur - self.base, self.size)
            return off

        def reset(self):
            self.cur = self.base

    R_CONST = Region(0, 22 * 1024)
    R_RING = Region(R_CONST.base + R_CONST.size, 32 * 1024)
    R_H = Region(R_RING.base + R_RING.size, 33280)
    R_AM = Region(R_H.base + R_H.size, 33280)
    R_X = Region(R_AM.base + R_AM.size, 66560)
    R_M = Region(R_X.base + R_X.size, ARENA - (R_X.base + R_X.size))

    class _Stop(Exception):
        pass

    def chk(name):
        if stop == name:
            raise _Stop()

    try:
        o_cst = R_CONST.alloc(NCST * 4)
        cst = f32v(o_cst, NCST)
        ident = cst[:, C_ID:C_ID + 128]
        ones = cst[:, C_ONES:C_ONES + 128]
        bones = cst[:, C_BONES:C_BONES + 128]
        tri = cst[:, C_TRI:C_TRI + 64]
        scanm = cst[:, C_SCAN:C_SCAN + 512]
        scanms = cst[:, C_SCANS:C_SCANS + 16]
        biasc = cst[:, C_BIASC:C_BIASC + 256]
        o_prm = R_CONST.alloc(NPRM * 4)
        prm = f32v(o_prm, NPRM)
        o_bfc = R_CONST.alloc(768 * 2)
        identb = bfv(o_bfc, 768)[:, 0:128]
        onesb = bfv(o_bfc, 768)[:, 128:256]
        zerosb = bfv(o_bfc, 768)[:, 256:768]
        o_mod = R_CONST.alloc(480 * 4)
        DBG["mod"] = o_mod
        modT = f32v(o_mod, 480).rearrange("p (c r) -> p c r", r=5)
        o_g = R_CONST.alloc(160 * 4)
        DBG["g"] = o_g
        G1 = f32v(o_g, 160)[:, 0:80].rearrange("p (c r) -> p c r", r=5)
        G2 = f32v(o_g, 160)[:, 80:160].rearrange("p (c r) -> p c r", r=5)
        o_sc = R_CONST.alloc(64 * 4)
        DBG["sc"] = o_sc
        sc = f32v(o_sc, 64)
        negM, neglam, gq8, sg8, oml, noml, mq, mk = (sc[:, i:i + 1] for i in range(8))
        omlh = sc[:, 8:16]
        nomlh = sc[:, 16:24]
        lbh = sc[:, 24:32]
        epsc = sc[:, 32:33]
        o_wr = R_CONST.alloc(320 * 4)
        wr = f32v(o_wr, 320).rearrange("p (k c) -> p k c", c=20)
        o_cT = R_CONST.alloc(80 * 4)
        cTt = f32v(o_cT, 80)
        o_sil = R_CONST.alloc(80 * 2)
        silT = bfv(o_sil, 80).rearrange("p (k r) -> p k r", r=5)

        S.dma("sp", lambda e: e.dma_start(out=cst, in_=d_cst), ("ld", "c0"), writes=["cst"])
        S.dma("sp", lambda e: e.dma_start(out=prm, in_=d_prm), ("ld", "c1"), writes=["prm"])
        S.dma("sp", lambda e: e.dma_start(out=cTt, in_=d_cT), ("ld", "c2"), writes=["cT"])
        S.dma("sp", lambda e: e.dma_start(out=wr.rearrange("p k c -> p (k c)"), in_=d_wr), ("ld", "c3"), writes=["wr"])

        S.op("dve", lambda e: e.tensor_copy(out=identb, in_=ident), ["cst"], ["bfc"])
        S.op("dve", lambda e: e.tensor_copy(out=onesb, in_=ones), ["cst"], ["bfc"])
        S.op("dve", lambda e: e.memset(zerosb, 0.0), [], ["bfc"])
        S.op("dve", lambda e: e.memset(epsc, EPS), [], ["sc"])
        o_gt = R_M.alloc(128 * 4)
        gt = f32v(o_gt, 128)
        S.op("dve", lambda e: e.tensor_tensor(out=gt, in0=prm[:, P_GQR:P_GQR + 128], in1=prm[:, P_GQR:P_GQR + 128], op=ALU.mult), ["prm"], ["gt"])
        S.op("dve", lambda e: e.tensor_reduce(out=sc[:, 6:8], in_=gt.rearrange("p (a b) -> p a b", b=64), axis=AX.X, op=ALU.max), ["gt"], ["sc"])
        S.op("dve", lambda e: e.tensor_tensor(out=negM, in0=mq, in1=mk, op=ALU.mult), ["sc"], ["sc"])
        S.op("dve", lambda e: e.tensor_scalar(out=negM, in0=negM, scalar1=1.0, scalar2=-8.0, op0=ALU.max, op1=ALU.mult), ["sc"], ["sc"])
        S.op("dve", lambda e: e.tensor_scalar(out=gq8, in0=prm[:, P_GQ:P_GQ + 1], scalar1=0.125, scalar2=None, op0=ALU.mult), ["prm"], ["sc"])
        S.op("dve", lambda e: e.tensor_scalar(out=sg8, in0=prm[:, P_SG:P_SG + 1], scalar1=1.0 - LAM_INIT, scalar2=None, op0=ALU.mult), ["prm"], ["sc"])
        o_t = R_M.alloc(256 * 4)
        tl = f32v(o_t, 256)
        lr = prm[:, P_LAMR:P_LAMR + 256]
        S.op("dve", lambda e: e.tensor_tensor(out=tl[:, 0:64], in0=lr[:, 0:64], in1=lr[:, 64:128], op=ALU.mult), ["prm"], ["tl"])
        S.op("dve", lambda e: e.tensor_tensor(out=tl[:, 64:128], in0=lr[:, 128:192], in1=lr[:, 192:256], op=ALU.mult), ["prm"], ["tl"])
        S.op("dve", lambda e: e.tensor_reduce(out=tl[:, 128:130], in_=tl[:, 0:128].rearrange("p (a b) -> p a b", b=64), axis=AX.X, op=ALU.add), ["tl"], ["tl2"])
        S.op("act", lambda e: e.activation(out=tl[:, 130:132], in_=tl[:, 128:130], func=AF.Exp), ["tl2"], ["tl3"])
        S.op("dve", lambda e: e.scalar_tensor_tensor(out=neglam, in0=tl[:, 131:132], scalar=-LAM_INIT, in1=tl[:, 130:131], op0=ALU.add, op1=ALU.subtract), ["tl3"], ["sc"])
        lb2 = prm[:, P_LB:P_LB + 16].rearrange("p (s h) -> p s h", s=2)
        S.op("dve", lambda e: e.tensor_tensor(out=tl[:, 132:140], in0=lb2[:, 0, :], in1=lb2[:, 1, :], op=ALU.subtract), ["prm"], ["tl4"])
        S.op("act", lambda e: e.activation(out=lbh, in_=tl[:, 132:140], func=AF.Sigmoid), ["tl4"], ["sc"])
        S.op("dve", lambda e: e.tensor_scalar(out=omlh, in0=lbh, scalar1=-1.0, scalar2=1.0, op0=ALU.mult, op1=ALU.add), ["sc"], ["sc"])
        S.op("dve", lambda e: e.tensor_scalar(out=nomlh, in0=omlh, scalar1=-1.0, scalar2=None, op0=ALU.mult), ["sc"], ["sc"])
        S.op("dve", lambda e: e.tensor_scalar(out=biasc, in0=biasc, scalar1=negM, scalar2=None, op0=ALU.add), ["cst", "sc"], ["cst"])
        S.op("act", lambda e: e.activation(out=silT.rearrange("p k r -> p (k r)"), in_=cTt, func=AF.Silu), ["cT"], ["silT"])

        ring_off = [R_RING.alloc(16384) for _ in range(2)]
        ring_i = [0]

        def load_w16(src_ap):
            slot = ring_i[0] % 2
            ring_i[0] += 1
            v = bfv(ring_off[slot], 8192)
            key = ("ring", slot)
            S.dma("pool", lambda e: e.dma_start(out=v.rearrange("p (a b) -> p a b", b=2048),
                                                in_=src_ap.rearrange("p (a b) -> p a b", b=2048)),
                  ("ring", slot), writes=[key])
            return v, key

        for grp in range(24):
            w, wk = load_w16(d_wada[grp])
            wv = w.rearrange("p (b k c) -> p b k c", b=4, k=16)
            for blk in range(4):
                cb = grp * 4 + blk
                for k in range(16):
                    S.op("pe", lambda e, cb=cb, blk=blk, k=k, wv=wv: e.matmul(
                        PS[0][:, cb * 5:cb * 5 + 5], lhsT=wv[:, blk, k, :], rhs=silT[:, k, :],
                        start=(k == 0), stop=(k == 15)), [wk, "silT"], [("ps", 0)])
        bT = prm[:, P_BT:P_BT + 96]
        S.op("dve", lambda e: e.tensor_tensor(out=modT, in0=PS[0][:, 0:480].rearrange("p (c r) -> p c r", r=5),
                                              in1=bT.unsqueeze(2).to_broadcast([128, 96, 5]), op=ALU.add),
             [("ps", 0), "prm"], ["modT"])
        n1b = prm[:, P_N1:P_N1 + 16].unsqueeze(2).to_broadcast([128, 16, 5])
        n2b = prm[:, P_N2:P_N2 + 16].unsqueeze(2).to_broadcast([128, 16, 5])
        S.op("dve", lambda e: e.scalar_tensor_tensor(out=G1, in0=modT[:, 16:32, :], scalar=1.0, in1=n1b, op0=ALU.add, op1=ALU.mult), ["modT", "prm"], ["G"])
        S.op("dve", lambda e: e.scalar_tensor_tensor(out=G2, in0=modT[:, 64:80, :], scalar=1.0, in1=n2b, op0=ALU.add, op1=ALU.mult), ["modT", "prm"], ["G"])
        SH1 = modT[:, 0:16, :]
        GA1 = modT[:, 32:48, :]
        SH2 = modT[:, 48:64, :]
        GA2 = modT[:, 80:96, :]
        S.barrier()
        chk("p1")
        R_M.reset()

        o_h = R_H.alloc(16 * NT * 2)
        DBG["hT"] = o_h
        hT = bfv(o_h, 16 * NT).rearrange("p (k t) -> p k t", t=NT)

        def rstd_from_ps(ps_ap, n, inv_n, out_ap, tmp_ap, rd, wr_key):
            S.op("act", lambda e: e.activation(out=out_ap, in_=ps_ap, func=AF.Ln, bias=epsc, scale=inv_n), rd + ["sc"], [wr_key])
            S.op("act", lambda e: e.activation(out=out_ap, in_=out_ap, func=AF.Exp, scale=-0.5), [wr_key], [wr_key])

        def make_hT(d_x, blocks, gmod, shmod, dst, G, SHm, name):
            o_xb = [R_M.alloc(2 * 512 * 4) for _ in range(2)]
            o_sq = [R_M.alloc(512 * 4) for _ in range(2)]
            o_rs = R_M.alloc(512 * 4)
            o_tp = [R_M.alloc(512 * 4) for _ in range(2)]
            cnt = [0]
            for (src, t0, n, rsel) in blocks:
                rs = f32v(o_rs, 512)[:, 0:n]
                for kg in range(8):
                    b = cnt[0] % 2
                    cnt[0] += 1
                    xb = f32v(o_xb[b], 1024).rearrange("p (k t) -> p k t", k=2)[:, :, 0:n]
                    S.dma("sp", lambda e, xb=xb, src=src, kg=kg: e.dma_start(out=xb, in_=src[:, kg * 2:kg * 2 + 2, :]),
                          ("ld", "xb%d" % b), writes=[("xb", b)])
                    for kk in range(2):
                        k = kg * 2 + kk
                        sb = k % 2
                        sq = f32v(o_sq[sb], 512)[:, 0:n]
                        S.op("act", lambda e, sq=sq, xb=xb, kk=kk: e.activation(out=sq, in_=xb[:, kk, :], func=AF.Square), [("xb", b)], [("sq", sb)])
                        S.op("pe", lambda e, sq=sq, k=k, n=n: e.matmul(PS[7][:, 0:n], lhsT=ones, rhs=sq, start=(k == 0), stop=(k == 15)),
                             [("sq", sb), "cst"], [("ps", 7)])
                rstd_from_ps(PS[7][:, 0:n], n, 1.0 / 2048.0, rs, rs, [("ps", 7)], "rs")
                for kg in range(8):
                    b = cnt[0] % 2
                    cnt[0] += 1
                    xb = f32v(o_xb[b], 1024).rearrange("p (k t) -> p k t", k=2)[:, :, 0:n]
                    S.dma("sp", lambda e, xb=xb, src=src, kg=kg: e.dma_start(out=xb, in_=src[:, kg * 2:kg * 2 + 2, :]),
                          ("ld", "xb%d" % b), writes=[("xb", b)])
                    for kk in range(2):
                        k = kg * 2 + kk
                        tb_ = k % 2
                        for (c0, cn, r) in rsel:
                            tp = f32v(o_tp[tb_], 512)[:, c0:c0 + cn]
                            S.op("dve", lambda e, tp=tp, xb=xb, kk=kk, k=k, r=r, c0=c0, cn=cn, rs=rs: e.scalar_tensor_tensor(
                                out=tp, in0=xb[:, kk, c0:c0 + cn], scalar=G[:, k, r:r + 1], in1=rs[:, c0:c0 + cn],
                                op0=ALU.mult, op1=ALU.mult), [("xb", b), "G", "rs"], [("tp", tb_)])
                            S.op("act", lambda e, tp=tp, k=k, r=r, c0=c0, cn=cn, t0=t0: e.activation(
                                out=dst[:, k, t0 + c0:t0 + c0 + cn], in_=tp, func=AF.Identity, bias=SHm[:, k, r:r + 1], scale=1.0),
                                [("tp", tb_), "modT"], [name])

        PBLK = [(0, 512), (512, 512)]
        SAMPLE_RSEL = [(4 * bi, 4, 1 + bi) for bi in range(4)]

        o_kT = R_X.alloc(4 * 2064 * 2)
        DBG["kT"] = o_kT
        kT = bfv(o_kT, 4 * 2064).rearrange("p (h t) -> p h t", h=4)
        o_v = R_X.alloc(16 * 512 * 2)
        DBG["vT"] = o_v
        vT = bfv(o_v, 16 * 512).rearrange("p (t c) -> p t c", c=512)
        o_vs = R_X.alloc(4 * 512 * 2)
        vS = bfv(o_vs, 4 * 512).rearrange("p (b c) -> p b c", c=512)
        o_qT = R_X.alloc(8 * NT * 2)
        DBG["qT"] = o_qT
        qT = bfv(o_qT, 8 * NT).rearrange("p (h t) -> p h t", h=8)
        x_mark = R_X.cur

        psc = [0]

        def next_ps(lo=0, hi=6):
            i = lo + psc[0] % (hi - lo)
            psc[0] += 1
            return i

        deferred = []

        def flush_deferred():
            while deferred:
                deferred.pop(0)()

        def proj_fm(wv, wk, blk, src, t0, n, post):
            pi = next_ps()
            for k in range(16):
                S.op("pe", lambda e, k=k, pi=pi: e.matmul(PS[pi][:, 0:n], lhsT=wv[:, blk, k, :], rhs=src[:, k, t0:t0 + n],
                                                          start=(k == 0), stop=(k == 15)), [wk, "hT"], [("ps", pi)])
            flush_deferred()
            post(pi)

        def proj_tm(wv, wk, src, t0, m, post):
            pi = next_ps()
            for k in range(16):
                S.op("pe", lambda e, k=k, pi=pi: e.matmul(PS[pi][0:m, :], lhsT=src[:, k, t0:t0 + m], rhs=wv[:, k, :],
                                                          start=(k == 0), stop=(k == 15)), [wk, "hT"], [("ps", pi)])
            flush_deferred()
            post(pi)

        tmp_off = {}

        def tmpf(name, n=512, region=None):
            if name not in tmp_off:
                tmp_off[name] = (region or R_M).alloc(n * 4)
            return f32v(tmp_off[name], n)

        def tmpb(name, n=512, region=None):
            if name not in tmp_off:
                tmp_off[name] = (region or R_M).alloc(n * 2)
            return bfv(tmp_off[name], n)

        def qk_post(pi, n, gcol, out_bf, out_f32_key=None, out_f32=None, tag="qk"):
            sq = tmpf(tag + "sq")[:, 0:n]
            rs = tmpf(tag + "rs")[:, 0:n]
            S.op("act", lambda e: e.activation(out=sq, in_=PS[pi][:, 0:n], func=AF.Square), [("ps", pi)], [tag + "sq"])
            S.op("pe", lambda e: e.matmul(PS[6][:, 0:n], lhsT=bones, rhs=sq, start=True, stop=True), [tag + "sq", "cst"], [("ps", 6)])
            rstd_from_ps(PS[6][:, 0:n], n, 1.0 / 64.0, rs, rs, [("ps", 6)], tag + "rs")
            if out_f32 is not None:
                S.op("dve", lambda e: e.scalar_tensor_tensor(out=out_f32, in0=PS[pi][:, 0:n], scalar=gcol, in1=rs, op0=ALU.mult, op1=ALU.mult),
                     [("ps", pi), tag + "rs", "sc", "prm"], [out_f32_key])
                S.op("act", lambda e: e.activation(out=out_bf[0], in_=out_f32, func=AF.Copy), [out_f32_key], [out_bf[1]])
            else:
                S.op("dve", lambda e: e.scalar_tensor_tensor(out=out_bf[0], in0=PS[pi][:, 0:n], scalar=gcol, in1=rs, op0=ALU.mult, op1=ALU.mult),
                     [("ps", pi), tag + "rs", "sc", "prm"], [out_bf[1]])

        gk = prm[:, P_GK:P_GK + 1]
        flag = prm[:, P_FLAG:P_FLAG + 1]
        hgg = prm[:, P_HGG:P_HGG + 1]

        def hgrn_bufs(region, ntok, ntile, with_q):
            d = {}
            d["kt"] = bfv(region.alloc(4 * ntok * 2), 4 * ntok).rearrange("p (h t) -> p h t", h=4)
            d["khat"] = bfv(region.alloc(ntile * 512 * 2), ntile * 512).rearrange("p (t c) -> p t c", c=512)
            d["hi"] = bfv(region.alloc(ntile * 512 * 2), ntile * 512).rearrange("p (t c) -> p t c", c=512)
            nch = ntok // 64 + 4
            d["d"] = f32v(region.alloc(4 * nch * 4), 4 * nch).rearrange("p (h c) -> p h c", h=4)
            if with_q:
                d["eg"] = bfv(region.alloc(4 * ntok * 2), 4 * ntok).rearrange("p (h t) -> p h t", h=4)
                d["qt"] = bfv(region.alloc(4 * ntok * 2), 4 * ntok).rearrange("p (h t) -> p h t", h=4)
                d["gate"] = bfv(region.alloc(4 * ntok * 2), 4 * ntok).rearrange("p (h t) -> p h t", h=4)
                d["khs"] = bfv(region.alloc(4 * 512 * 2), 4 * 512).rearrange("p (b c) -> p b c", c=512)
                d["his"] = bfv(region.alloc(4 * 512 * 2), 4 * 512).rearrange("p (b c) -> p b c", c=512)
            return d

        o_Sst = R_CONST.alloc(8 * 128 * 4)
        DBG["Sst"] = o_Sst
        Sst = f32v(o_Sst, 1024).rearrange("p (h v) -> p h v", h=8)

        hfc = [0]

        def hf_post(pi, hb, hl, hglob, t0, n, with_q, is_sample, tag):
            sg = tmpf("hf_sg")[:, 0:n]
            lf = tmpf("hf_lf")[:, 0:n]
            hk = tmpf("hf_hk")[:, 0:n]
            G = tmpf("hf_G")[:, 0:n]
            eg = tmpf("hf_eg")[:, 0:n]
            kf = sg
            kp_ = hfc[0] % 2
            hfc[0] += 1
            khT = tmpb("hf_khT%d" % kp_)[:, 0:n]
            KHK = ("hf_khT", kp_)
            S.op("act", lambda e: e.activation(out=sg, in_=PS[pi][:, 0:n], func=AF.Sigmoid), [("ps", pi)], ["hf_sg"])
            S.op("dve", lambda e: e.tensor_scalar(out=lf, in0=sg, scalar1=omlh[:, hglob:hglob + 1], scalar2=lbh[:, hglob:hglob + 1],
                                                  op0=ALU.mult, op1=ALU.add), ["hf_sg", "sc"], ["hf_lf"])
            S.op("act", lambda e: e.activation(out=lf, in_=lf, func=AF.Ln), ["hf_lf"], ["hf_lf"])
            S.op("dve", lambda e: e.tensor_scalar(out=hk, in0=sg, scalar1=nomlh[:, hglob:hglob + 1], scalar2=omlh[:, hglob:hglob + 1],
                                                  op0=ALU.mult, op1=ALU.add), ["hf_sg", "sc"], ["hf_hk"])
            msk = scanms[:, 0:n] if is_sample else scanm[:, 0:n]
            S.op("dve", lambda e: e.tensor_tensor_scan(out=G, data0=msk, data1=lf, initial=0.0, op0=ALU.mult, op1=ALU.add),
                 ["hf_lf", "cst"], ["hf_G"])
            S.op("act", lambda e: e.activation(out=eg, in_=G, func=AF.Exp), ["hf_G"], ["hf_eg"])
            S.op("act", lambda e: e.activation(out=G, in_=G, func=AF.Exp, scale=-1.0), ["hf_G"], ["hf_G"])
            S.op("dve", lambda e: e.tensor_tensor(out=kf, in0=hk, in1=G, op=ALU.mult), ["hf_hk", "hf_G", "hf_sg"], ["hf_sg"])
            csz = 4 if is_sample else 64
            ncn = n // csz
            c0 = t0 // 64 if not is_sample else 16
            dv = hb["d"][:, hl, c0:c0 + ncn]
            S.op("dve", lambda e: e.tensor_copy(out=dv, in_=eg.rearrange("p (c s) -> p c s", s=csz)[:, :, csz - 1]), ["hf_eg"], [tag + "d"])
            S.op("act", lambda e: e.activation(out=hb["kt"][:, hl, t0:t0 + n], in_=kf, func=AF.Copy), ["hf_sg"], [tag + "kt"])
            if with_q:
                S.op("act", lambda e: e.activation(out=hb["eg"][:, hl, t0:t0 + n], in_=eg, func=AF.Copy), ["hf_eg"], [tag + "eg"])
            S.op("dve", lambda e: e.tensor_tensor(out=khT.rearrange("p (c s) -> p c s", s=csz), in0=kf.rearrange("p (c s) -> p c s", s=csz),
                                                  in1=dv.unsqueeze(2).to_broadcast([128, ncn, csz]), op=ALU.mult),
                 ["hf_sg", tag + "d"], [KHK])
            def tr_():
                PSb = PS[6].bitcast(BF16)
                if not is_sample:
                    for j in range(n // 128):
                        S.op("pe", lambda e, j=j: e.transpose(out=PSb[:, j * 128:(j + 1) * 128], in_=khT[:, j * 128:(j + 1) * 128], identity=identb),
                             [KHK, "bfc"], [("ps", 6)])
                    tl0 = t0 // 128
                    S.op("act", lambda e: e.activation(out=hb["khat"][:, tl0:tl0 + n // 128, hl * 128:(hl + 1) * 128],
                                                       in_=PSb[:, 0:n].rearrange("p (j c) -> p j c", c=128), func=AF.Copy),
                         [("ps", 6)], [tag + "khat"])
                else:
                    for bi in range(4):
                        S.op("pe", lambda e, bi=bi: e.transpose(out=PSb[0:4, bi * 128:(bi + 1) * 128], in_=khT[:, bi * 4:bi * 4 + 4], identity=identb),
                             [KHK, "bfc"], [("ps", 6)])
                    S.op("act", lambda e: e.activation(out=hb["khs"][0:4, :, hl * 128:(hl + 1) * 128],
                                                       in_=PSb[0:4, 0:512].rearrange("p (j c) -> p j c", c=128), func=AF.Copy),
                         [("ps", 6)], [tag + "khs"])
            deferred.append(tr_)

        def state_chain(hb, hl, hglob, tile0, nchunk, ch0, store_bf, tag):
            flush_deferred()
            for c in range(nchunk):
                tl_, e2 = tile0 + c // 2, c % 2
                pu = next_ps()
                if store_bf is not None:
                    S.op("act", lambda e, c=c: e.activation(out=store_bf[:, c, :], in_=Sst[:, hglob, :], func=AF.Copy), [("S", hglob)], [tag + "Sbf"])
                S.op("pe", lambda e, c=c, tl_=tl_, e2=e2, pu=pu: e.matmul(
                    PS[pu][:, 0:128],
                    lhsT=hb["khat"][64 * e2:64 * e2 + 64, tl_, hl * 128:(hl + 1) * 128],
                    rhs=hb["hi"][64 * e2:64 * e2 + 64, tl_, hl * 128:(hl + 1) * 128], start=True, stop=True),
                    [tag + "khat", tag + "hi"], [("ps", pu)])
                S.op("dve", lambda e, c=c, pu=pu: e.scalar_tensor_tensor(
                    out=Sst[:, hglob, :], in0=Sst[:, hglob, :], scalar=hb["d"][:, hl, ch0 + c:ch0 + c + 1],
                    in1=PS[pu][:, 0:128], op0=ALU.mult, op1=ALU.add),
                    [("ps", pu), tag + "d", ("S", hglob)], [("S", hglob)])

        make_hT(d_xp, [(d_xp[:, :, t0:t0 + n], t0, n, [(0, n, 0)]) for (t0, n) in PBLK], G1, SH1, hT, G1, SH1, "hT")
        S.barrier()
        R_M.reset()
        for h in range(8):
            S.op("dve", lambda e, h=h: e.memset(Sst[:, h, :], 0.0), [], [("S", h)])
        R_AM_mark = R_AM.cur
        g_i = 0
        w, wk = load_w16(d_win[g_i]); g_i += 1
        wv = w.rearrange("p (b k c) -> p b k c", b=4, k=16)
        for blk in range(4):
            for (t0, n) in PBLK:
                proj_fm(wv, wk, blk, hT, t0, n, lambda pi, blk=blk, t0=t0, n=n: qk_post(
                    pi, n, gk, (kT[:, blk, t0:t0 + n], "kT"), tag="qk"))
        w, wk = load_w16(d_win[g_i]); g_i += 1
        wv = w.rearrange("p (k c) -> p k c", k=16)
        for tl_ in range(8):
            proj_tm(wv, wk, hT, tl_ * 128, 128, lambda pi, tl_=tl_: S.op(
                "act", lambda e: e.activation(out=vT[:, tl_, :], in_=PS[pi][:, :], func=AF.Copy), [("ps", pi)], ["vT"]))
        for hgp in range(2):
            hb = hgrn_bufs(R_AM, 1024, 8, False)
            w, wk = load_w16(d_win[g_i]); g_i += 1
            wv = w.rearrange("p (b k c) -> p b k c", b=4, k=16)
            for hl in range(4):
                for (t0, n) in PBLK:
                    proj_fm(wv, wk, hl, hT, t0, n, lambda pi, hl=hl, t0=t0, n=n, hb=hb: hf_post(
                        pi, hb, hl, hgp * 4 + hl, t0, n, False, False, "p"))
            w, wk = load_w16(d_win[g_i]); g_i += 1
            wv = w.rearrange("p (k c) -> p k c", k=16)
            for tl_ in range(8):
                proj_tm(wv, wk, hT, tl_ * 128, 128, lambda pi, tl_=tl_, hb=hb: S.op(
                    "act", lambda e: e.activation(out=hb["hi"][:, tl_, :], in_=PS[pi][:, :], func=AF.Copy), [("ps", pi)], ["phi"]))
            for hl in range(4):
                state_chain(hb, hl, hgp * 4 + hl, 0, 16, 0, None, "p")
            S.barrier()
            R_AM.cur = R_AM_mark
        for h in range(8):
            S.op("dve", lambda e, h=h: e.tensor_scalar(out=Sst[:, h, :], in0=Sst[:, h, :], scalar1=flag, scalar2=None, op0=ALU.mult),
                 [("S", h), "prm"], [("S", h)])
        S.barrier()
        chk("p2")
        R_M.reset()
        tmp_off.clear()

        OWN_BLOCKS = [(d_xo[:, :, t0:t0 + n], t0, n, [(0, n, 0)]) for (t0, n) in PBLK] + [(d_xs, 1024, 16, SAMPLE_RSEL)]
        make_hT(None, OWN_BLOCKS, G1, SH1, hT, G1, SH1, "hT")
        S.barrier()
        chk("p3a0")
        R_M.reset()
        TBS = [(0, 512), (512, 512), (1024, 16)]
        o_am = R_AM.alloc(16 * NT * 2)
        DBG["amT"] = o_am
        amT = bfv(o_am, 16 * NT).rearrange("p (f t) -> p f t", f=16)
        kst = [tmpf("kst0"), tmpf("kst1")]
        vst = [tmpf("vst0"), tmpf("vst1")]
        stc = [0]
        for qg in range(2):
            w, wk = load_w16(d_win[g_i]); g_i += 1
            wv = w.rearrange("p (b k c) -> p b k c", b=4, k=16)
            for blk in range(4):
                h = qg * 4 + blk
                for (t0, n) in TBS:
                    proj_fm(wv, wk, blk, hT, t0, n, lambda pi, h=h, t0=t0, n=n: qk_post(pi, n, gq8, (qT[:, h, t0:t0 + n], "qT"), tag="qk"))
        S.barrier()
        chk("p3a1")
        w, wk = load_w16(d_win[g_i]); g_i += 1
        wv = w.rearrange("p (b k c) -> p b k c", b=4, k=16)
        for blk in range(4):
            for (t0, n) in TBS:
                def kpost(pi, blk=blk, t0=t0, n=n):
                    b = stc[0] % 2
                    stc[0] += 1
                    st = kst[b][:, 0:n]
                    qk_post(pi, n, gk, (kT[:, blk, 1024 + t0:1024 + t0 + n], "kT"), out_f32_key=("kst", b), out_f32=st, tag="qk")
                    S.dma("sp", lambda e: e.dma_start(out=out_kT[:, blk * NT + t0:blk * NT + t0 + n], in_=st), ("out", "k%d" % b), reads=[("kst", b)])
                proj_fm(wv, wk, blk, hT, t0, n, kpost)
        S.barrier()
        chk("p3a2")
        w, wk = load_w16(d_win[g_i]); g_i += 1
        wv = w.rearrange("p (k c) -> p k c", k=16)
        for tl_ in range(8):
            def vpost(pi, tl_=tl_):
                b = stc[0] % 2
                stc[0] += 1
                S.op("act", lambda e: e.activation(out=vT[:, 8 + tl_, :], in_=PS[pi][:, :], func=AF.Copy), [("ps", pi)], ["vT"])
                S.op("dve", lambda e: e.tensor_copy(out=vst[b], in_=PS[pi][:, :]), [("ps", pi)], [("vst", b)])
                S.dma("sp", lambda e: e.dma_start(out=out_v[tl_ * 128:(tl_ + 1) * 128, :], in_=vst[b]), ("out", "v%d" % b), reads=[("vst", b)])
            proj_tm(wv, wk, hT, tl_ * 128, 128, vpost)
        S.barrier()
        chk("p3a3")
        for bi in range(4):
            def vspost(pi, bi=bi):
                b = stc[0] % 2
                stc[0] += 1
                S.op("act", lambda e: e.activation(out=vS[0:4, bi, :], in_=PS[pi][0:4, :], func=AF.Copy), [("ps", pi)], ["vS"])
                S.op("dve", lambda e: e.tensor_copy(out=vst[b][0:4, :], in_=PS[pi][0:4, :]), [("ps", pi)], [("vst", b)])
                S.dma("sp", lambda e: e.dma_start(out=out_v[1024 + bi * 4:1028 + bi * 4, :], in_=vst[b][0:4, :]), ("out", "v%d" % b), reads=[("vst", b)])
            proj_tm(wv, wk, hT, 1024 + bi * 4, 4, vspost)
        S.barrier()
        chk("p3a")
        R_M.reset()
        tmp_off.clear()

        att_tmp = [tmpf("att_t0"), tmpf("att_t1")]
        att_P = [tmpb("att_P0"), tmpb("att_P1"), tmpb("att_P2"), tmpb("att_P3")]
        att_o = tmpf("att_o")
        att_r = [tmpf("att_r0"), tmpf("att_r1")]
        pc = [0]

        def subln_finish(o_ap, n, out_ap, tag, gcol, extra=None, a3=None):
            sq = tmpf("sl_sq")[:, 0:n]
            rs = tmpf("sl_rs")[:, 0:n]
            o3, rs3 = o_ap, rs
            if a3 is not None:
                o3 = o_ap.rearrange("p (a b) -> p a b", a=a3)
                rs3 = rs.rearrange("p (a b) -> p a b", a=a3)
            S.op("act", lambda e: e.activation(out=sq, in_=o_ap, func=AF.Square), [tag], ["sl_sq"])
            S.op("pe", lambda e: e.matmul(PS[6][:, 0:n], lhsT=ones, rhs=sq, start=True, stop=True), ["sl_sq", "cst"], [("ps", 6)])
            rstd_from_ps(PS[6][:, 0:n], n, 1.0 / 128.0, rs, rs, [("ps", 6)], "sl_rs")
            if extra is None:
                S.op("dve", lambda e: e.scalar_tensor_tensor(out=out_ap, in0=o3, scalar=gcol, in1=rs3, op0=ALU.mult, op1=ALU.mult),
                     [tag, "sl_rs", "sc", "prm"], ["amT"])
            else:
                S.op("dve", lambda e: e.scalar_tensor_tensor(out=rs, in0=o_ap, scalar=gcol, in1=rs, op0=ALU.mult, op1=ALU.mult),
                     [tag, "sl_rs", "sc", "prm"], ["sl_rs"])
                S.op("dve", lambda e: e.tensor_tensor(out=out_ap, in0=rs, in1=extra[0], op=ALU.mult), ["sl_rs", extra[1]], ["amT"])

        att_tmp.append(tmpf("att_t2"))
        att_tmp.append(tmpf("att_t3"))
        SBK = [4, 5, 7, 6]
        LA = 3
        units = []
        for h in range(8):
            for qb in range(2):
                tiles = _tile_list(qb)
                for ti, kt in enumerate(tiles):
                    for m in range(2):
                        units.append((h, qb, ti, kt, m, len(tiles)))

        def stage_a(i, u):
            h, qb, ti, kt, m, nt = u
            kvh = h // 2
            sb, pb = i % 4, i % 4
            d0 = 1024 + 512 * qb - 128 * kt
            ws = 512 if d0 >= 128 else d0 + 384
            ndv = cst[:, C_ND + ws:C_ND + ws + 512]
            S.op("pe", lambda e: e.matmul(
                PS[SBK[sb]][:, :], lhsT=kT[64 * m:64 * m + 64, kvh, kt * 128:(kt + 1) * 128],
                rhs=qT[64 * m:64 * m + 64, h, qb * 512:(qb + 1) * 512], start=True, stop=True),
                ["kT", "qT"], [("ps", SBK[sb])])
            S.op("dve", lambda e: e.scalar_tensor_tensor(
                out=att_tmp[sb], in0=ndv, scalar=SLOPES[h], in1=PS[SBK[sb]][:, :], op0=ALU.mult, op1=ALU.add),
                [("ps", SBK[sb]), "cst"], [("att_t", sb)])
            bc = biasc[:, (h * 2 + qb) * 16 + kt:(h * 2 + qb) * 16 + kt + 1]
            S.op("act", lambda e: e.activation(out=att_P[pb], in_=att_tmp[sb], func=AF.Exp, bias=bc, scale=1.0),
                 [("att_t", sb), "cst"], [("att_P", pb)])

        def fin1(h, qb):
            for m in range(2):
                S.op("act", lambda e, m=m: e.activation(out=att_r[m], in_=PS[2 + m][:, :], func=AF.Ln), [("ps", 2 + m)], [("att_r", m)])
                S.op("act", lambda e, m=m: e.activation(out=att_r[m], in_=att_r[m], func=AF.Exp, scale=-1.0), [("att_r", m)], [("att_r", m)])
                S.op("dve", lambda e, m=m: e.tensor_tensor(out=att_r[m], in0=PS[m][:, :], in1=att_r[m], op=ALU.mult),
                     [("ps", m), ("att_r", m)], [("att_r", m)])
            S.op("dve", lambda e: e.scalar_tensor_tensor(out=att_o, in0=att_r[1], scalar=neglam, in1=att_r[0], op0=ALU.mult, op1=ALU.add),
                 [("att_r", 0), ("att_r", 1), "sc"], ["att_o"])
            sq = tmpf("sl_sq")
            S.op("act", lambda e: e.activation(out=sq, in_=att_o, func=AF.Square), ["att_o"], ["sl_sq"])

        def fin2(h, qb):
            sq = tmpf("sl_sq")
            rs = tmpf("sl_rs")
            S.op("pe", lambda e: e.matmul(PS[6][:, :], lhsT=ones, rhs=sq, start=True, stop=True), ["sl_sq", "cst"], [("ps", 6)])
            rstd_from_ps(PS[6][:, :], 512, 1.0 / 128.0, rs, rs, [("ps", 6)], "sl_rs")
            S.op("dve", lambda e: e.scalar_tensor_tensor(out=amT[:, h, qb * 512:(qb + 1) * 512], in0=att_o, scalar=sg8, in1=rs,
                                                         op0=ALU.mult, op1=ALU.mult), ["att_o", "sl_rs", "sc", "prm"], ["amT"])

        def stage_b(i, u):
            h, qb, ti, kt, m, nt = u
            kvh = h // 2
            pb = i % 4
            S.op("pe", lambda e: e.matmul(PS[m][:, :], lhsT=vT[:, kt, kvh * 128:(kvh + 1) * 128], rhs=att_P[pb],
                                          start=(ti == 0), stop=(ti == nt - 1)), [("att_P", pb), "vT"], [("ps", m)])
            S.op("pe", lambda e: e.matmul(PS[2 + m][:, :], lhsT=onesb, rhs=att_P[pb],
                                          start=(ti == 0), stop=(ti == nt - 1)), [("att_P", pb), "bfc"], [("ps", 2 + m)])

        pend = []
        NU = len(units)
        for i in range(NU + LA):
            if i < NU:
                stage_a(i, units[i])
            if i >= LA:
                u = units[i - LA]
                stage_b(i - LA, u)
                for p_ in pend:
                    p_[2] -= 1
                while pend and pend[0][2] <= 0:
                    hh, qq, _ = pend.pop(0)
                    fin2(hh, qq)
                if u[2] == u[5] - 1 and u[4] == 1:
                    fin1(u[0], u[1])
                    pend.append([u[0], u[1], 4])
        for hh, qq, _ in pend:
            fin2(hh, qq)
        S.barrier()
        chk("p3b")
        R_M.reset()
        tmp_off.clear()

        o_pos = R_X.alloc(2048 * 4)
        posT = f32v(o_pos, 2048, 3).rearrange("p (b i) -> p b i", b=16)
        o_R3 = R_X.alloc(512 * 4)
        R3 = f32v(o_R3, 512, 3)
        o_nb = R_X.alloc(64 * 4)
        newb = f32v(o_nb, 64, 4)
        o_idx = R_X.alloc(512 * 4)
        idx = i32v(o_idx, 512)
        S.dma("sp", lambda e: e.dma_start(out=posT.rearrange("p b i -> p (b i)"), in_=d_posT), ("ld", "c0"), writes=["posT"])
        S.dma("sp", lambda e: e.dma_start(out=R3, in_=d_R3), ("ld", "c1"), writes=["R3"])
        S.dma("sp", lambda e: e.dma_start(out=newb, in_=d_newb), ("ld", "c2"), writes=["newb"])
        tmpf("sl_sq", 64)
        tmpf("sl_rs", 64)
        o_pt_ = R_M.alloc(512 * 4)
        pti = i32v(o_pt_, 512)
        ptf = f32v(o_pt_, 512)
        pid = f32v(R_M.alloc(4), 1)
        S.dma("sp", lambda e: e.dma_start(out=pti, in_=d_pt.rearrange("b n -> (b n)").partition_broadcast(128)), ("ld", "c3"), writes=["pti"])
        S.op("pool", lambda e: e.iota(pid, pattern=[[0, 1]], base=0, channel_multiplier=1, allow_small_or_imprecise_dtypes=True), [], ["pid"])
        S.op("dve", lambda e: e.tensor_copy(out=ptf, in_=pti), ["pti"], ["ptf"])
        S.op("dve", lambda e: e.tensor_scalar(out=ptf, in0=ptf, scalar1=128.0, scalar2=pid, op0=ALU.mult, op1=ALU.add), ["ptf", "pid"], ["ptf"])
        S.op("dve", lambda e: e.tensor_copy(out=idx, in_=ptf), ["ptf"], ["idx"])
        NSL = 8
        kvr = [bfv(R_M.alloc(2048), 1024) for _ in range(NSL)]
        kr = [t[:, 0:512] for t in kvr]
        vr = [t[:, 512:1024] for t in kvr]
        qbd = bfv(R_M.alloc(64 * 2), 64).rearrange("p (k c) -> p k c", k=4)
        Ps = [bfv(R_M.alloc(1024), 512) for _ in range(2)]
        Pn = bfv(R_M.alloc(128), 64, 4)
        sa_t = f32v(R_M.alloc(64 * 4), 64)
        sa_r = f32v(R_M.alloc(64 * 4), 64)
        sa_o = f32v(R_M.alloc(32 * 4), 32)
        pgc = [0]
        for bi in range(4):
            S.op("dve", lambda e: e.memset(qbd.rearrange("p k c -> p (k c)"), 0.0), [], ["qbd"])
            for kvh in range(4):
                for g in range(2):
                    for m in range(2):
                        S.op("dve", lambda e, kvh=kvh, g=g, m=m, bi=bi: e.tensor_copy(
                            out=qbd[64 * m:64 * m + 64, kvh, m * 8 + g * 4:m * 8 + g * 4 + 4],
                            in_=qT[64 * m:64 * m + 64, kvh * 2 + g, 1024 + bi * 4:1028 + bi * 4]), ["qT"], ["qbd"])
            S.op("pe", lambda e: e.matmul(PS[2][:, 0:128], lhsT=zerosb[:, 0:128], rhs=zerosb[:, 0:128], start=True, stop=False),
                 ["bfc"], [("ps", 2)])
            NPB, NRB = 2, 4
            NBT = 128 // NPB

            def sa_stage_a(Bq):
                sb = Bq % 2
                B8, r0 = (Bq * NPB) // 8, (Bq * NPB) % 8
                W_ = NPB * 64
                S.op("pe", lambda e: e.matmul(PS[sb][:, 0:W_], lhsT=posT[:, B8, :], rhs=R3[:, r0 * 64:(r0 + NPB) * 64], start=True, stop=False),
                     ["posT", "R3"], [("ps", sb)])
                for r in range(NPB):
                    pg = Bq * NPB + r
                    sl = (Bq % NRB) * NPB + r
                    ia = idx[:, bi * 128 + pg:bi * 128 + pg + 1]
                    S.dma("pool", lambda e: e.indirect_dma_start(
                        out=kvr[sl], out_offset=None, in_=d_ckv, in_offset=bass.IndirectOffsetOnAxis(ap=ia, axis=0)),
                        ("kvr", sl), reads=["idx"], writes=[("kr", sl), ("vr", sl)])
                    for kvh in range(4):
                        S.op("pe", lambda e, kvh=kvh: e.matmul(
                            PS[sb][:, r * 64 + kvh * 16:r * 64 + kvh * 16 + 16], lhsT=kr[sl][:, kvh * 128:(kvh + 1) * 128],
                            rhs=qbd[:, kvh, :], start=False, stop=(r == NPB - 1 and kvh == 3)), [("kr", sl), "qbd"], [("ps", sb)])
                S.op("act", lambda e: e.activation(out=Ps[sb][:, 0:W_], in_=PS[sb][:, 0:W_], func=AF.Exp, bias=negM, scale=1.0),
                     [("ps", sb), "sc"], [("Ps", sb)])

            def sa_stage_b(Bq):
                sb = Bq % 2
                for r in range(NPB):
                    sl = (Bq % NRB) * NPB + r
                    for kvh in range(4):
                        S.op("pe", lambda e, kvh=kvh: e.matmul(
                            PS[2][:, kvh * 16:kvh * 16 + 16], lhsT=vr[sl][:, kvh * 128:(kvh + 1) * 128],
                            rhs=Ps[sb][:, r * 64 + kvh * 16:r * 64 + kvh * 16 + 16], start=False, stop=False),
                            [("vr", sl), ("Ps", sb)], [("ps", 2)])
                    S.op("pe", lambda e: e.matmul(PS[2][:, 64:128], lhsT=onesb, rhs=Ps[sb][:, r * 64:(r + 1) * 64],
                                                  start=False, stop=False), [("Ps", sb), "bfc"], [("ps", 2)])

            sa_stage_a(0)
            for Bq in range(NBT):
                if Bq + 1 < NBT:
                    sa_stage_a(Bq + 1)
                sa_stage_b(Bq)
            for kvh in range(4):
                S.op("pe", lambda e, kvh=kvh, bi=bi: e.matmul(PS[3][0:4, kvh * 16:kvh * 16 + 16], lhsT=kT[:, kvh, 2048 + bi * 4:2052 + bi * 4],
                                                              rhs=qbd[:, kvh, :], start=True, stop=True), ["kT", "qbd"], [("ps", 3)])
            S.op("dve", lambda e: e.tensor_tensor(out=sa_t[0:4, :], in0=PS[3][0:4, 0:64], in1=newb, op=ALU.add), [("ps", 3), "newb"], ["sa_t"])
            S.op("act", lambda e: e.activation(out=Pn, in_=sa_t[0:4, :], func=AF.Exp, bias=negM[0:4, :], scale=1.0), ["sa_t", "sc"], ["Pn"])
            for kvh in range(4):
                S.op("pe", lambda e, kvh=kvh, bi=bi: e.matmul(PS[2][:, kvh * 16:kvh * 16 + 16], lhsT=vS[0:4, bi, kvh * 128:(kvh + 1) * 128],
                                                              rhs=Pn[:, kvh * 16:kvh * 16 + 16], start=False, stop=False), ["vS", "Pn"], [("ps", 2)])
            S.op("pe", lambda e: e.matmul(PS[2][:, 64:128], lhsT=onesb[0:4, :], rhs=Pn, start=False, stop=True), ["Pn", "bfc"], [("ps", 2)])
            S.op("act", lambda e: e.activation(out=sa_r, in_=PS[2][:, 64:128], func=AF.Ln), [("ps", 2)], ["sa_r"])
            S.op("act", lambda e: e.activation(out=sa_r, in_=sa_r, func=AF.Exp, scale=-1.0), ["sa_r"], ["sa_r"])
            S.op("dve", lambda e: e.tensor_tensor(out=sa_r, in0=PS[2][:, 0:64], in1=sa_r, op=ALU.mult), [("ps", 2), "sa_r"], ["sa_r"])
            T4 = sa_r.rearrange("p (k m c) -> p k m c", k=4, m=2)
            S.op("dve", lambda e: e.scalar_tensor_tensor(out=sa_o.rearrange("p (k c) -> p k c", k=4), in0=T4[:, :, 1, :], scalar=neglam,
                                                         in1=T4[:, :, 0, :], op0=ALU.mult, op1=ALU.add), ["sa_r", "sc"], ["sa_o"])
            subln_finish(sa_o, 32, amT[:, 0:8, 1024 + bi * 4:1028 + bi * 4], "sa_o", sg8, a3=8)
        S.barrier()
        chk("p3c")
        R_M.reset()
        tmp_off.clear()
        R_X.reset()

        o_Sbf = R_X.alloc(17 * 128 * 2)
        Sbf = bfv(o_Sbf, 17 * 128).rearrange("p (c v) -> p c v", c=17)
        o_abf = R_X.alloc(256 * 2)
        abf = bfv(o_abf, 256).rearrange("p (j t) -> p j t", j=4)
        o_as = R_X.alloc(16 * 2)
        a_s = bfv(o_as, 16, 4)[:, 0:4]
        Ss2 = [f32v(R_M.alloc(128 * 4), 128) for _ in range(2)]
        Ssb2 = [bfv(R_M.alloc(128 * 2), 128) for _ in range(2)]
        Sso = [f32v(R_M.alloc(128 * 4), 128) for _ in range(2)]
        Spo = f32v(R_M.alloc(128 * 4), 128)
        m_mark = R_M.cur
        xm = R_X.cur
        ssc = [0]
        for hgp in range(2):
            R_X.cur = xm
            hb = hgrn_bufs(R_X, NT, 9, True)
            w, wk = load_w16(d_win[g_i]); g_i += 1
            wv = w.rearrange("p (b k c) -> p b k c", b=4, k=16)
            for hl in range(4):
                for (t0, n) in TBS:
                    proj_fm(wv, wk, hl, hT, t0, n, lambda pi, hl=hl, t0=t0, n=n, hb=hb: hf_post(
                        pi, hb, hl, hgp * 4 + hl, t0, n, True, n == 16, "o"))
            w, wk = load_w16(d_win[g_i]); g_i += 1
            wv = w.rearrange("p (b k c) -> p b k c", b=4, k=16)
            for hl in range(4):
                for (t0, n) in TBS:
                    def hqpost(pi, hl=hl, t0=t0, n=n, hb=hb):
                        sl_ = tmpf("hf_sg")[:, 0:n]
                        S.op("act", lambda e: e.activation(out=sl_, in_=PS[pi][:, 0:n], func=AF.Silu), [("ps", pi)], ["hf_sg"])
                        S.op("dve", lambda e: e.tensor_tensor(out=hb["qt"][:, hl, t0:t0 + n], in0=sl_, in1=hb["eg"][:, hl, t0:t0 + n], op=ALU.mult),
                             ["hf_sg", "oeg"], ["oqt"])
                    proj_fm(wv, wk, hl, hT, t0, n, hqpost)
            w, wk = load_w16(d_win[g_i]); g_i += 1
            wv = w.rearrange("p (k c) -> p k c", k=16)
            for tl_ in range(8):
                proj_tm(wv, wk, hT, tl_ * 128, 128, lambda pi, tl_=tl_, hb=hb: S.op(
                    "act", lambda e: e.activation(out=hb["hi"][:, tl_, :], in_=PS[pi][:, :], func=AF.Copy), [("ps", pi)], ["ohi"]))
            for bi in range(4):
                proj_tm(wv, wk, hT, 1024 + bi * 4, 4, lambda pi, bi=bi, hb=hb: S.op(
                    "act", lambda e: e.activation(out=hb["his"][0:4, bi, :], in_=PS[pi][0:4, :], func=AF.Copy), [("ps", pi)], ["ohis"]))
            w, wk = load_w16(d_win[g_i]); g_i += 1
            wv = w.rearrange("p (b k c) -> p b k c", b=4, k=16)
            for hl in range(4):
                for (t0, n) in TBS:
                    proj_fm(wv, wk, hl, hT, t0, n, lambda pi, hl=hl, t0=t0, n=n, hb=hb: S.op(
                        "act", lambda e: e.activation(out=hb["gate"][:, hl, t0:t0 + n], in_=PS[pi][:, 0:n], func=AF.Silu), [("ps", pi)], ["ogate"]))
            for hl in range(4):
                hglob = hgp * 4 + hl
                state_chain(hb, hl, hglob, 0, 16, 0, Sbf, "o")
                S.op("dve", lambda e, hglob=hglob: e.tensor_copy(out=Spo, in_=Sst[:, hglob, :]), [("S", hglob)], ["Spo"])
                S.dma("sp", lambda e, hglob=hglob: e.dma_start(out=out_s[:, hglob * 128:(hglob + 1) * 128], in_=Spo), ("out", "s"), reads=["Spo"])
                for qb in range(2):
                    pa = next_ps()
                    for c in range(8):
                        j, e2 = c // 2, c % 2
                        tk = qb * 512 + c * 64
                        S.op("pe", lambda e, j=j, e2=e2, tk=tk, pa=pa: e.matmul(
                            PS[pa][64 * e2:64 * e2 + 64, j * 64:(j + 1) * 64], lhsT=hb["kt"][:, hl, tk:tk + 64],
                            rhs=hb["qt"][:, hl, tk:tk + 64], start=True, stop=True), ["okt", "oqt"], [("ps", pa)])
                    S.op("dve", lambda e, pa=pa: e.tensor_tensor(out=abf, in0=PS[pa][:, 0:256].rearrange("p (j t) -> p j t", j=4),
                                                                 in1=tri.unsqueeze(1).to_broadcast([128, 4, 64]), op=ALU.mult),
                         [("ps", pa), "cst"], ["abf"])
                    po = next_ps()
                    for c in range(8):
                        j, e2 = c // 2, c % 2
                        tk = qb * 512 + c * 64
                        tl_ = qb * 4 + j
                        S.op("pe", lambda e, c=c, tk=tk, po=po: e.matmul(
                            PS[po][:, c * 64:(c + 1) * 64], lhsT=Sbf[:, qb * 8 + c, :], rhs=hb["qt"][:, hl, tk:tk + 64], start=True, stop=False),
                            ["oSbf", "oqt"], [("ps", po)])
                        S.op("pe", lambda e, c=c, j=j, e2=e2, tl_=tl_, po=po: e.matmul(
                            PS[po][:, c * 64:(c + 1) * 64], lhsT=hb["hi"][64 * e2:64 * e2 + 64, tl_, hl * 128:(hl + 1) * 128],
                            rhs=abf[64 * e2:64 * e2 + 64, j, :], start=False, stop=True), ["ohi", "abf"], [("ps", po)])
                    subln_finish(PS[po][:, :], 512, amT[:, 8 + hglob, qb * 512:(qb + 1) * 512], ("ps", po), hgg,
                                 extra=(hb["gate"][:, hl, qb * 512:(qb + 1) * 512], "ogate"))
                pos_ = next_ps()
                for bi in range(4):
                    tk = 1024 + bi * 4
                    b = ssc[0] % 2
                    ssc[0] += 1
                    Ss, Ssb = Ss2[b], Ssb2[b]
                    S.dma("sp", lambda e, bi=bi, hglob=hglob: e.dma_start(out=Ss, in_=d_st0[bi, hglob]), ("ld", "ss%d" % b), writes=[("Ss", b)])
                    S.op("act", lambda e: e.activation(out=Ssb, in_=Ss, func=AF.Copy), [("Ss", b)], [("Ssb", b)])
                    pa = next_ps()
                    S.op("pe", lambda e, tk=tk, pa=pa: e.matmul(PS[pa][0:4, 0:4], lhsT=hb["kt"][:, hl, tk:tk + 4], rhs=hb["qt"][:, hl, tk:tk + 4],
                                                                start=True, stop=True), ["okt", "oqt"], [("ps", pa)])
                    S.op("dve", lambda e, pa=pa: e.tensor_tensor(out=a_s, in0=PS[pa][0:4, 0:4], in1=tri[0:4, 0:4], op=ALU.mult), [("ps", pa), "cst"], ["a_s"])
                    S.op("pe", lambda e, tk=tk, bi=bi: e.matmul(PS[pos_][:, bi * 4:bi * 4 + 4], lhsT=Ssb, rhs=hb["qt"][:, hl, tk:tk + 4],
                                                                start=True, stop=False), [("Ssb", b), "oqt"], [("ps", pos_)])
                    S.op("pe", lambda e, bi=bi: e.matmul(PS[pos_][:, bi * 4:bi * 4 + 4], lhsT=hb["his"][0:4, bi, hl * 128:(hl + 1) * 128], rhs=a_s,
                                                         start=False, stop=True), ["ohis", "a_s"], [("ps", pos_)])
                    S.op("pe", lambda e, bi=bi, pa=pa: e.matmul(PS[pa][:, 128:256], lhsT=hb["khs"][0:4, bi, hl * 128:(hl + 1) * 128],
                                                                rhs=hb["his"][0:4, bi, hl * 128:(hl + 1) * 128], start=True, stop=True),
                         ["okhs", "ohis"], [("ps", pa)])
                    S.op("dve", lambda e, bi=bi, pa=pa, b=b: e.scalar_tensor_tensor(
                        out=Sso[b], in0=Ss, scalar=hb["d"][:, hl, 16 + bi:17 + bi], in1=PS[pa][:, 128:256], op0=ALU.mult, op1=ALU.add),
                        [("ps", pa), "od", ("Ss", b)], [("Sso", b)])
                    S.dma("sp", lambda e, bi=bi, hglob=hglob, b=b: e.dma_start(out=out_ss[bi, :, hglob * 128:(hglob + 1) * 128], in_=Sso[b]),
                          ("out", "ss%d" % b), reads=[("Sso", b)])
                subln_finish(PS[pos_][:, 0:16], 16, amT[:, 8 + hglob, 1024:1040], ("ps", pos_), hgg, extra=(hb["gate"][:, hl, 1024:1040], "ogate"))
            S.barrier()
        S.barrier()
        chk("p3d")
        R_M.reset()
        tmp_off.clear()
        R_X.reset()

        o_x1 = R_X.alloc(16 * NT * 4)
        DBG["x1"] = o_x1
        x1 = f32v(o_x1, 16 * NT).rearrange("p (k t) -> p k t", k=16)
        xrb = [f32v(R_M.alloc(512 * 4), 512) for _ in range(2)]
        xrc = [0]
        o4t = f32v(R_M.alloc(16 * 4), 16)
        for og in range(4):
            w, wk = load_w16(d_wout[og])
            wv = w.rearrange("p (b f c) -> p b f c", b=4, f=16)
            for cbl in range(4):
                cb = og * 4 + cbl
                for (t0, n) in TBS:
                    pi = next_ps()
                    for f in range(16):
                        S.op("pe", lambda e, f=f, pi=pi, cbl=cbl, t0=t0, n=n: e.matmul(PS[pi][:, 0:n], lhsT=wv[:, cbl, f, :], rhs=amT[:, f, t0:t0 + n],
                                                                                       start=(f == 0), stop=(f == 15)), [wk, "amT"], [("ps", pi)])
                    b = xrc[0] % 2
                    xrc[0] += 1
                    src = d_xo[:, cb, t0:t0 + n] if n == 512 else d_xs[:, cb, :]
                    S.dma("sp", lambda e, b=b, src=src, n=n: e.dma_start(out=xrb[b][:, 0:n], in_=src), ("ld", "xr%d" % b), writes=[("xr", b)])
                    if n == 512:
                        S.op("dve", lambda e, pi=pi, b=b, cb=cb, t0=t0: e.scalar_tensor_tensor(
                            out=x1[:, cb, t0:t0 + 512], in0=PS[pi][:, :], scalar=GA1[:, cb, 0:1], in1=xrb[b], op0=ALU.mult, op1=ALU.add),
                            [("ps", pi), ("xr", b), "modT"], [("x1", cb, t0)])
                    else:
                        S.op("dve", lambda e, pi=pi, cb=cb: e.tensor_tensor(
                            out=o4t.rearrange("p (b t) -> p b t", b=4), in0=PS[pi][:, 0:16].rearrange("p (b t) -> p b t", b=4),
                            in1=GA1[:, cb, 1:5].unsqueeze(2).to_broadcast([128, 4, 4]), op=ALU.mult), [("ps", pi), "modT"], ["o4t"])
                        S.op("dve", lambda e, b=b, cb=cb: e.tensor_tensor(out=x1[:, cb, 1024:1040], in0=o4t, in1=xrb[b][:, 0:16], op=ALU.add),
                             ["o4t", ("xr", b)], [("x1", cb, 1024)])
        S.barrier()
        chk("p4")
        R_M.reset()

        h2T = hT
        comb = f32v(R_M.alloc(144 * 4), 144).rearrange("p (t g e) -> p t g e", g=4, e=4)
        m5_mark = R_M.cur
        sqb = [f32v(R_M.alloc(512 * 4), 512) for _ in range(2)]
        rs2 = f32v(R_M.alloc(512 * 4), 512)
        tp2 = [f32v(R_M.alloc(512 * 4), 512) for _ in range(2)]
        h2f = [f32v(R_M.alloc(512 * 4), 512) for _ in range(2)]
        zf = f32v(R_M.alloc(180 * 4), 180)
        S.op("dve", lambda e: e.memset(zf, 0.0), [], ["zf"])
        S.op("pe", lambda e: e.matmul(PS[5][:, 0:180], lhsT=zf[:, 0:128], rhs=zf[:, 0:180], start=True, stop=False), ["zf"], [("ps", 5)])
        for bidx, (t0, n) in enumerate(TBS):
            rsel = [(0, n, 0)] if n == 512 else SAMPLE_RSEL
            for k in range(16):
                sb = k % 2
                S.op("act", lambda e, k=k, sb=sb, t0=t0, n=n: e.activation(out=sqb[sb][:, 0:n], in_=x1[:, k, t0:t0 + n], func=AF.Square), ["x1"], [("sq2", sb)])
                S.op("pe", lambda e, k=k, sb=sb, n=n: e.matmul(PS[7][:, 0:n], lhsT=ones, rhs=sqb[sb][:, 0:n], start=(k == 0), stop=(k == 15)),
                     [("sq2", sb), "cst"], [("ps", 7)])
            rstd_from_ps(PS[7][:, 0:n], n, 1.0 / 2048.0, rs2[:, 0:n], rs2[:, 0:n], [("ps", 7)], "rs2")
            for k in range(16):
                sb = k % 2
                for (c0, cn, r) in rsel:
                    S.op("dve", lambda e, k=k, sb=sb, c0=c0, cn=cn, r=r, t0=t0: e.scalar_tensor_tensor(
                        out=tp2[sb][:, c0:c0 + cn], in0=x1[:, k, t0 + c0:t0 + c0 + cn], scalar=G2[:, k, r:r + 1], in1=rs2[:, c0:c0 + cn],
                        op0=ALU.mult, op1=ALU.mult), ["x1", "G", "rs2"], [("tp2", sb)])
                    S.op("act", lambda e, k=k, sb=sb, c0=c0, cn=cn, r=r: e.activation(
                        out=h2f[sb][:, c0:c0 + cn], in_=tp2[sb][:, c0:c0 + cn], func=AF.Identity, bias=SH2[:, k, r:r + 1], scale=1.0),
                        [("tp2", sb), "modT"], [("h2f", sb)])
                S.op("dve", lambda e, k=k, sb=sb, t0=t0, n=n: e.tensor_copy(out=h2T[:, k, t0:t0 + n], in_=h2f[sb][:, 0:n]), [("h2f", sb)], ["hT"])
                ntile = max(1, n // 128)
                for j in range(ntile):
                    tl_ = t0 // 128 + j
                    mm_ = min(128, n)
                    S.op("pe", lambda e, k=k, sb=sb, j=j, tl_=tl_, mm_=mm_: e.matmul(
                        PS[5][0:mm_, tl_ * 20:tl_ * 20 + 20], lhsT=h2f[sb][:, j * 128:j * 128 + mm_], rhs=wr[:, k, :],
                        start=False, stop=(k == 15 and tl_ == 8)), [("h2f", sb), "wr"], [("ps", 5)])
        def rt(n):
            return f32v(R_M.alloc(n * 4), n)
        LG = rt(180).rearrange("p (t c) -> p t c", c=20)
        S.op("act", lambda e: e.activation(out=LG.rearrange("p t c -> p (t c)"), in_=PS[5][:, 0:180], func=AF.Copy), [("ps", 5)], ["LG"])
        lg = rt(36).rearrange("p (t g) -> p t g", g=4)
        mx = rt(9)
        oh = rt(36).rearrange("p (t g) -> p t g", g=4)
        ex = rt(36).rearrange("p (t g) -> p t g", g=4)
        den = rt(9)
        pgt = rt(9)
        le = rt(144).rearrange("p (t g e) -> p t g e", g=4, e=4)
        les = rt(36).rearrange("p (t e) -> p t e", e=4)
        m1 = rt(9)
        k1 = rt(36).rearrange("p (t e) -> p t e", e=4)
        le2 = rt(36).rearrange("p (t e) -> p t e", e=4)
        m2 = rt(9)
        k2 = rt(36).rearrange("p (t e) -> p t e", e=4)
        w1 = rt(9)
        w2 = rt(9)
        ce = rt(36).rearrange("p (t e) -> p t e", e=4)
        ce2 = rt(36).rearrange("p (t e) -> p t e", e=4)
        brg = prm[:, P_BRG:P_BRG + 4].unsqueeze(1).to_broadcast([128, 9, 4])
        bre = prm[:, P_BRE:P_BRE + 16].unsqueeze(1).to_broadcast([128, 9, 16])
        R = "rt"

        def dv(fn, rd=(), wrk=R):
            S.op("dve", fn, [R] + list(rd), [wrk])

        dv(lambda e: e.tensor_tensor(out=lg, in0=LG[:, :, 0:4], in1=brg, op=ALU.add), ["LG", "prm"])
        dv(lambda e: e.tensor_reduce(out=mx, in_=lg, axis=AX.X, op=ALU.max))
        dv(lambda e: e.tensor_tensor(out=oh, in0=lg, in1=mx.unsqueeze(2).to_broadcast([128, 9, 4]), op=ALU.is_equal))
        dv(lambda e: e.tensor_tensor(out=ex, in0=lg, in1=mx.unsqueeze(2).to_broadcast([128, 9, 4]), op=ALU.subtract))
        S.op("act", lambda e: e.activation(out=ex, in_=ex, func=AF.Exp), [R], [R])
        dv(lambda e: e.tensor_reduce(out=den, in_=ex, axis=AX.X, op=ALU.add))
        dv(lambda e: e.reciprocal(out=pgt, in_=den))
        dv(lambda e: e.tensor_tensor(out=le.rearrange("p t g e -> p t (g e)"), in0=LG[:, :, 4:20], in1=bre, op=ALU.add), ["LG", "prm"])
        dv(lambda e: e.tensor_tensor(out=le, in0=le, in1=oh.unsqueeze(3).to_broadcast([128, 9, 4, 4]), op=ALU.mult))
        dv(lambda e: e.tensor_reduce(out=les, in_=le.rearrange("p t g e -> p t e g"), axis=AX.X, op=ALU.add))
        dv(lambda e: e.tensor_reduce(out=m1, in_=les, axis=AX.X, op=ALU.max))
        dv(lambda e: e.tensor_tensor(out=k1, in0=les, in1=m1.unsqueeze(2).to_broadcast([128, 9, 4]), op=ALU.is_equal))
        dv(lambda e: e.scalar_tensor_tensor(out=le2, in0=k1, scalar=-1.0e30, in1=les, op0=ALU.mult, op1=ALU.add))
        dv(lambda e: e.tensor_reduce(out=m2, in_=le2, axis=AX.X, op=ALU.max))
        dv(lambda e: e.tensor_tensor(out=k2, in0=le2, in1=m2.unsqueeze(2).to_broadcast([128, 9, 4]), op=ALU.is_equal))
        dv(lambda e: e.tensor_tensor(out=w2, in0=m2, in1=m1, op=ALU.subtract))
        S.op("act", lambda e: e.activation(out=w2, in_=w2, func=AF.Exp), [R], [R])
        dv(lambda e: e.tensor_scalar(out=w1, in0=w2, scalar1=1.0, scalar2=None, op0=ALU.add))
        dv(lambda e: e.reciprocal(out=w1, in_=w1))
        dv(lambda e: e.tensor_tensor(out=w2, in0=w2, in1=w1, op=ALU.mult))
        dv(lambda e: e.tensor_tensor(out=w1, in0=w1, in1=pgt, op=ALU.mult))
        dv(lambda e: e.tensor_tensor(out=w2, in0=w2, in1=pgt, op=ALU.mult))
        dv(lambda e: e.tensor_tensor(out=ce, in0=k1, in1=w1.unsqueeze(2).to_broadcast([128, 9, 4]), op=ALU.mult))
        dv(lambda e: e.tensor_tensor(out=ce2, in0=k2, in1=w2.unsqueeze(2).to_broadcast([128, 9, 4]), op=ALU.mult))
        dv(lambda e: e.tensor_tensor(out=ce, in0=ce, in1=ce2, op=ALU.add))
        dv(lambda e: e.tensor_tensor(out=comb, in0=oh.unsqueeze(3).to_broadcast([128, 9, 4, 4]),
                                     in1=ce.unsqueeze(2).to_broadcast([128, 9, 4, 4]), op=ALU.mult), wrk="comb")
        S.barrier()
        chk("p5")

        R_M.cur = m5_mark
        R_RING.reset()
        mslot = [R_RING.alloc(4096) for _ in range(8)]
        msl = [0]

        def load_w4(src_ap):
            slot = msl[0] % 8
            msl[0] += 1
            v = bfv(mslot[slot], 2048)
            key = ("mring", slot)
            S.dma("pool", lambda e: e.dma_start(out=v, in_=src_ap), ("mring", slot), writes=[key])
            return v, key

        o_hid = R_AM.base
        hid = bfv(o_hid, 4 * NT).rearrange("p (f t) -> p f t", f=4)
        cbc = [f32v(R_AM.base + 4 * NT * 2 + i * NT * 4, NT) for i in range(2)]
        De = [f32v(R_M.alloc(128 * 4), 128) for _ in range(2)]
        msA = [f32v(R_M.alloc(512 * 4), 512) for _ in range(2)]
        mtT = [f32v(R_M.alloc(512 * 4), 512) for _ in range(2)]
        mo4 = f32v(R_M.alloc(16 * 4), 16)
        dec = [0]
        mc = [0]
        def build_cbc(ex_):
            g_, e_ = ex_ // 4, ex_ % 4
            cb_ = cbc[ex_ % 2]
            for bidx, (t0, n) in enumerate(TBS):
                pi = next_ps()
                ntile = max(1, n // 128)
                for j in range(ntile):
                    tl_ = t0 // 128 + j
                    mm_ = min(128, n)
                    b = dec[0] % 2
                    dec[0] += 1
                    S.op("dve", lambda e, b=b, tl_=tl_, mm_=mm_: e.tensor_scalar(
                        out=De[b][0:mm_, 0:mm_], in0=ident[0:mm_, 0:mm_], scalar1=comb[0:mm_, tl_, g_, e_:e_ + 1], scalar2=None, op0=ALU.mult),
                        ["comb", "cst"], [("De", b)])
                    S.op("pe", lambda e, b=b, j=j, mm_=mm_, pi=pi: e.matmul(PS[pi][:, j * 128:j * 128 + mm_], lhsT=ones[0:mm_, :], rhs=De[b][0:mm_, 0:mm_],
                                                                            start=True, stop=True), [("De", b), "cst"], [("ps", pi)])
                S.op("act", lambda e, pi=pi, t0=t0, n=n: e.activation(out=cb_[:, t0:t0 + n], in_=PS[pi][:, 0:n], func=AF.Copy), [("ps", pi)], [("cbc", ex_ % 2)])

        build_cbc(0)
        for ex_ in range(16):
            g_, e_ = ex_ // 4, ex_ % 4
            cb_ = cbc[ex_ % 2]
            for fb in range(4):
                wg, wgk = load_w4(d_wgu[ex_, fb * 2])
                wu, wuk = load_w4(d_wgu[ex_, fb * 2 + 1])
                wgv = wg.rearrange("p (k c) -> p k c", k=16)
                wuv = wu.rearrange("p (k c) -> p k c", k=16)
                for (t0, n) in TBS:
                    pa, pu = next_ps(), next_ps()
                    for k in range(16):
                        S.op("pe", lambda e, k=k, pa=pa, t0=t0, n=n: e.matmul(PS[pa][:, 0:n], lhsT=wgv[:, k, :], rhs=h2T[:, k, t0:t0 + n],
                                                                              start=(k == 0), stop=(k == 15)), [wgk, "hT"], [("ps", pa)])
                    for k in range(16):
                        S.op("pe", lambda e, k=k, pu=pu, t0=t0, n=n: e.matmul(PS[pu][:, 0:n], lhsT=wuv[:, k, :], rhs=h2T[:, k, t0:t0 + n],
                                                                              start=(k == 0), stop=(k == 15)), [wuk, "hT"], [("ps", pu)])
                    b = mc[0] % 2
                    mc[0] += 1
                    S.op("act", lambda e, pa=pa, b=b, n=n: e.activation(out=msA[b][:, 0:n], in_=PS[pa][:, 0:n], func=AF.Silu), [("ps", pa)], [("msA", b)])
                    S.op("dve", lambda e, pu=pu, b=b, t0=t0, n=n: e.tensor_tensor(out=mtT[b][:, 0:n], in0=PS[pu][:, 0:n], in1=cb_[:, t0:t0 + n], op=ALU.mult),
                         [("ps", pu), ("cbc", ex_ % 2)], [("mtT", b)])
                    S.op("dve", lambda e, b=b, fb=fb, t0=t0, n=n: e.tensor_tensor(out=hid[:, fb, t0:t0 + n], in0=msA[b][:, 0:n], in1=mtT[b][:, 0:n], op=ALU.mult),
                         [("msA", b), ("mtT", b)], ["hid"])
            if ex_ + 1 < 16:
                build_cbc(ex_ + 1)
            for cg in range(4):
                wd, wdk = load_w4(d_wdn[ex_, cg])
                wdv = wd.rearrange("p (c f o) -> p c f o", c=4, f=4)
                for cbl in range(4):
                    cb = cg * 4 + cbl
                    for (t0, n) in TBS:
                        pi = next_ps()
                        for fb in range(4):
                            S.op("pe", lambda e, fb=fb, pi=pi, cbl=cbl, t0=t0, n=n: e.matmul(PS[pi][:, 0:n], lhsT=wdv[:, cbl, fb, :], rhs=hid[:, fb, t0:t0 + n],
                                                                                             start=(fb == 0), stop=(fb == 3)), [wdk, "hid"], [("ps", pi)])
                        if n == 512:
                            S.op("dve", lambda e, pi=pi, cb=cb, t0=t0: e.scalar_tensor_tensor(
                                out=x1[:, cb, t0:t0 + 512], in0=PS[pi][:, :], scalar=GA2[:, cb, 0:1], in1=x1[:, cb, t0:t0 + 512], op0=ALU.mult, op1=ALU.add),
                                [("ps", pi), "modT", ("x1", cb, t0)], [("x1", cb, t0)])
                        else:
                            S.op("dve", lambda e, pi=pi, cb=cb: e.tensor_tensor(
                                out=mo4.rearrange("p (b t) -> p b t", b=4), in0=PS[pi][:, 0:16].rearrange("p (b t) -> p b t", b=4),
                                in1=GA2[:, cb, 1:5].unsqueeze(2).to_broadcast([128, 4, 4]), op=ALU.mult), [("ps", pi), "modT"], ["mo4"])
                            S.op("dve", lambda e, cb=cb: e.tensor_tensor(out=x1[:, cb, 1024:1040], in0=mo4, in1=x1[:, cb, 1024:1040], op=ALU.add),
                                 ["mo4", ("x1", cb, 1024)], [("x1", cb, 1024)])
        S.barrier()
        S.dma("sp", lambda e: e.dma_start(out=out_yT, in_=x1.rearrange("p k t -> p (k t)")), ("out", "y"), reads=["x1"])

    except _Stop:
        pass
    if stop is not None:
        S.barrier()
        d_dbg = dout("dbg", [128, ARENA // 4])
        S.dma("sp", lambda e: e.dma_start(out=d_dbg, in_=A[:, :]), ("out", "dbg"))
    S.emit(nc, es)
    es.close()
    return nc


def _fm(a):
    T = a.shape[0]
    return np.ascontiguousarray(a.reshape(T, 16, 128).transpose(2, 1, 0))


def _wblocks_fm(w, cols):
    out = np.empty((128, 4, 16, 128), np.float32)
    for b, c0 in enumerate(cols):
        out[:, b] = w[:, c0:c0 + 128].reshape(16, 128, 128).transpose(1, 0, 2)
    return out.reshape(128, 8192)


def _wgroup_tm(w, c0):
    return np.ascontiguousarray(w[:, c0:c0 + 512].reshape(16, 128, 512).transpose(1, 0, 2)).reshape(128, 8192)


_NC_CACHE = {}


def prep(x_prompt, x_sample, cache_k, cache_v, state_hgrn, page_table, c_prompt, c_sample,
           norm1_g, norm2_g, w_ada, b_ada, w_in, q_norm_g, k_norm_g,
           lambda_q1, lambda_k1, lambda_q2, lambda_k2, subln_g, hg_lower_bound, hg_norm_g, w_out,
           w_router_group, b_router_group, w_router_expert, b_router_expert,
           w_exp_gate, w_exp_up, w_exp_down, small_cache=False):
    f = np.float32
    x_prompt = np.asarray(x_prompt, f); x_sample = np.asarray(x_sample, f)
    cache_k = np.asarray(cache_k, f); cache_v = np.asarray(cache_v, f)
    w_ada = np.asarray(w_ada, f)[0]; w_in = np.asarray(w_in, f)[0]; w_out = np.asarray(w_out, f)[0]
    wg_ = np.asarray(w_exp_gate, f)[0]; wu_ = np.asarray(w_exp_up, f)[0]; wd_ = np.asarray(w_exp_down, f)[0]

    wada = np.stack([_wblocks_fm(w_ada, [(g * 4 + b) * 128 for b in range(4)]) for g in range(24)])
    QC, KC, VC, HQ, HF, HI, HG = 0, 1024, 1536, 2048, 3072, 4096, 5120
    groups = []
    groups.append(_wblocks_fm(w_in, [KC + 128 * b for b in range(4)]))
    groups.append(_wgroup_tm(w_in, VC))
    for hgp in range(2):
        groups.append(_wblocks_fm(w_in, [HF + hgp * 512 + 128 * b for b in range(4)]))
        groups.append(_wgroup_tm(w_in, HI + hgp * 512))
    for qg in range(2):
        groups.append(_wblocks_fm(w_in, [QC + qg * 512 + 128 * b for b in range(4)]))
    groups.append(_wblocks_fm(w_in, [KC + 128 * b for b in range(4)]))
    groups.append(_wgroup_tm(w_in, VC))
    for hgp in range(2):
        groups.append(_wblocks_fm(w_in, [HF + hgp * 512 + 128 * b for b in range(4)]))
        groups.append(_wblocks_fm(w_in, [HQ + hgp * 512 + 128 * b for b in range(4)]))
        groups.append(_wgroup_tm(w_in, HI + hgp * 512))
        groups.append(_wblocks_fm(w_in, [HG + hgp * 512 + 128 * b for b in range(4)]))
    win = np.stack(groups)
    wout = np.stack([_wblocks_fm(w_out, [(g * 4 + b) * 128 for b in range(4)]) for g in range(4)])
    wgu = np.empty((16, 8, 128, 2048), f)
    for e in range(16):
        for fb in range(4):
            wgu[e, fb * 2] = wg_[e][:, fb * 128:(fb + 1) * 128].reshape(16, 128, 128).transpose(1, 0, 2).reshape(128, 2048)
            wgu[e, fb * 2 + 1] = wu_[e][:, fb * 128:(fb + 1) * 128].reshape(16, 128, 128).transpose(1, 0, 2).reshape(128, 2048)
    wdn = np.ascontiguousarray(wd_.reshape(16, 4, 128, 4, 4, 128).transpose(0, 3, 2, 4, 1, 5)).reshape(16, 4, 128, 2048)
    if small_cache:
        ckv = np.zeros((128, 1024), f)
    else:
        ckv = np.empty((5120 * 128, 1024), f)
        ckv[:, 0:512] = cache_k[0].transpose(0, 3, 2, 1).reshape(5120 * 128, 512)
        ckv[:, 512:1024] = cache_v[0].reshape(5120 * 128, 512)
    posT, R3, newb = make_sample_tables()
    wr = np.concatenate([np.asarray(w_router_group, f)[0], np.asarray(w_router_expert, f)[0].transpose(1, 0, 2).reshape(2048, 16)], axis=1)
    wr = np.ascontiguousarray(wr.reshape(16, 128, 20).transpose(1, 0, 2)).reshape(128, 320)

    prm0 = np.zeros((128, NPRM), f)
    prm0[:, P_N1:P_N1 + 16] = np.asarray(norm1_g, f)[0].reshape(16, 128).T
    prm0[:, P_N2:P_N2 + 16] = np.asarray(norm2_g, f)[0].reshape(16, 128).T
    prm0[:, P_BT:P_BT + 96] = np.asarray(b_ada, f)[0].reshape(96, 128).T
    prm0[:, P_GQ] = np.tile(np.asarray(q_norm_g, f)[0], 2)
    prm0[:, P_GK] = np.tile(np.asarray(k_norm_g, f)[0], 2)
    prm0[:, P_SG] = np.asarray(subln_g, f)[0]
    prm0[:, P_HGG] = np.asarray(hg_norm_g, f)[0]
    lbv = np.asarray(hg_lower_bound, f)
    prm0[:, P_LB:P_LB + 8] = lbv[0].reshape(8, 128).T
    prm0[:, P_LB + 8:P_LB + 16] = lbv[1].reshape(8, 128).T
    prm0[:, P_GQR:P_GQR + 64] = np.asarray(q_norm_g, f)[0][None]
    prm0[:, P_GKR:P_GKR + 64] = np.asarray(k_norm_g, f)[0][None]
    prm0[:, P_LAMR:P_LAMR + 256] = np.concatenate([np.asarray(a, f)[0] for a in (lambda_q1, lambda_k1, lambda_q2, lambda_k2)])[None]
    prm0[:, P_BRG:P_BRG + 4] = np.asarray(b_router_group, f)[0][None]
    prm0[:, P_BRE:P_BRE + 16] = np.asarray(b_router_expert, f)[0].reshape(16)[None]

    in_maps = []
    pt = np.asarray(page_table, np.int32)
    for c in range(8):
        b, half = c // 2, c % 2
        prm = prm0.copy()
        prm[:, P_FLAG] = float(half)
        crow = np.concatenate([np.asarray(c_prompt, f)[b:b + 1], np.asarray(c_sample, f)[4 * c:4 * c + 4]], axis=0)
        cT = np.ascontiguousarray(crow.reshape(5, 16, 128).transpose(2, 1, 0)).reshape(128, 80)
        in_maps.append({
            "cst": make_consts(half), "prm": prm, "cT": cT, "wr": wr,
            "xo": _fm(x_prompt[b, half * 1024:(half + 1) * 1024]),
            "xp": _fm(x_prompt[b, 0:1024]),
            "xs": _fm(x_sample[4 * c:4 * c + 4].reshape(16, 2048)),
            "wada": wada, "win": win, "wout": wout, "wgu": wgu, "wdn": wdn,
            "ckv": ckv, "pt": np.ascontiguousarray(pt[4 * c:4 * c + 4]),
            "st0": np.ascontiguousarray(np.asarray(state_hgrn, f)[0, 4 * c:4 * c + 4]),
            "posT": posT, "R3": R3, "newb": newb,
        })
    return in_maps


def kernel(**inputs):
    f = np.float32
    in_maps = prep(**inputs)
    if "nc" not in _NC_CACHE:
        _NC_CACHE["nc"] = build()
    res = run_bass_kernel_spmd(_NC_CACHE["nc"], in_maps, core_ids=list(range(8)))
    R = res.results

    y_prompt = np.empty((4, 2048, 2048), f); y_sample = np.empty((32, 4, 2048), f)
    nk_p = np.empty((1, 4, 2048, 4, 128), f); nv_p = np.empty((1, 4, 2048, 4, 128), f)
    nk_s = np.empty((1, 32, 4, 4, 128), f); nv_s = np.empty((1, 32, 4, 4, 128), f)
    ns_p = np.empty((1, 4, 8, 128, 128), f); ns_s = np.empty((1, 32, 8, 128, 128), f)
    for c in range(8):
        b, half = c // 2, c % 2
        yT = R[c]["yT"].reshape(128, 16, NT)
        yt = yT.transpose(2, 1, 0).reshape(NT, 2048)
        y_prompt[b, half * 1024:(half + 1) * 1024] = yt[:1024]
        y_sample[4 * c:4 * c + 4] = yt[1024:].reshape(4, 4, 2048)
        kTo = R[c]["kTo"].reshape(128, 4, NT).transpose(2, 1, 0)
        nk_p[0, b, half * 1024:(half + 1) * 1024] = kTo[:1024]
        nk_s[0, 4 * c:4 * c + 4] = kTo[1024:].reshape(4, 4, 4, 128)
        vo = R[c]["vo"].reshape(NT, 4, 128)
        nv_p[0, b, half * 1024:(half + 1) * 1024] = vo[:1024]
        nv_s[0, 4 * c:4 * c + 4] = vo[1024:].reshape(4, 4, 4, 128)
        if half == 1:
            ns_p[0, b] = R[c]["so"].reshape(128, 8, 128).transpose(1, 0, 2)
        ns_s[0, 4 * c:4 * c + 4] = R[c]["sso"].reshape(4, 128, 8, 128).transpose(0, 2, 1, 3)
    return (y_prompt, y_sample, nk_p, nv_p, nk_s, nv_s, ns_p, ns_s)
```
